# Optimizing a Trainium2 kernel written in Bass

```python
import jax, jax.numpy as jnp
from jax import lax
import numpy as np

D_MODEL = 1024
BATCH = 4
SEQ = 8192
DEPTH = 2

MEM_LEN = 256
EPS = 1e-6
MIX_WIDTH = D_MODEL
ML_HEADS = 4
ML_WIDTH = MIX_WIDTH // 2
ML_HD = ML_WIDTH // ML_HEADS
ML_CHUNK = 64
RG_WIDTH = MIX_WIDTH - ML_WIDTH
RG_BLOCKS = 4
RG_BW = RG_WIDTH // RG_BLOCKS
RG_CONV = 4
RG_C = 8.0
IN_COLS = 4 * ML_WIDTH + 2 * ML_HEADS + 2 * RG_WIDTH
SW_HEADS = 16
SW_KV_HEADS = 4
SW_HD = D_MODEL // SW_HEADS
SW_GROUP = SW_HEADS // SW_KV_HEADS
SW_WINDOW = 128
SW_BLOCK = 128
QKV_COLS = (SW_HEADS + 2 * SW_KV_HEADS) * SW_HD
ROPE_THETA = 10000.0
X_HEADS = 4
X_HD = D_MODEL // X_HEADS
D_FF = 4 * D_MODEL
N_EVEN = (DEPTH + 1) // 2
N_ODD = DEPTH // 2

kernel_name = 'hybrid_mlstm_rglru_swa_sink_block'


def rms_norm(x, g):
    xf = x.astype(jnp.float32)
    y = xf * lax.rsqrt(jnp.mean(xf * xf, axis=-1, keepdims=True) + EPS)
    return (y * g.astype(jnp.float32)).astype(x.dtype)


def rotary(t, positions):
    d = t.shape[-1]
    half = d // 2
    inv = jnp.power(ROPE_THETA, -jnp.arange(half, dtype=jnp.float32) * 2.0 / d)
    ang = positions.astype(jnp.float32)[..., None] * inv
    cos = jnp.cos(ang)[:, :, None, :]
    sin = jnp.sin(ang)[:, :, None, :]
    t1 = t[..., :half].astype(jnp.float32)
    t2 = t[..., half:].astype(jnp.float32)
    return jnp.concatenate([t1 * cos - t2 * sin, t2 * cos + t1 * sin], axis=-1).astype(t.dtype)


def mlstm_chunkwise(q, k, v, i_pre, f_pre):
    B, S, H, dh = q.shape
    nc = S // ML_CHUNK
    f32 = jnp.float32

    def to_chunks(t):
        t = t.astype(f32).reshape((B, nc, ML_CHUNK) + t.shape[2:])
        return jnp.moveaxis(t, (1, 3), (0, 2))

    qc = to_chunks(q)
    kc = to_chunks(k) * (dh ** -0.5)
    vc = to_chunks(v)
    logi = to_chunks(i_pre)
    logf = to_chunks(jax.nn.log_sigmoid(f_pre.astype(f32)))
    causal = jnp.tril(jnp.ones((ML_CHUNK, ML_CHUNK), dtype=bool))

    def step(carry, inp):
        C, n, m = carry
        qb, kb, vb, li, lf = inp
        b = jnp.cumsum(lf, axis=-1)
        dmat = b[..., :, None] - b[..., None, :] + li[..., None, :]
        dmat = jnp.where(causal, dmat, -jnp.inf)
        m_t = jnp.maximum(b + m[..., None], jnp.max(dmat, axis=-1))
        decay_prev = jnp.exp(b + m[..., None] - m_t)
        s = jnp.einsum('bhtd,bhsd->bhts', qb, kb) * jnp.exp(dmat - m_t[..., None])
        num = jnp.einsum('bhts,bhsd->bhtd', s, vb) + decay_prev[..., None] * jnp.einsum('bhtd,bhde->bhte', qb, C)
        den = jnp.sum(s, axis=-1) + decay_prev * jnp.einsum('bhtd,bhd->bht', qb, n)
        h = num / jnp.maximum(jnp.abs(den), jnp.exp(-m_t))[..., None]
        b_last = b[..., -1]
        g = b_last[..., None] - b + li
        m_new = jnp.maximum(b_last + m, jnp.max(g, axis=-1))
        w_prev = jnp.exp(b_last + m - m_new)
        w_s = jnp.exp(g - m_new[..., None])
        C_new = w_prev[..., None, None] * C + jnp.einsum('bhs,bhsd,bhse->bhde', w_s, kb, vb)
        n_new = w_prev[..., None] * n + jnp.einsum('bhs,bhsd->bhd', w_s, kb)
        return (C_new, n_new, m_new), h

    init = (jnp.zeros((B, H, dh, dh), f32), jnp.zeros((B, H, dh), f32), jnp.zeros((B, H), f32))
    _, hs = lax.scan(step, init, (qc, kc, vc, logi, logf))
    hs = jnp.moveaxis(hs, (0, 2), (1, 3))
    return hs.reshape(B, S, H * dh)


def rg_lru_branch(rx, conv_w, conv_b, rg_wa, rg_ba, rg_wx, rg_bx, rg_lambda):
    B, S, _ = rx.shape
    xc = lax.conv_general_dilated(rx, conv_w[:, None, :], window_strides=(1,),
                                  padding=[(RG_CONV - 1, 0)],
                                  dimension_numbers=('NWC', 'WIO', 'NWC'),
                                  feature_group_count=RG_WIDTH) + conv_b
    xb = xc.reshape(B, S, RG_BLOCKS, RG_BW)
    r = jax.nn.sigmoid((jnp.einsum('bsnc,ncd->bsnd', xb, rg_wa).reshape(B, S, RG_WIDTH) + rg_ba).astype(jnp.float32))
    i = jax.nn.sigmoid((jnp.einsum('bsnc,ncd->bsnd', xb, rg_wx).reshape(B, S, RG_WIDTH) + rg_bx).astype(jnp.float32))
    log_a = -RG_C * r * jax.nn.softplus(-rg_lambda.astype(jnp.float32))
    a = jnp.exp(log_a)
    u = jnp.sqrt(-jnp.expm1(2.0 * log_a)) * (i * xc.astype(jnp.float32))

    def combine(e1, e2):
        a1, b1 = e1
        a2, b2 = e2
        return a1 * a2, a2 * b1 + b2

    _, h = lax.associative_scan(combine, (a, u), axis=1)
    return h


def mlstm_rglru_mixer(u, w_in, b_if, conv_w, conv_b, rg_wa, rg_ba, rg_wx, rg_bx, rg_lambda, w_out):
    B, S, _ = u.shape
    z = u @ w_in
    q, k, v, o, gif, rx, ry = jnp.split(z, [ML_WIDTH, 2 * ML_WIDTH, 3 * ML_WIDTH, 4 * ML_WIDTH,
                                            4 * ML_WIDTH + 2 * ML_HEADS,
                                            4 * ML_WIDTH + 2 * ML_HEADS + RG_WIDTH], axis=-1)
    gif = gif + b_if
    i_pre, f_pre = gif[..., :ML_HEADS], gif[..., ML_HEADS:]
    hm = mlstm_chunkwise(q.reshape(B, S, ML_HEADS, ML_HD), k.reshape(B, S, ML_HEADS, ML_HD),
                         v.reshape(B, S, ML_HEADS, ML_HD), i_pre, f_pre)
    y_a = jax.nn.sigmoid(o) * hm.astype(u.dtype)
    h_rg = rg_lru_branch(rx, conv_w, conv_b, rg_wa, rg_ba, rg_wx, rg_bx, rg_lambda)
    y_b = jax.nn.gelu(ry) * h_rg.astype(u.dtype)
    return jnp.concatenate([y_a, y_b], axis=-1) @ w_out


def sliding_window_sink_attention(u, positions, w_qkv, sinks, w_o):
    B, S, _ = u.shape
    qkv = u @ w_qkv
    q, k, v = jnp.split(qkv, [SW_HEADS * SW_HD, (SW_HEADS + SW_KV_HEADS) * SW_HD], axis=-1)
    q = rotary(q.reshape(B, S, SW_HEADS, SW_HD), positions).reshape(B, S, SW_KV_HEADS, SW_GROUP, SW_HD)
    k = rotary(k.reshape(B, S, SW_KV_HEADS, SW_HD), positions)
    v = v.reshape(B, S, SW_KV_HEADS, SW_HD)
    pad = ((0, 0), (SW_BLOCK, 0), (0, 0), (0, 0))
    kp = jnp.pad(k, pad)
    vp = jnp.pad(v, pad)
    nb = S // SW_BLOCK
    r = jnp.arange(SW_BLOCK)[:, None]
    c = jnp.arange(2 * SW_BLOCK)[None, :]
    diff = r + SW_BLOCK - c
    band = (diff >= 0) & (diff < SW_WINDOW)
    sink = sinks.astype(jnp.float32).reshape(SW_KV_HEADS, SW_GROUP)[None, :, :, None]
    scale = SW_HD ** -0.5

    def block(j):
        start = j * SW_BLOCK
        qb = lax.dynamic_slice_in_dim(q, start, SW_BLOCK, axis=1)
        kb = lax.dynamic_slice_in_dim(kp, start, 2 * SW_BLOCK, axis=1)
        vb = lax.dynamic_slice_in_dim(vp, start, 2 * SW_BLOCK, axis=1)
        s = jnp.einsum('blkgd,bckd->bkglc', qb, kb).astype(jnp.float32) * scale
        valid = band & (start + c - SW_BLOCK >= 0)
        s = jnp.where(valid, s, -jnp.inf)
        m = jnp.maximum(jnp.max(s, axis=-1), sink)
        p = jnp.exp(s - m[..., None])
        den = jnp.sum(p, axis=-1) + jnp.exp(sink - m)
        ob = jnp.einsum('bkglc,bckd->blkgd', p, vb.astype(jnp.float32))
        ob = ob / jnp.moveaxis(den, -1, 1)[..., None]
        return ob.astype(u.dtype)

    out = lax.map(block, jnp.arange(nb))
    out = jnp.moveaxis(out, 0, 1).reshape(B, S, SW_HEADS * SW_HD)
    return out @ w_o


def memory_cross_attention(u, mem, g_mem, w_q, w_kv, w_o):
    B, S, _ = u.shape
    M = mem.shape[1]
    q = (u @ w_q).reshape(B, S, X_HEADS, X_HD)
    kv = (rms_norm(mem, g_mem) @ w_kv).reshape(B, M, 2, X_HEADS, X_HD)
    k, v = kv[:, :, 0], kv[:, :, 1]
    s = jnp.einsum('bshd,bmhd->bhsm', q, k).astype(jnp.float32) * (X_HD ** -0.5)
    p = jax.nn.softmax(s, axis=-1)
    o = jnp.einsum('bhsm,bmhd->bshd', p.astype(v.dtype), v).reshape(B, S, D_MODEL)
    return o @ w_o


def squared_relu_mlp(u, w1, w2):
    return jnp.square(jax.nn.relu(u @ w1)) @ w2


def setup_inputs(seed: int = 0) -> dict:
    key = jax.random.key(seed)
    ks = iter(jax.random.split(key, 40))

    def nrm(shape, scale):
        return jax.random.normal(next(ks), shape, jnp.float32) * scale

    def gains(n):
        return 1.0 + nrm((n, D_MODEL), 0.02)

    x = nrm((BATCH, SEQ, D_MODEL), 1.0)
    mem = nrm((BATCH, MEM_LEN, D_MODEL), 1.0)
    positions = jnp.broadcast_to(jnp.arange(SEQ, dtype=jnp.int32)[None, :], (BATCH, SEQ))
    ev_w_in = nrm((N_EVEN, D_MODEL, IN_COLS), D_MODEL ** -0.5)
    ev_b_if = jnp.concatenate([nrm((N_EVEN, ML_HEADS), 0.1),
                               jnp.linspace(3.0, 6.0, ML_HEADS)[None, :] + nrm((N_EVEN, ML_HEADS), 0.01)], axis=-1)
    ev_conv_w = nrm((N_EVEN, RG_CONV, RG_WIDTH), RG_CONV ** -0.5)
    ev_conv_b = nrm((N_EVEN, RG_WIDTH), 0.01)
    ev_rg_wa = nrm((N_EVEN, RG_BLOCKS, RG_BW, RG_BW), RG_BW ** -0.5)
    ev_rg_ba = nrm((N_EVEN, RG_WIDTH), 0.01)
    ev_rg_wx = nrm((N_EVEN, RG_BLOCKS, RG_BW, RG_BW), RG_BW ** -0.5)
    ev_rg_bx = nrm((N_EVEN, RG_WIDTH), 0.01)
    a_c = jax.random.uniform(next(ks), (N_EVEN, RG_WIDTH), jnp.float32, 0.9, 0.999)
    a_base = a_c ** (1.0 / RG_C)
    ev_rg_lambda = jnp.log(a_base) - jnp.log1p(-a_base)
    ev_w_out = nrm((N_EVEN, MIX_WIDTH, D_MODEL), MIX_WIDTH ** -0.5)
    od_w_qkv = nrm((N_ODD, D_MODEL, QKV_COLS), D_MODEL ** -0.5)
    od_sinks = nrm((N_ODD, SW_HEADS), 1.0)
    od_w_o = nrm((N_ODD, SW_HEADS * SW_HD, D_MODEL), (SW_HEADS * SW_HD) ** -0.5)
    return {
        'x': x, 'mem': mem, 'positions': positions,
        'ev_w_in': ev_w_in, 'ev_b_if': ev_b_if, 'ev_conv_w': ev_conv_w, 'ev_conv_b': ev_conv_b,
        'ev_rg_wa': ev_rg_wa, 'ev_rg_ba': ev_rg_ba, 'ev_rg_wx': ev_rg_wx, 'ev_rg_bx': ev_rg_bx,
        'ev_rg_lambda': ev_rg_lambda, 'ev_w_out': ev_w_out,
        'od_w_qkv': od_w_qkv, 'od_sinks': od_sinks, 'od_w_o': od_w_o,
        'g_mix_pre': gains(DEPTH), 'g_mix_post': gains(DEPTH),
        'g_x_pre': gains(DEPTH), 'g_x_post': gains(DEPTH), 'g_mem': gains(DEPTH),
        'w_xq': nrm((DEPTH, D_MODEL, D_MODEL), D_MODEL ** -0.5),
        'w_xkv': nrm((DEPTH, D_MODEL, 2 * D_MODEL), D_MODEL ** -0.5),
        'w_xo': nrm((DEPTH, D_MODEL, D_MODEL), D_MODEL ** -0.5),
        'g_ff_pre': gains(DEPTH), 'g_ff_post': gains(DEPTH),
        'w_ff1': nrm((DEPTH, D_MODEL, D_FF), D_MODEL ** -0.5),
        'w_ff2': nrm((DEPTH, D_FF, D_MODEL), D_FF ** -0.5),
    }


def reference(x, mem, positions, ev_w_in, ev_b_if, ev_conv_w, ev_conv_b, ev_rg_wa, ev_rg_ba,
              ev_rg_wx, ev_rg_bx, ev_rg_lambda, ev_w_out, od_w_qkv, od_sinks, od_w_o,
              g_mix_pre, g_mix_post, g_x_pre, g_x_post, g_mem, w_xq, w_xkv, w_xo,
              g_ff_pre, g_ff_post, w_ff1, w_ff2):
    h = x
    for l in range(DEPTH):
        u = rms_norm(h, g_mix_pre[l])
        if l % 2 == 0:
            e = l // 2
            u = mlstm_rglru_mixer(u, ev_w_in[e], ev_b_if[e], ev_conv_w[e], ev_conv_b[e],
                                  ev_rg_wa[e], ev_rg_ba[e], ev_rg_wx[e], ev_rg_bx[e],
                                  ev_rg_lambda[e], ev_w_out[e])
        else:
            o = l // 2
            u = sliding_window_sink_attention(u, positions, od_w_qkv[o], od_sinks[o], od_w_o[o])
        h = h + rms_norm(u, g_mix_post[l])
        u = memory_cross_attention(rms_norm(h, g_x_pre[l]), mem, g_mem[l], w_xq[l], w_xkv[l], w_xo[l])
        h = h + rms_norm(u, g_x_post[l])
        u = squared_relu_mlp(rms_norm(h, g_ff_pre[l]), w_ff1[l], w_ff2[l])
        h = h + rms_norm(u, g_ff_post[l])
    return h
```

```python
import math
import os
from contextlib import ExitStack

import numpy as np
import concourse.bass as bass
import concourse.mybir as mybir
from concourse.bass_utils import run_bass_kernel_spmd

F32 = mybir.dt.float32
BF16 = mybir.dt.bfloat16
I32 = mybir.dt.int32
AF = mybir.ActivationFunctionType
ALU = mybir.AluOpType
AX = mybir.AxisListType


class Buf:
    __slots__ = ("name", "w", "r")

    def __init__(self, name):
        self.name = name
        self.w = None
        self.r = []


class Op:
    __slots__ = ("eng", "fn", "deps", "is_dma", "sem", "cnt", "has_dep", "idx", "pre")

    def __init__(self, eng, fn, is_dma):
        self.eng = eng
        self.fn = fn
        self.deps = []
        self.is_dma = is_dma
        self.sem = None
        self.cnt = None
        self.has_dep = False
        self.pre = None


class Prog:
    ENGS = ("pe", "act", "dve", "pool", "sp")
    NDMA = {"sp": 12, "pool": 12, "act": 4}

    def __init__(self, nc):
        self.nc = nc
        self.streams = {e: [] for e in self.ENGS}
        self.nbuf = 0

    def buf(self, name=None):
        self.nbuf += 1
        return Buf(name or f"b{self.nbuf}")

    def bufs(self, n, name="b"):
        return [self.buf(f"{name}{i}") for i in range(n)]

    def _add(self, o, reads, writes):
        deps = []
        for b in reads:
            if b.w is not None:
                deps.append(("raw", b.w))
            b.r.append(o)
        for b in writes:
            if b.w is not None:
                deps.append(("waw", b.w))
            for r in b.r:
                if r is not o:
                    deps.append(("war", r))
            b.r = []
            b.w = o
        seen = set()
        for kind, d in deps:
            if d is o or id(d) in seen:
                continue
            if (not d.is_dma) and d.eng == o.eng and not o.is_dma:
                if kind != "raw" or o.eng == "pe":
                    continue
            seen.add(id(d))
            o.deps.append(d)
            d.has_dep = True
        self.streams[o.eng].append(o)
        return o

    def op(self, eng, fn, reads=(), writes=()):
        return self._add(Op(eng, fn, False), list(reads), list(writes))

    def dma(self, eng, fn, reads=(), writes=()):
        return self._add(Op(eng, fn, True), list(reads), list(writes))

    def pe(self, fn, reads=(), writes=()):
        return self.op("pe", fn, reads, writes)

    def act(self, fn, reads=(), writes=()):
        return self.op("act", fn, reads, writes)

    def dve(self, fn, reads=(), writes=()):
        return self.op("dve", fn, reads, writes)

    def pool(self, fn, reads=(), writes=()):
        return self.op("pool", fn, reads, writes)

    def barrier(self):
        lasts = []
        for e in ("pe", "act", "dve", "pool"):
            for o in reversed(self.streams[e]):
                if not o.is_dma and o.fn is not None:
                    lasts.append(o)
                    break
        for e in ("pe", "act", "dve", "pool"):
            w = Op(e, None, False)
            for d in lasts:
                w.deps.append(d)
                d.has_dep = True
            self.streams[e].append(w)

    def emit(self, final_deps=()):
        nc = self.nc
        es = ExitStack()
        with es:
            csem = {e: es.enter_context(nc.semaphore(f"c_{e}")) for e in ("pe", "act", "dve", "pool")}
            dsem = {q: [es.enter_context(nc.semaphore(f"d_{q}{i}")) for i in range(n)]
                    for q, n in self.NDMA.items()}
            fin = Op("pool", None, False)
            for d in final_deps:
                fin.deps.append(d)
                d.has_dep = True
            for e in self.ENGS:
                if e != "pool":
                    for last in reversed(self.streams[e]):
                        if last.fn is not None:
                            fin.deps.append(last)
                            last.has_dep = True
                            break
            self.streams["pool"].append(fin)
            ccount = {e: 0 for e in csem}
            dcount = {q: 0 for q in dsem}
            for e in self.ENGS:
                for o in self.streams[e]:
                    if o.is_dma:
                        j = dcount[e]
                        dcount[e] += 1
                        k = len(dsem[e])
                        o.sem = dsem[e][j % k]
                        o.cnt = 16 * (j // k + 1)
                        o.pre = (o.sem, 16 * (j // k)) if j >= k else None
                    elif o.has_dep:
                        ccount[e] += 1
                        o.sem = csem[e]
                        o.cnt = ccount[e]
            self.stats = {"ccount": ccount, "dcount": dcount,
                          "n": {e: len(self.streams[e]) for e in self.ENGS}}
            engobj = {"pe": "tensor", "act": "scalar", "dve": "vector", "pool": "gpsimd", "sp": "sync"}

            def run_stream(e, eng):
                waited = {}
                nwait = 0
                for o in self.streams[e]:
                    need = {}
                    if o.pre is not None:
                        need[o.pre[0]] = o.pre[1]
                    for d in o.deps:
                        assert d.sem is not None, "dep without semaphore"
                        if need.get(d.sem, 0) < d.cnt:
                            need[d.sem] = d.cnt
                    for s, c in need.items():
                        if waited.get(s, 0) < c:
                            eng.wait_ge(s, c)
                            waited[s] = c
                            nwait += 1
                    if o.fn is None:
                        continue
                    ins = o.fn(eng)
                    if o.sem is not None:
                        ins.then_inc(o.sem, 16 if o.is_dma else 1)
                self.stats.setdefault("nwait", {})[e] = nwait

            with nc.Block() as block:
                for e in self.ENGS:
                    if not self.streams[e]:
                        continue

                    def f(eng, e=e):
                        run_stream(e, eng)

                    getattr(block, engobj[e])(f)


D = 1024
KC = 8
EPS = 1e-6
NEG = -1.0e9
PI = math.pi

W_NAMES = ["ev_w_in", "ev_b_if", "ev_conv_w", "ev_conv_b", "ev_rg_wa", "ev_rg_ba", "ev_rg_wx", "ev_rg_bx",
           "ev_rg_lambda", "ev_w_out", "od_w_qkv", "od_sinks", "od_w_o", "g_mix_pre", "g_mix_post",
           "g_x_pre", "g_x_post", "g_mem", "w_xq", "w_xkv", "w_xo", "g_ff_pre", "g_ff_post", "w_ff1", "w_ff2"]
W_SHAPES = {"ev_w_in": [1, 1024, 3080], "ev_b_if": [1, 8], "ev_conv_w": [1, 4, 512], "ev_conv_b": [1, 512],
            "ev_rg_wa": [1, 4, 128, 128], "ev_rg_ba": [1, 512], "ev_rg_wx": [1, 4, 128, 128], "ev_rg_bx": [1, 512],
            "ev_rg_lambda": [1, 512], "ev_w_out": [1, 1024, 1024], "od_w_qkv": [1, 1024, 1536],
            "od_sinks": [1, 16], "od_w_o": [1, 1024, 1024], "g_mix_pre": [2, 1024], "g_mix_post": [2, 1024],
            "g_x_pre": [2, 1024], "g_x_post": [2, 1024], "g_mem": [2, 1024], "w_xq": [2, 1024, 1024],
            "w_xkv": [2, 1024, 2048], "w_xo": [2, 1024, 1024], "g_ff_pre": [2, 1024], "g_ff_post": [2, 1024],
            "w_ff1": [2, 1024, 4096], "w_ff2": [2, 4096, 1024]}


def build_nc(NPRE, BLOCKS, dbg=False):
    nc = bass.Bass("TRN2", target_bir_lowering=False)
    NMAIN = sum(BLOCKS)
    NOWN = NMAIN - 1
    NLOC = NPRE + NMAIN
    NTB = max(BLOCKS)
    TB = NTB * 128

    def din(name, shape, dt=F32):
        return nc.dram_tensor(name, list(shape), dt, kind="ExternalInput").ap()

    xloc = din("xloc", [NLOC * 128, D])
    mem = din("mem", [256, D])
    posd = din("pos", [NMAIN, 128], I32)
    flagd = din("flag", [128, 1])
    maskfd = din("mask_first", [128, 128])
    invfd = din("invf", [128, 32])
    Wd = {n: din(n, W_SHAPES[n]) for n in W_NAMES}
    outd = nc.dram_tensor("out", [NOWN * 128, D], F32, kind="ExternalOutput").ap()
    dbgd = nc.dram_tensor("dbg", [6, NMAIN * 128, D], F32, kind="ExternalOutput").ap() if dbg else None

    P = Prog(nc)
    es = ExitStack()
    with es:
        ncd = es.enter_context(nc.allow_non_contiguous_dma(reason="small parameter loads"))

        sbn = [0]

        def sb(name, shape, dt, st=es):
            sbn[0] += 1
            return st.enter_context(nc.sbuf_tensor(f"{name}_{sbn[0]}", list(shape), dt))

        def MM(out, lhsT, rhs, start, stop, r, w):
            return P.pe(lambda e: e.matmul(out, lhsT=lhsT, rhs=rhs, start=start, stop=stop), r, w)

        def ACTF(out, in_, func, r, w, **kw):
            return P.act(lambda e: e.activation(out=out, in_=in_, func=func, **kw), r, w)

        def TTo(eng, out, in0, in1, op, r, w):
            return P.op(eng, lambda e: e.tensor_tensor(out=out, in0=in0, in1=in1, op=op), r, w)

        def TSo(eng, out, in0, s1, s2, op0, op1, r, w):
            if s2 is None:
                return P.op(eng, lambda e: e.tensor_scalar(out=out, in0=in0, scalar1=s1, scalar2=None, op0=op0), r, w)
            return P.op(eng, lambda e: e.tensor_scalar(out=out, in0=in0, scalar1=s1, scalar2=s2, op0=op0, op1=op1), r, w)

        def STTo(out, in0, scalar, in1, op0, op1, r, w):
            return P.dve(lambda e: e.scalar_tensor_tensor(out=out, in0=in0, scalar=scalar, in1=in1, op0=op0, op1=op1), r, w)

        def CPo(eng, out, in_, r, w):
            if eng == "act":
                return P.act(lambda e: e.activation(out=out, in_=in_, func=AF.Copy), r, w)
            return P.op(eng, lambda e: e.tensor_copy(out=out, in_=in_), r, w)

        def MSET(eng, ap, val, w):
            return P.op(eng, lambda e: e.memset(ap, val), [], w)

        def DMA(q, out, in_, r, w):
            return P.dma(q, lambda e: e.dma_start(out=out, in_=in_), r, w)

        ps_t = [es.enter_context(nc.psum_tensor(f"ps{i}", [128, 512], F32)) for i in range(8)]
        ps_b = P.bufs(8, "ps")
        ps_i = [0]

        def PS():
            i = ps_i[0]
            ps_i[0] = (i + 1) % 6
            return ps_t[i], ps_b[i]

        b_c = P.buf("const")
        ident_f = sb("ident_f", [128, 128], F32)
        ident_bf = sb("ident_bf", [128, 128], BF16)
        ones4 = sb("ones4", [4, 128], F32)
        onesrow = sb("onesrow", [4, TB], F32)
        tri = sb("tri", [128, 128], F32)
        mask_pc = sb("mask_pc", [128, 256], F32)
        mask_fc = sb("mask_fc", [128, 256], F32)
        invf = sb("invf_sb", [128, 32], F32)
        flag = sb("flag_sb", [128, 1], F32)
        MSET("pool", ident_f[:], 0.0, [b_c])
        P.pool(lambda e: e.affine_select(out=ident_f[:], in_=ident_f[:], pattern=[[-1, 128]], compare_op=ALU.not_equal,
                                         fill=1.0, base=0, channel_multiplier=1), [b_c], [b_c])
        CPo("dve", ident_bf[:], ident_f[:], [b_c], [b_c])
        MSET("pool", ones4[:], 1.0, [b_c])
        MSET("pool", onesrow[:], 1.0, [b_c])
        MSET("pool", tri[:], 1.0, [b_c])
        P.pool(lambda e: e.affine_select(out=tri[:], in_=tri[:], pattern=[[1, 128]], compare_op=ALU.is_ge,
                                         fill=0.0, base=0, channel_multiplier=-1), [b_c], [b_c])
        MSET("pool", mask_pc[:], 0.0, [b_c])
        P.pool(lambda e: e.affine_select(out=mask_pc[:, 0:128], in_=mask_pc[:, 0:128], pattern=[[1, 128]],
                                         compare_op=ALU.is_gt, fill=NEG, base=0, channel_multiplier=-1), [b_c], [b_c])
        P.pool(lambda e: e.affine_select(out=mask_pc[:, 128:256], in_=mask_pc[:, 128:256], pattern=[[-1, 128]],
                                         compare_op=ALU.is_ge, fill=NEG, base=0, channel_multiplier=1), [b_c], [b_c])
        DMA("pool", mask_fc[:, 0:128], maskfd, [], [b_c])
        CPo("pool", mask_fc[:, 128:256], mask_pc[:, 128:256], [b_c], [b_c])
        DMA("pool", invf[:], invfd, [], [b_c])
        DMA("pool", flag[:], flagd, [], [b_c])

        bif = sb("bif", [4, 2], F32)
        DMA("pool", bif[:, 0:1], Wd["ev_b_if"][0, 0:4].rearrange("(p o) -> p o", o=1), [], [b_c])
        DMA("pool", bif[:, 1:2], Wd["ev_b_if"][0, 4:8].rearrange("(p o) -> p o", o=1), [], [b_c])
        TSo("dve", bif[:, 1:2], bif[:, 1:2], -1.0, None, ALU.mult, None, [b_c], [b_c])
        cw = sb("cw", [128, 4, 4], F32)
        for j in range(4):
            DMA("pool", cw[:, j, :], Wd["ev_conv_w"][0, j, :].rearrange("(n p) -> p n", p=128), [], [b_c])
        cb = sb("cb", [128, 4], F32)
        rba = sb("rba", [128, 4], F32)
        rbx = sb("rbx", [128, 4], F32)
        lam = sb("lam", [128, 4], F32)
        cl = sb("cl", [128, 8], F32)
        for t_, n_ in ((cb, "ev_conv_b"), (rba, "ev_rg_ba"), (rbx, "ev_rg_bx"), (lam, "ev_rg_lambda")):
            DMA("pool", t_[:], Wd[n_][0, :].rearrange("(n p) -> p n", p=128), [], [b_c])
        ACTF(lam[:], lam[:], AF.Exp, [b_c], [b_c], scale=-1.0)
        ACTF(lam[:], lam[:], AF.Ln, [b_c], [b_c], bias=1.0)
        TSo("dve", cl[:, 0:4], lam[:], -8.0, None, ALU.mult, None, [b_c], [b_c])
        TSo("dve", cl[:, 4:8], lam[:], -16.0, None, ALU.mult, None, [b_c], [b_c])
        sinks = sb("sinks", [128, 16], F32)
        DMA("pool", sinks[:], Wd["od_sinks"][0:1, :].to_broadcast([128, 16]), [], [b_c])
        posi = sb("posi", [128, NMAIN], I32)
        posf = sb("posf", [128, NMAIN], F32)
        DMA("pool", posi[:], posd.rearrange("m p -> p m"), [], [b_c])
        CPo("dve", posf[:], posi[:], [b_c], [b_c])
        wgif = sb("wgif", [128, 8, 8], BF16)
        DMA("pool", wgif[:], Wd["ev_w_in"][0, :, 2048:2056].rearrange("(k p) n -> p k n", p=128), [], [b_c])
        wrg = sb("wrg", [128, 2, 4, 128], BF16)
        DMA("pool", wrg[:, 0], Wd["ev_rg_wa"][0].rearrange("n c d -> c n d"), [], [b_c])
        DMA("pool", wrg[:, 1], Wd["ev_rg_wx"][0].rearrange("n c d -> c n d"), [], [b_c])

        pieces = {}
        cast_q = []
        cast_done = {}
        cast_pos = [0]

        def cast_some(n):
            while n > 0 and cast_pos[0] < len(cast_q):
                name, t, src3, b = cast_q[cast_pos[0]]
                cast_pos[0] += 1
                DMA("pool", t, src3, [], [b])
                cast_done[name] = True
                n -= 1

        def mk_piece(name, src3):
            kk = src3.shape[1]
            nn = src3.shape[2]
            t = nc.dram_tensor("wsc_" + name, [128, kk, nn], BF16, kind="Internal").ap()
            b = P.buf("wsc_" + name)
            cast_q.append((name, t, src3, b))
            pieces[name] = (t, b, kk, nn)

        def std_pieces(prefix, w2d, ncols):
            names = []
            for c in range(ncols // 512):
                nm = f"{prefix}_{c}"
                mk_piece(nm, w2d[:, c * 512:(c + 1) * 512].rearrange("(k p) n -> p k n", p=128))
                names.append(nm)
            return names

        def mlp_pieces(l):
            names = []
            for half in range(2):
                for c in range(4 * half, 4 * half + 4):
                    nm = f"ff1_{l}_{c}"
                    mk_piece(nm, Wd["w_ff1"][l][:, c * 512:(c + 1) * 512].rearrange("(k p) n -> p k n", p=128))
                    names.append(nm)
                for c in range(4 * half, 4 * half + 4):
                    nm = f"ff2_{l}_{c}"
                    mk_piece(nm, Wd["w_ff2"][l][c * 512:(c + 1) * 512, :].rearrange("(k p) n -> p k n", p=128))
                    names.append(nm)
            return names

        seq = []
        xkv_names = []
        for l in range(2):
            xkv_names.append(std_pieces(f"xkv{l}", Wd["w_xkv"][l], 2048))
        win = Wd["ev_w_in"][0]
        in_names = []
        for nm, c0 in (("in_q", 0), ("in_k", 512), ("in_v", 1024), ("in_o", 1536), ("in_rx", 2056), ("in_ry", 2568)):
            mk_piece(nm, win[:, c0:c0 + 512].rearrange("(k p) n -> p k n", p=128))
            in_names.append(nm)
        L0 = list(in_names)
        L0 += std_pieces("out", Wd["ev_w_out"][0], 1024)
        L0 += std_pieces("xq0", Wd["w_xq"][0], 1024)
        L0 += std_pieces("xo0", Wd["w_xo"][0], 1024)
        L0 += mlp_pieces(0)
        L1 = std_pieces("qkv", Wd["od_w_qkv"][0], 1536)
        L1 += std_pieces("o1", Wd["od_w_o"][0], 1024)
        L1 += std_pieces("xq1", Wd["w_xq"][1], 1024)
        L1 += std_pieces("xo1", Wd["w_xo"][1], 1024)
        L1 += mlp_pieces(1)
        seq = xkv_names[0] + xkv_names[1]
        for _ in BLOCKS:
            seq += L0 + L1

        NSLOT = 8
        slot_t = [sb(f"slot{i}", [128, 4096], BF16) for i in range(NSLOT)]
        slot_b = P.bufs(NSLOT, "slot")
        ring = {"use": 0, "done": 0, "loaded": 0}

        def ring_load():
            i = ring["loaded"]
            if i >= len(seq):
                return
            t, b, kk, nn = pieces[seq[i]]
            while seq[i] not in cast_done:
                cast_some(1)
            s = i % NSLOT
            DMA("sp", slot_t[s][:].rearrange("p (k n) -> p k n", k=kk), t, [b], [slot_b[s]])
            ring["loaded"] += 1

        def acquire(expect=None):
            i = ring["use"]
            ring["use"] += 1
            assert i < ring["loaded"], "ring underflow"
            if expect is not None and not os.environ.get("MK_STOP"):
                assert seq[i].startswith(expect), (seq[i], expect)
            t, b, kk, nn = pieces[seq[i]]
            s = i % NSLOT
            return slot_t[s][:].rearrange("p (k n) -> p k n", k=kk), slot_b[s]

        def release(n=1):
            for _ in range(n):
                ring["done"] += 1
                ring_load()

        for _ in range(NSLOT):
            ring_load()

        h = sb("h", [128, NTB, D], F32)
        h_b = P.bufs(NTB, "h")
        grep = [sb(f"grep{i}", [128, D], F32) for i in range(2)]
        grep_b = P.bufs(2, "grep")
        gcnt = [0]

        def gload(name, l):
            i = gcnt[0] % 2
            gcnt[0] += 1
            DMA("pool", grep[i][:], Wd[name][l:l + 1, :].to_broadcast([128, D]), [], [grep_b[i]])
            return grep[i], grep_b[i]

        KT = sb("KT", [128, 2, 8, 256], BF16)
        VM = sb("VM", [128, 2, 2, D], BF16)
        b_kv = P.buf("kvmem")
        Cst = sb("Cst", [128, 4, 129], F32)
        b_C = P.buf("C")
        gcar = sb("gcar", [4, 2], F32)
        b_gc = P.buf("gcar")
        hst = sb("hst", [128, 4], F32)
        rxhist = sb("rxhist", [128, 4, 3], F32)
        b_rg = P.buf("rgst")
        MSET("dve", Cst[:], 0.0, [b_C])
        MSET("dve", gcar[:], 0.0, [b_gc])
        MSET("dve", hst[:], 0.0, [b_rg])
        MSET("dve", rxhist[:], 0.0, [b_rg])
        kTs = sb("kTs", [128, 2, 8, 128], BF16)
        vs = sb("vs", [128, 2, 256], BF16)
        kv_b = P.bufs(2, "swakv")
        MSET("dve", kTs[:], 0.0, [kv_b[0], kv_b[1]])
        MSET("dve", vs[:], 0.0, [kv_b[0], kv_b[1]])

        def prenorm(src_fn, src_bufs, nt, g, g_b, uT, uT_b, ws, tag):
            ssq = sb(f"pn_ssq{tag}", [128, NTB], F32, ws)
            junk = sb(f"pn_junk{tag}", [128, D], BF16, ws)
            ubf = [sb(f"pn_u{tag}{i}", [128, D], BF16, ws) for i in range(2)]
            b_ssq = P.bufs(nt, "ssq")
            b_junk = P.buf()
            b_u = P.bufs(2, "u")
            for j in range(nt):
                src = src_fn(j)
                ACTF(junk[:], src, AF.Square, [src_bufs[j]], [b_junk, b_ssq[j]], accum_out=ssq[:, j:j + 1])
                ACTF(ssq[:, j:j + 1], ssq[:, j:j + 1], AF.Sqrt, [b_ssq[j]], [b_ssq[j]], scale=1.0 / D, bias=EPS)
                P.dve(lambda e, j=j: e.reciprocal(out=ssq[:, j:j + 1], in_=ssq[:, j:j + 1]), [b_ssq[j]], [b_ssq[j]])
                u = ubf[j % 2]
                STTo(u[:], src, ssq[:, j:j + 1], g[:], ALU.mult, ALU.mult, [src_bufs[j], b_ssq[j], g_b], [b_u[j % 2]])
                pt, pb = PS()
                ptb = pt[:].bitcast(BF16)
                for kc in range(KC):
                    P.pe(lambda e, kc=kc, u=u, ptb=ptb: e.transpose(out=ptb[:, kc * 128:(kc + 1) * 128],
                                                                   in_=u[:, kc * 128:(kc + 1) * 128],
                                                                   identity=ident_bf[:]),
                         [b_u[j % 2], b_c], [pb])
                CPo("act", uT[:, :, j * 128:(j + 1) * 128], ptb.rearrange("p (k t) -> p k t", k=KC), [pb], [uT_b[j]])

        def postnorm(psA, pbA, psB, pbB, g, g_b, j, ws_t):
            ss2, tmp, junk = ws_t["ss2"], ws_t["tmp"], ws_t["junk"]
            k = ws_t["cnt"] % 2
            ws_t["cnt"] += 1
            b_ss, b_tmp, b_jk = ws_t["b_ss"][k], ws_t["b_tmp"][k], ws_t["b_jk"]
            s2 = ss2[:, 4 * k:4 * k + 4]
            ACTF(junk[:, 0:512], psA[:], AF.Square, [pbA], [b_jk, b_ss], accum_out=s2[:, 0:1])
            ACTF(junk[:, 512:1024], psB[:], AF.Square, [pbB], [b_jk, b_ss], accum_out=s2[:, 1:2])
            TTo("dve", s2[:, 2:3], s2[:, 0:1], s2[:, 1:2], ALU.add, [b_ss], [b_ss])
            ACTF(s2[:, 2:3], s2[:, 2:3], AF.Sqrt, [b_ss], [b_ss], scale=1.0 / D, bias=EPS)
            P.dve(lambda e: e.reciprocal(out=s2[:, 3:4], in_=s2[:, 2:3]), [b_ss], [b_ss])
            tm = tmp[k]
            STTo(tm[:, 0:512], psA[:], s2[:, 3:4], g[:, 0:512], ALU.mult, ALU.mult, [pbA, b_ss, g_b], [b_tmp])
            STTo(tm[:, 512:1024], psB[:], s2[:, 3:4], g[:, 512:1024], ALU.mult, ALU.mult, [pbB, b_ss, g_b], [b_tmp])
            TTo("pool", h[:, j, :], h[:, j, :], tm[:], ALU.add, [h_b[j], b_tmp], [h_b[j]])

        def mk_post_ws(ws, tag):
            return {"ss2": sb(f"po_ss{tag}", [128, 8], F32, ws), "junk": sb(f"po_junk{tag}", [128, D], BF16, ws),
                    "tmp": [sb(f"po_tmp{tag}0", [128, D], F32, ws)] * 2,
                    "cnt": 0, "b_ss": P.bufs(2, "poss"), "b_tmp": [P.buf("potmp")] * 2, "b_jk": P.buf()}

        def proj_tm(uT, uT_b, j, wv, wb, c0, ncol):
            pt, pb = PS()
            for kc in range(KC):
                MM(pt[:, 0:ncol], uT[:, kc, j * 128:(j + 1) * 128], wv[:, kc, c0:c0 + ncol], kc == 0, kc == KC - 1,
                   [uT_b[j], wb], [pb])
            return pt, pb

        def tok_groups(t0, nt):
            out = []
            j = t0
            while j < t0 + nt:
                n = min(4, t0 + nt - j)
                out.append((j, n))
                j += n
            return out

        def proj_fm(uT, uT_b, t0, nt, wv, wb, c0, evac):
            for (j0, n) in tok_groups(t0, nt):
                pt, pb = PS()
                for kc in range(KC):
                    MM(pt[:, 0:n * 128], wv[:, kc, c0:c0 + 128], uT[:, kc, j0 * 128:(j0 + n) * 128], kc == 0, kc == KC - 1,
                       [uT_b[j] for j in range(j0, j0 + n)] + [wb], [pb])
                evac(pt, pb, j0, n)

        def dbg_dump(k, mbase, nt):
            if dbgd is None:
                return
            for j in range(nt):
                m = mbase + j
                DMA("pool", dbgd[k, m * 128:(m + 1) * 128, :], h[:, j, :], [h_b[j]], [])

        with ExitStack() as ws:
            memsb = sb("memsb", [128, 2, D], F32, ws)
            b_mem = P.bufs(2, "mem")
            for j in range(2):
                DMA("pool", memsb[:, j, :], mem[j * 128:(j + 1) * 128, :], [], [b_mem[j]])
            memT = sb("memT", [128, KC, 256], BF16, ws)
            b_memT = P.bufs(2, "memT")
            for l in range(2):
                g, g_b = gload("g_mem", l)
                prenorm(lambda j: memsb[:, j, :], b_mem, 2, g, g_b, memT, b_memT, ws, f"m{l}")
                for pc in range(2):
                    wv, wb = acquire(f"xkv{l}")
                    for c in range(4):
                        pt, pb = PS()
                        for kc in range(KC):
                            MM(pt[:, 0:256], wv[:, kc, c * 128:(c + 1) * 128], memT[:, kc, :], kc == 0, kc == KC - 1,
                               b_memT + [wb], [pb])
                        CPo("act", KT[:, l, pc * 4 + c, :], pt[:, 0:256], [pb], [b_kv])
                    release()
                for pc in range(2):
                    wv, wb = acquire(f"xkv{l}")
                    for j in range(2):
                        pt, pb = proj_tm(memT, b_memT, j, wv, wb, 0, 512)
                        CPo("dve", VM[:, l, j, pc * 512:(pc + 1) * 512], pt[:], [pb], [b_kv])
                    release()
            P.barrier()

        def layer0_mix(nt, ranges, state_only, pcs, reset_before=None):
            with ExitStack() as ws:
                g, g_b = gload("g_mix_pre", 0)
                if not state_only:
                    gp, gp_b = gload("g_mix_post", 0)
                uT = sb("uT", [128, KC, TB], BF16, ws)
                uT_b = P.bufs(nt, "uT")
                prenorm(lambda j: h[:, j, :], h_b, nt, g, g_b, uT, uT_b, ws, "a")
                g1 = sb("g1", [4, TB], F32, ws)
                g2 = sb("g2", [4, TB], F32, ws)
                g3 = sb("g3", [4, TB], F32, ws)
                gsm = sb("gsm", [4, 4 * NTB + 8 + 4 * NTB], F32, ws)
                tmx = gsm[:, 0:NTB]
                MgE = gsm[:, NTB:2 * NTB + 1]
                wpv = gsm[:, 2 * NTB + 4:3 * NTB + 4]
                wpd = gsm[:, 3 * NTB + 8:3 * NTB + 8 + 4 * NTB]
                gtm = sb("gtm", [128, NTB, 8], F32, ws)
                wpr = sb("wpr", [128, NTB, 4], F32, ws)
                b_g = P.buf("gates")
                k_tm = sb("k_tm", [128, NTB, 512], BF16, ws)
                v_aug = sb("v_aug", [128, NTB, 4, 129], BF16, ws)
                b_kt = P.bufs(nt, "ktm")
                b_va = P.bufs(nt, "vaug")
                MSET("pool", v_aug[:, :, :, 128:129], 1.0, b_va)
                ktl = [sb(f"ktl{i}", [128, 4, 128], BF16, ws) for i in range(2)]
                b_ktl = P.bufs(2, "ktl")
                if not state_only:
                    qT = sb("qT", [128, 4, TB], BF16, ws)
                    kT = sb("kT", [128, 4, TB], BF16, ws)
                    b_qT = P.bufs(nt, "qT")
                    b_kT = P.bufs(nt, "kT")
                    sig_o = sb("sig_o", [128, NTB, 512], BF16, ws)
                    b_so = P.bufs(nt, "sigo")
                    yT = sb("yT", [128, KC, TB], BF16, ws)
                    b_yTa = P.bufs(nt, "yTa")
                    b_yTb = P.bufs(nt, "yTb")
                    Cbf = sb("Cbf", [128, 4, 129], BF16, ws)
                    b_Cbf = P.buf("Cbf")
                    ptl = [sb(f"ptl{i}", [128, 4, 128], BF16, ws) for i in range(2)]
                    b_ptl = P.bufs(2, "ptl")
                    yal = [sb(f"yal{i}", [128, 512], BF16, ws) for i in range(2)]
                    b_yal = P.bufs(2, "yal")
                    dn = sb("dn", [128, 8], F32, ws)
                    b_dn = P.bufs(2, "dn")
                rgs = []
                for i_ in range(2):
                    rgs.append(dict(rxn=sb("rxn", [128, TB + 3], F32, ws), xc=sb("xc", [128, TB], F32, ws),
                                    xcb=sb("xcb", [128, TB], BF16, ws), rr=sb("rr", [128, TB], F32, ws),
                                    ii=sb("ii", [128, TB], F32, ws), a2=sb("a2", [128, TB], F32, ws),
                                    hs=sb("hs", [128, TB], F32, ws), b=P.bufs(7, "rgw")))

                for ri, (t0, n) in enumerate(ranges):
                    if reset_before is not None and ri == reset_before:
                        TSo("dve", Cst[:], Cst[:], flag[:, 0:1], None, ALU.mult, None, [b_C, b_c], [b_C])
                        TSo("dve", gcar[:], gcar[:], flag[0:4, 0:1], None, ALU.mult, None, [b_gc, b_c], [b_gc])
                        TSo("dve", hst[:], hst[:], flag[:, 0:1], None, ALU.mult, None, [b_rg, b_c], [b_rg])
                        TSo("dve", rxhist[:].rearrange("p a b -> p (a b)"), rxhist[:].rearrange("p a b -> p (a b)"),
                            flag[:, 0:1], None, ALU.mult, None, [b_rg, b_c], [b_rg])
                    T_ = n * 128
                    c0 = t0 * 128
                    R = slice(c0, c0 + T_)
                    ub = [uT_b[j] for j in range(t0, t0 + n)]
                    wq, wqb = pcs["in_q"]
                    wk_, wkb = pcs["in_k"]
                    wv_, wvb = pcs["in_v"]
                    wo_, wob = pcs["in_o"]
                    wrx, wrxb = pcs["in_rx"]
                    wry, wryb = pcs["in_ry"]
                    for (j0, gn) in tok_groups(t0, n):
                        cs = slice(j0 * 128, (j0 + gn) * 128)
                        gub = [uT_b[j] for j in range(j0, j0 + gn)]
                        pti, pbi = PS()
                        for kc in range(KC):
                            MM(pti[0:4, 0:gn * 128], wgif[:, kc, 0:4], uT[:, kc, cs], kc == 0, kc == KC - 1, gub + [b_c], [pbi])
                        ACTF(g1[:, cs], pti[0:4, 0:gn * 128], AF.Identity, [pbi, b_c], [b_g], bias=bif[:, 0:1])
                        ptf, pbf = PS()
                        for kc in range(KC):
                            MM(ptf[0:4, 0:gn * 128], wgif[:, kc, 4:8], uT[:, kc, cs], kc == 0, kc == KC - 1, gub + [b_c], [pbf])
                        ACTF(g2[:, cs], ptf[0:4, 0:gn * 128], AF.Exp, [pbf, b_c], [b_g], scale=-1.0, bias=bif[:, 1:2])
                    ACTF(g2[:, R], g2[:, R], AF.Ln, [b_g], [b_g], bias=1.0)
                    P.dve(lambda e, R=R, T_=T_: e.tensor_tensor_scan(out=g3[:, R], data0=onesrow[:, 0:T_], data1=g2[:, R],
                                                                     initial=gcar[:, 0:1], op0=ALU.mult, op1=ALU.subtract),
                          [b_g, b_gc, b_c], [b_g])
                    CPo("dve", gcar[:, 0:1], g3[:, c0 + T_ - 1:c0 + T_], [b_g], [b_gc])
                    TTo("dve", g1[:, R], g1[:, R], g3[:, R], ALU.subtract, [b_g], [b_g])
                    P.dve(lambda e, R=R, n=n: e.tensor_reduce(out=tmx[:, 0:n], in_=g1[:, R].rearrange("p (j t) -> p j t", t=128),
                                                              axis=AX.X, op=ALU.max), [b_g], [b_g])
                    CPo("dve", MgE[:, 0:1], gcar[:, 1:2], [b_gc], [b_g])
                    P.dve(lambda e, n=n: e.tensor_tensor_scan(out=MgE[:, 1:n + 1], data0=onesrow[:, 0:n], data1=tmx[:, 0:n],
                                                              initial=MgE[:, 0:1], op0=ALU.mult, op1=ALU.max),
                          [b_g, b_c], [b_g])
                    CPo("dve", gcar[:, 1:2], MgE[:, n:n + 1], [b_g], [b_gc])
                    TTo("dve", wpv[:, 0:n], MgE[:, 0:n], MgE[:, 1:n + 1], ALU.subtract, [b_g], [b_g])
                    ACTF(wpv[:, 0:n], wpv[:, 0:n], AF.Exp, [b_g], [b_g])
                    Mb = MgE[:, 1:n + 1].unsqueeze(2).to_broadcast([4, n, 128])
                    g1v = g1[:, R].rearrange("p (j t) -> p j t", t=128)
                    g2v = g2[:, R].rearrange("p (j t) -> p j t", t=128)
                    g3v = g3[:, R].rearrange("p (j t) -> p j t", t=128)
                    TTo("dve", g2v, g1v, Mb, ALU.subtract, [b_g], [b_g])
                    ACTF(g2[:, R], g2[:, R], AF.Exp, [b_g], [b_g], bias=math.log(128 ** -0.5))
                    TTo("dve", g3v, g3v, Mb, ALU.add, [b_g], [b_g])
                    ACTF(g3[:, R], g3[:, R], AF.Exp, [b_g], [b_g], scale=-1.0)
                    if not state_only:
                        for c in range(4):
                            proj_fm(uT, uT_b, t0, n, wq, wqb, c * 128,
                                    lambda pt, pb, j0, gn, c=c: CPo("act", qT[:, c, j0 * 128:(j0 + gn) * 128], pt[:, 0:gn * 128],
                                                                    [pb], [b_qT[j] for j in range(j0, j0 + gn)]))
                            proj_fm(uT, uT_b, t0, n, wk_, wkb, c * 128,
                                    lambda pt, pb, j0, gn, c=c: CPo("dve", kT[:, c, j0 * 128:(j0 + gn) * 128], pt[:, 0:gn * 128],
                                                                    [pb], [b_kT[j] for j in range(j0, j0 + gn)]))
                    for j in range(t0, t0 + n):
                        pt, pb = proj_tm(uT, uT_b, j, wk_, wkb, 0, 512)
                        CPo("act", k_tm[:, j, :], pt[:], [pb], [b_kt[j]])
                        pt, pb = proj_tm(uT, uT_b, j, wv_, wvb, 0, 512)
                        CPo("dve", v_aug[:, j, :, 0:128], pt[:].rearrange("p (h d) -> p h d", h=4), [pb], [b_va[j]])
                        if not state_only:
                            pt, pb = proj_tm(uT, uT_b, j, wo_, wob, 0, 512)
                            ACTF(sig_o[:, j, :], pt[:], AF.Sigmoid, [pb], [b_so[j]])

                    wpdv = wpd[:, 0:4 * n].rearrange("p (j h) -> p j h", h=4)
                    TTo("dve", wpdv, wpv[:, 0:n].unsqueeze(2).to_broadcast([4, n, 4]),
                        ident_f[0:4, 0:4].unsqueeze(1).to_broadcast([4, n, 4]), ALU.mult, [b_g, b_c], [b_g])
                    pt, pb = PS()
                    MM(pt[:, 0:4 * n], ones4[:], wpd[:, 0:4 * n], True, True, [b_g, b_c], [pb])
                    CPo("dve", wpr[:, t0:t0 + n, :], pt[:, 0:4 * n].rearrange("p (j h) -> p j h", h=4), [pb], [b_g])
                    pt, pb = PS()
                    for jj in range(n):
                        cj = slice(c0 + jj * 128, c0 + (jj + 1) * 128)
                        MM(pt[:, jj * 8:jj * 8 + 4], g2[:, cj], ident_f[0:4, 0:4], True, True, [b_g, b_c], [pb])
                        MM(pt[:, jj * 8 + 4:jj * 8 + 8], g3[:, cj], ident_f[0:4, 0:4], True, True, [b_g, b_c], [pb])
                    CPo("dve", gtm[:, t0:t0 + n, :], pt[:, 0:8 * n].rearrange("p (j h) -> p j h", h=8), [pb], [b_g])

                    for j in range(t0, t0 + n):
                        cj = slice(j * 128, (j + 1) * 128)
                        wk4 = gtm[:, j, 0:4]
                        kt = ktl[j % 2]
                        TTo("pool", kt[:], k_tm[:, j, :].rearrange("p (h d) -> p h d", h=4),
                            wk4.unsqueeze(2).to_broadcast([128, 4, 128]), ALU.mult, [b_kt[j], b_g], [b_ktl[j % 2]])
                        TTo("dve", Cst[:], Cst[:], wpr[:, j, :].unsqueeze(2).to_broadcast([128, 4, 129]), ALU.mult,
                            [b_C, b_g], [b_C])
                        if not state_only:
                            CPo("act", Cbf[:], Cst[:], [b_C], [b_Cbf])
                            pss, pssb = PS()
                            for h_ in range(4):
                                MM(pss[:, h_ * 128:(h_ + 1) * 128], kT[:, h_, cj], qT[:, h_, cj], True, True,
                                   [b_kT[j], b_qT[j]], [pssb])
                            ptb_ = ptl[j % 2]
                            for h_ in range(4):
                                STTo(ptb_[:, h_, :], pss[:, h_ * 128:(h_ + 1) * 128], gtm[:, j, h_:h_ + 1], tri[:],
                                     ALU.mult, ALU.mult, [pssb, b_g, b_c], [b_ptl[j % 2]])
                            psn = [PS(), PS()]
                            for h_ in range(4):
                                pn_, pnb = psn[h_ // 2]
                                reg = pn_[:, (h_ % 2) * 129:(h_ % 2) * 129 + 129]
                                MM(reg, ptb_[:, h_, :], v_aug[:, j, h_, :], True, False, [b_ptl[j % 2], b_va[j]], [pnb])
                                MM(reg, qT[:, h_, cj], Cbf[:, h_, :], False, True, [b_qT[j], b_Cbf], [pnb])
                            d_ = dn[:, 4 * (j % 2):4 * (j % 2) + 4]
                            for bk in range(2):
                                pn_, pnb = psn[bk]
                                ACTF(d_[:, 2 * bk:2 * bk + 2], pn_[:, 0:258].rearrange("p (h e) -> p h e", e=129)[:, :, 128],
                                     AF.Abs, [pnb], [b_dn[j % 2]])
                            TTo("dve", d_, d_, gtm[:, j, 4:8], ALU.max, [b_dn[j % 2], b_g], [b_dn[j % 2]])
                            P.dve(lambda e, d_=d_: e.reciprocal(out=d_, in_=d_), [b_dn[j % 2]], [b_dn[j % 2]])
                            ya = yal[j % 2]
                            for h_ in range(4):
                                pn_, pnb = psn[h_ // 2]
                                STTo(ya[:, h_ * 128:(h_ + 1) * 128], pn_[:, (h_ % 2) * 129:(h_ % 2) * 129 + 128], d_[:, h_:h_ + 1],
                                     sig_o[:, j, h_ * 128:(h_ + 1) * 128], ALU.mult, ALU.mult,
                                     [pnb, b_dn[j % 2], b_so[j]], [b_yal[j % 2]])
                            pt, pb = PS()
                            ptb16 = pt[:].bitcast(BF16)
                            for h_ in range(4):
                                P.pe(lambda e, h_=h_, ya=ya, ptb16=ptb16: e.transpose(out=ptb16[:, h_ * 128:(h_ + 1) * 128],
                                                                                      in_=ya[:, h_ * 128:(h_ + 1) * 128],
                                                                                      identity=ident_bf[:]),
                                     [b_yal[j % 2], b_c], [pb])
                            CPo("act", yT[:, 0:4, cj], ptb16[:, 0:512].rearrange("p (h t) -> p h t", h=4), [pb], [b_yTa[j]])
                        psk = [PS(), PS()]
                        for h_ in range(4):
                            pk_, pkb = psk[h_ // 2]
                            MM(pk_[:, (h_ % 2) * 129:(h_ % 2) * 129 + 129], kt[:, h_, :], v_aug[:, j, h_, :], True, True,
                               [b_ktl[j % 2], b_va[j]], [pkb])
                        for bk in range(2):
                            pk_, pkb = psk[bk]
                            TTo("dve", Cst[:, 2 * bk:2 * bk + 2, :], Cst[:, 2 * bk:2 * bk + 2, :],
                                pk_[:, 0:258].rearrange("p (h e) -> p h e", e=129), ALU.add, [pkb, b_C], [b_C])

                    for n_ in range(4):
                        rg_ = rgs[n_ % 2]
                        rxn, xc, xcb, rr, ii, a2, hs = (rg_["rxn"], rg_["xc"], rg_["xcb"], rg_["rr"], rg_["ii"], rg_["a2"], rg_["hs"])
                        b_rxn, b_xc, b_xcb, b_rr, b_ii, b_a2, b_hs = rg_["b"]
                        proj_fm(uT, uT_b, t0, n, wrx, wrxb, n_ * 128,
                                lambda pt, pb, j0, gn: CPo("act", rxn[:, 3 + (j0 - t0) * 128:3 + (j0 - t0 + gn) * 128],
                                                           pt[:, 0:gn * 128], [pb], [b_rxn]))
                        CPo("pool", rxn[:, 0:3], rxhist[:, n_, :], [b_rg], [b_rxn])
                        TSo("dve", xc[:, 0:T_], rxn[:, 0:T_], cw[:, 0, n_:n_ + 1], cb[:, n_:n_ + 1], ALU.mult, ALU.add,
                            [b_rxn, b_c], [b_xc])
                        for jj in range(1, 4):
                            STTo(xc[:, 0:T_], rxn[:, jj:jj + T_], cw[:, jj, n_:n_ + 1], xc[:, 0:T_], ALU.mult, ALU.add,
                                 [b_rxn, b_c, b_xc], [b_xc])
                        CPo("pool", rxhist[:, n_, :], rxn[:, T_:T_ + 3], [b_rxn], [b_rg])
                        CPo("pool", xcb[:, 0:T_], xc[:, 0:T_], [b_xc], [b_xcb])
                        for (j0, gn) in tok_groups(0, n):
                            cs = slice(j0 * 128, (j0 + gn) * 128)
                            pr, prb = PS()
                            MM(pr[:, 0:gn * 128], wrg[:, 0, n_, :], xcb[:, cs], True, True, [b_xcb, b_c], [prb])
                            ACTF(rr[:, cs], pr[:, 0:gn * 128], AF.Sigmoid, [prb, b_c], [b_rr], bias=rba[:, n_:n_ + 1])
                            pi_, pib = PS()
                            MM(pi_[:, 0:gn * 128], wrg[:, 1, n_, :], xcb[:, cs], True, True, [b_xcb, b_c], [pib])
                            ACTF(ii[:, cs], pi_[:, 0:gn * 128], AF.Sigmoid, [pib, b_c], [b_ii], bias=rbx[:, n_:n_ + 1])
                        ACTF(a2[:, 0:T_], rr[:, 0:T_], AF.Exp, [b_rr, b_c], [b_a2], scale=cl[:, 4 + n_:5 + n_])
                        ACTF(rr[:, 0:T_], rr[:, 0:T_], AF.Exp, [b_rr, b_c], [b_rr], scale=cl[:, n_:n_ + 1])
                        TSo("dve", a2[:, 0:T_], a2[:, 0:T_], 1.0, None, ALU.min, None, [b_a2], [b_a2])
                        ACTF(a2[:, 0:T_], a2[:, 0:T_], AF.Sqrt, [b_a2], [b_a2], scale=-1.0, bias=1.0)
                        TTo("pool", ii[:, 0:T_], ii[:, 0:T_], xc[:, 0:T_], ALU.mult, [b_ii, b_xc], [b_ii])
                        TTo("pool", a2[:, 0:T_], a2[:, 0:T_], ii[:, 0:T_], ALU.mult, [b_a2, b_ii], [b_a2])
                        P.dve(lambda e, n_=n_, T_=T_, hs=hs, rr=rr, a2=a2: e.tensor_tensor_scan(out=hs[:, 0:T_], data0=rr[:, 0:T_], data1=a2[:, 0:T_],
                                                                           initial=hst[:, n_:n_ + 1], op0=ALU.mult, op1=ALU.add),
                              [b_rr, b_a2, b_rg], [b_hs])
                        CPo("pool", hst[:, n_:n_ + 1], hs[:, T_ - 1:T_], [b_hs], [b_rg])
                        if not state_only:
                            proj_fm(uT, uT_b, t0, n, wry, wryb, n_ * 128,
                                    lambda pt, pb, j0, gn: ACTF(yT[:, 4 + n_, j0 * 128:(j0 + gn) * 128], pt[:, 0:gn * 128],
                                                                AF.Gelu_apprx_tanh, [pb],
                                                                [b_yTb[j] for j in range(j0, j0 + gn)]))
                            ybs = [b_yTb[j] for j in range(t0, t0 + n)]
                            TTo("dve", yT[:, 4 + n_, R], yT[:, 4 + n_, R], hs[:, 0:T_], ALU.mult, ybs + [b_hs], ybs)
                return_release = 6
                if not state_only:
                    release(6)
                    wo0, wo0b = acquire("out")
                    wo1, wo1b = acquire("out")
                    pws = mk_post_ws(ws, "a")
                    for j in range(nt):
                        yb_ = [b_yTa[j], b_yTb[j]]
                        pA, pAb = PS()
                        pB, pBb = PS()
                        for kc in range(KC):
                            MM(pA[:], yT[:, kc, j * 128:(j + 1) * 128], wo0[:, kc, :], kc == 0, kc == KC - 1, yb_ + [wo0b], [pAb])
                        for kc in range(KC):
                            MM(pB[:], yT[:, kc, j * 128:(j + 1) * 128], wo1[:, kc, :], kc == 0, kc == KC - 1, yb_ + [wo1b], [pBb])
                        postnorm(pA, pAb, pB, pBb, gp, gp_b, j, pws)
                    release(2)
                P.barrier()

        def xattn(l, nt):
            SC = 256 ** -0.5
            with ExitStack() as ws:
                g, g_b = gload("g_x_pre", l)
                gp, gp_b = gload("g_x_post", l)
                uT = sb("uT", [128, KC, TB], BF16, ws)
                uT_b = P.bufs(nt, "uT")
                prenorm(lambda j: h[:, j, :], h_b, nt, g, g_b, uT, uT_b, ws, "x")
                xqT = sb("xqT", [128, 8, TB], BF16, ws)
                b_xq = P.bufs(nt, "xq")
                for pc in range(2):
                    wv, wb = acquire(f"xq{l}")
                    for c in range(4):
                        proj_fm(uT, uT_b, 0, nt, wv, wb, c * 128,
                                lambda pt, pb, j0, gn, cc=pc * 4 + c: CPo("act", xqT[:, cc, j0 * 128:(j0 + gn) * 128],
                                                                          pt[:, 0:gn * 128], [pb],
                                                                          [b_xq[j] for j in range(j0, j0 + gn)]))
                    release()
                wo0, wo0b = acquire(f"xo{l}")
                wo1, wo1b = acquire(f"xo{l}")
                pf = [sb(f"xpf{i}", [128, 4, 256], F32, ws) for i in range(2)]
                pn = [sb(f"xpn{i}", [128, 4, 256], BF16, ws) for i in range(2)]
                pT = [sb(f"xpT{i}", [128, 8, 128], BF16, ws) for i in range(2)]
                xoT = [sb(f"xoT{i}", [128, 8, 128], BF16, ws) for i in range(2)]
                sm = sb("xsm", [128, 2, 12], F32, ws)
                b_pf, b_pn, b_pT, b_xo, b_sm = (P.bufs(2, "xpf"), P.bufs(2, "xpn"), P.bufs(2, "xpT"),
                                                 P.bufs(2, "xoT"), P.bufs(2, "xsm"))
                pws = mk_post_ws(ws, "x")
                for j in range(nt):
                    q = j % 2
                    cj = slice(j * 128, (j + 1) * 128)
                    psc = [PS(), PS()]
                    for h_ in range(4):
                        pt, pb = psc[h_ // 2]
                        for dc in range(2):
                            MM(pt[:, (h_ % 2) * 256:(h_ % 2) * 256 + 256], xqT[:, 2 * h_ + dc, cj], KT[:, l, 2 * h_ + dc, :],
                               dc == 0, dc == 1, [b_xq[j], b_kv], [pb])
                    mx = sm[:, q, 0:4]
                    rs = sm[:, q, 4:8]
                    rrs = sm[:, q, 8:12]
                    for bk in range(2):
                        pt, pb = psc[bk]
                        P.dve(lambda e, pt=pt, mx=mx, bk=bk: e.tensor_reduce(out=mx[:, 2 * bk:2 * bk + 2],
                                                                             in_=pt[:].rearrange("p (h m) -> p h m", h=2),
                                                                             axis=AX.X, op=ALU.max), [pb], [b_sm[q]])
                    TSo("dve", mx, mx, -SC, None, ALU.mult, None, [b_sm[q]], [b_sm[q]])
                    for h_ in range(4):
                        pt, pb = psc[h_ // 2]
                        ACTF(pf[q][:, h_, :], pt[:, (h_ % 2) * 256:(h_ % 2) * 256 + 256], AF.Exp, [pb, b_sm[q]], [b_pf[q], b_sm[q]],
                             scale=SC, bias=mx[:, h_:h_ + 1], accum_out=rs[:, h_:h_ + 1])
                    P.dve(lambda e, rs=rs, rrs=rrs: e.reciprocal(out=rrs, in_=rs), [b_sm[q]], [b_sm[q]])
                    TTo("pool", pn[q][:], pf[q][:], rrs.unsqueeze(2).to_broadcast([128, 4, 256]), ALU.mult,
                        [b_pf[q], b_sm[q]], [b_pn[q]])
                    pt, pb = PS()
                    ptb16 = pt[:].bitcast(BF16)
                    for h_ in range(4):
                        for mc in range(2):
                            P.pe(lambda e, h_=h_, mc=mc, q=q, ptb16=ptb16: e.transpose(
                                out=ptb16[:, (h_ * 2 + mc) * 128:(h_ * 2 + mc + 1) * 128],
                                in_=pn[q][:, h_, mc * 128:(mc + 1) * 128], identity=ident_bf[:]), [b_pn[q], b_c], [pb])
                    CPo("act", pT[q][:], ptb16.rearrange("p (a t) -> p a t", a=8), [pb], [b_pT[q]])
                    pso = [PS(), PS()]
                    for h_ in range(4):
                        for dc in range(2):
                            idx = 2 * h_ + dc
                            pt, pb = pso[idx // 4]
                            for mc in range(2):
                                MM(pt[:, (idx % 4) * 128:(idx % 4 + 1) * 128],
                                   VM[:, l, mc, h_ * 256 + dc * 128:h_ * 256 + (dc + 1) * 128], pT[q][:, h_ * 2 + mc, :],
                                   mc == 0, mc == 1, [b_kv, b_pT[q]], [pb])
                    for bk in range(2):
                        pt, pb = pso[bk]
                        CPo("dve", xoT[q][:, 4 * bk:4 * bk + 4, :], pt[:].rearrange("p (a t) -> p a t", a=4), [pb], [b_xo[q]])
                    pA, pAb = PS()
                    pB, pBb = PS()
                    for kc in range(KC):
                        MM(pA[:], xoT[q][:, kc, :], wo0[:, kc, :], kc == 0, kc == KC - 1, [b_xo[q], wo0b], [pAb])
                    for kc in range(KC):
                        MM(pB[:], xoT[q][:, kc, :], wo1[:, kc, :], kc == 0, kc == KC - 1, [b_xo[q], wo1b], [pBb])
                    postnorm(pA, pAb, pB, pBb, gp, gp_b, j, pws)
                release(2)
                P.barrier()

        def mlp(l, nt):
            with ExitStack() as ws:
                g, g_b = gload("g_ff_pre", l)
                gp, gp_b = gload("g_ff_post", l)
                uT = sb("uT", [128, KC, TB], BF16, ws)
                uT_b = P.bufs(nt, "uT")
                prenorm(lambda j: h[:, j, :], h_b, nt, g, g_b, uT, uT_b, ws, "f")
                hid = sb("hid", [128, 16, TB], BF16, ws)
                acc = sb("acc", [128, NTB, D], F32, ws)
                b_acc = P.bufs(nt, "acc")
                rl = [sb(f"rl{i}", [128, 512], BF16, ws) for i in range(2)]
                b_rl = P.bufs(2, "rl")
                cnt = [0]
                pws = mk_post_ws(ws, "f")
                b_hid = P.bufs(nt, "hid")
                for half in range(2):
                    for pc in range(4):
                        wv, wb = acquire(f"ff1_{l}")
                        for c in range(4):
                            ch = pc * 4 + c

                            def ev(pt, pb, j0, gn, ch=ch, b_hid=b_hid):
                                k = cnt[0] % 2
                                cnt[0] += 1
                                ACTF(rl[k][:, 0:gn * 128], pt[:, 0:gn * 128], AF.Relu, [pb], [b_rl[k]])
                                TTo("pool", hid[:, ch, j0 * 128:(j0 + gn) * 128], rl[k][:, 0:gn * 128], rl[k][:, 0:gn * 128],
                                    ALU.mult, [b_rl[k]], [b_hid[j] for j in range(j0, j0 + gn)])
                            proj_fm(uT, uT_b, 0, nt, wv, wb, c * 128, ev)
                        release()
                    w2 = [acquire(f"ff2_{l}") for _ in range(4)]
                    for j in range(nt):
                        pA, pAb = PS()
                        pB, pBb = PS()
                        for (pp, ppb, c0) in ((pA, pAb, 0), (pB, pBb, 512)):
                            for k in range(16):
                                wv, wb = w2[k // 4]
                                MM(pp[:], hid[:, k, j * 128:(j + 1) * 128], wv[:, k % 4, c0:c0 + 512], k == 0, k == 15,
                                   [b_hid[j], wb], [ppb])
                        if half == 0:
                            CPo("act", acc[:, j, 0:512], pA[:], [pAb], [b_acc[j]])
                            CPo("dve", acc[:, j, 512:1024], pB[:], [pBb], [b_acc[j]])
                        else:
                            TTo("dve", acc[:, j, 0:512], acc[:, j, 0:512], pA[:], ALU.add, [pAb, b_acc[j]], [b_acc[j]])
                            TTo("dve", acc[:, j, 512:1024], acc[:, j, 512:1024], pB[:], ALU.add, [pBb, b_acc[j]], [b_acc[j]])
                            postnorm(acc[:, j, 0:512], b_acc[j], acc[:, j, 512:1024], b_acc[j], gp, gp_b, j, pws)
                    release(4)
                P.barrier()

        def swa(nt, mbase):
            SC = 64 ** -0.5
            PIB = 3.1415925
            with ExitStack() as ws:
                g, g_b = gload("g_mix_pre", 1)
                gp, gp_b = gload("g_mix_post", 1)
                uT = sb("uT", [128, KC, TB], BF16, ws)
                uT_b = P.bufs(nt, "uT")
                prenorm(lambda j: h[:, j, :], h_b, nt, g, g_b, uT, uT_b, ws, "s")
                wq0, wq0b = acquire("qkv")
                wq1, wq1b = acquire("qkv")
                wkv, wkvb = acquire("qkv")
                wo0, wo0b = acquire("o1")
                wo1, wo1b = acquire("o1")
                ang = sb("ang", [128, NTB, 32], F32, ws)
                tmd = sb("tmd", [128, NTB, 32], F32, ws)
                cosv = sb("cosv", [128, NTB, 32], F32, ws)
                sinv = sb("sinv", [128, NTB, 32], F32, ws)
                b_rp = P.buf("rope")
                A_ = ang[:, 0:nt, :]
                TTo("dve", A_, posf[:, mbase:mbase + nt].unsqueeze(2).to_broadcast([128, nt, 32]),
                    invf[:].unsqueeze(1).to_broadcast([128, nt, 32]), ALU.mult, [b_c], [b_rp])
                kf = sb("kf", [128, NTB, 32], F32, ws)

                def red_sin(dst, shift):
                    T_ = tmd[:, 0:nt, :]
                    K_ = kf[:, 0:nt, :]
                    TSo("dve", T_, A_, shift, None, ALU.add, None, [b_rp], [b_rp])
                    TSo("dve", K_, T_, 1.0 / (2 * PI), None, ALU.mult, None, [b_rp], [b_rp])
                    TSo("dve", K_, K_, 12582912.0, None, ALU.add, None, [b_rp], [b_rp])
                    TSo("dve", K_, K_, 12582912.0, None, ALU.subtract, None, [b_rp], [b_rp])
                    STTo(T_, K_, -2 * PI, T_, ALU.mult, ALU.add, [b_rp], [b_rp])
                    TSo("dve", K_, T_, PI, 2 * PI, ALU.is_gt, ALU.mult, [b_rp], [b_rp])
                    TTo("dve", T_, T_, K_, ALU.subtract, [b_rp], [b_rp])
                    TSo("dve", K_, T_, -PI, 2 * PI, ALU.is_lt, ALU.mult, [b_rp], [b_rp])
                    TTo("dve", T_, T_, K_, ALU.add, [b_rp], [b_rp])
                    TSo("dve", T_, T_, PIB, -PIB, ALU.min, ALU.max, [b_rp], [b_rp])
                    ACTF(dst, T_, AF.Sin, [b_rp], [b_rp])

                red_sin(sinv[:, 0:nt, :], 0.0)
                red_sin(cosv[:, 0:nt, :], PI / 2)
                qkf = [sb(f"qkf{i}", [128, 20, 64], F32, ws) for i in range(2)]
                rt = [sb(f"rt{i}", [128, 20, 32], F32, ws) for i in range(4)]
                qkr = [sb(f"qkr{i}", [128, 24, 64], BF16, ws) for i in range(2)]
                qTl = [sb(f"qTl{i}", [128, 8, 128], BF16, ws) for i in range(2)]
                sf = [sb(f"sf{i}", [128, 4, 256], F32, ws) for i in range(2)]
                pn = [sb(f"spn{i}", [128, 4, 256], BF16, ws) for i in range(2)]
                pT = [sb(f"spT{i}", [128, 8, 128], BF16, ws) for i in range(2)]
                obf = [sb(f"obf{i}", [128, D], BF16, ws) for i in range(2)]
                oT = [sb(f"oT{i}", [128, 8, 128], BF16, ws) for i in range(2)]
                sm = sb("ssm", [128, 2, 20], F32, ws)
                b_qkf, b_qkr, b_qTl, b_sf, b_pn, b_pT, b_obf, b_oT, b_sm = [P.bufs(2, "sw") for _ in range(9)]
                b_rt = P.bufs(4, "rt")
                pws = mk_post_ws(ws, "s")
                gi = [0]
                for j in range(nt):
                    m = mbase + j
                    q = j % 2
                    cur, prv = m % 2, (m - 1) % 2
                    pq0, pq0b = proj_tm(uT, uT_b, j, wq0, wq0b, 0, 512)
                    pq1, pq1b = proj_tm(uT, uT_b, j, wq1, wq1b, 0, 512)
                    pkv, pkvb = proj_tm(uT, uT_b, j, wkv, wkvb, 0, 512)
                    CPo("act", qkf[q][:, 0:8, :], pq0[:].rearrange("p (a d) -> p a d", d=64), [pq0b], [b_qkf[q]])
                    CPo("act", qkf[q][:, 8:16, :], pq1[:].rearrange("p (a d) -> p a d", d=64), [pq1b], [b_qkf[q]])
                    CPo("dve", qkf[q][:, 16:20, :], pkv[:, 0:256].rearrange("p (a d) -> p a d", d=64), [pkvb], [b_qkf[q]])
                    CPo("dve", vs[:, cur, :], pkv[:, 256:512], [pkvb], [kv_b[cur]])
                    t1 = qkf[q][:, :, 0:32]
                    t2 = qkf[q][:, :, 32:64]
                    cbb = cosv[:, j, :].unsqueeze(1).to_broadcast([128, 20, 32])
                    sbb = sinv[:, j, :].unsqueeze(1).to_broadcast([128, 20, 32])
                    TTo("dve", rt[0][:], t1, cbb, ALU.mult, [b_qkf[q], b_rp], [b_rt[0]])
                    TTo("dve", rt[1][:], t2, sbb, ALU.mult, [b_qkf[q], b_rp], [b_rt[1]])
                    TTo("pool", rt[2][:], t2, cbb, ALU.mult, [b_qkf[q], b_rp], [b_rt[2]])
                    TTo("pool", rt[3][:], t1, sbb, ALU.mult, [b_qkf[q], b_rp], [b_rt[3]])
                    TTo("dve", qkr[q][:, 0:20, 0:32], rt[0][:], rt[1][:], ALU.subtract, [b_rt[0], b_rt[1]], [b_qkr[q]])
                    TTo("pool", qkr[q][:, 0:20, 32:64], rt[2][:], rt[3][:], ALU.add, [b_rt[2], b_rt[3]], [b_qkr[q]])
                    for dst, src in ((20, 17), (21, 16), (22, 19), (23, 18)):
                        CPo("pool", qkr[q][:, dst, :], qkr[q][:, src, :], [b_qkr[q]], [b_qkr[q]])
                    pt, pb = PS()
                    ptb16 = pt[:].bitcast(BF16)
                    for p_ in range(8):
                        P.pe(lambda e, p_=p_, q=q, ptb16=ptb16: e.transpose(
                            out=ptb16[:, p_ * 128:(p_ + 1) * 128],
                            in_=qkr[q][:, 2 * p_:2 * p_ + 2, :].rearrange("p a d -> p (a d)"), identity=ident_bf[:]),
                            [b_qkr[q], b_c], [pb])
                    CPo("act", qTl[q][:], ptb16.rearrange("p (a t) -> p a t", a=8), [pb], [b_qTl[q]])
                    pt, pb = PS()
                    ptb16 = pt[:].bitcast(BF16)
                    for i_ in range(4):
                        P.pe(lambda e, i_=i_, q=q, ptb16=ptb16: e.transpose(
                            out=ptb16[:, i_ * 128:(i_ + 1) * 128],
                            in_=qkr[q][:, 16 + 2 * i_:18 + 2 * i_, :].rearrange("p a d -> p (a d)"), identity=ident_bf[:]),
                            [b_qkr[q], b_c], [pb])
                    for i_, (ga, gb) in enumerate(((0, 1), (2, 3), (1, 0), (3, 2))):
                        CPo("dve", kTs[0:64, cur, ga * 2, :], ptb16[0:64, i_ * 128:(i_ + 1) * 128], [pb], [kv_b[cur]])
                        CPo("act", kTs[64:128, cur, gb * 2 + 1, :], ptb16[64:128, i_ * 128:(i_ + 1) * 128], [pb], [kv_b[cur]])
                    mask = mask_fc if m == 1 else mask_pc
                    pso = [(ps_t[6], ps_b[6]), (ps_t[7], ps_b[7])]
                    SWL = int(os.environ.get("MK_SWA", "9"))
                    if SWL < 2:
                        continue
                    for gq in range(4):
                        w_ = gi[0] % 2
                        gi[0] += 1
                        psc = [PS(), PS()]
                        for hh in range(4):
                            h_ = 4 * gq + hh
                            p_ = h_ // 2
                            o_ = (h_ % 2) * 64
                            var = gq * 2 + (1 if o_ else 0)
                            pt, pb = psc[hh // 2]
                            base = (hh % 2) * 256
                            MM(pt[:, base:base + 128], qTl[q][:, p_, :], kTs[:, prv, var, :], True, True,
                               [b_qTl[q], kv_b[prv]], [pb])
                            MM(pt[:, base + 128:base + 256], qTl[q][:, p_, :], kTs[:, cur, var, :], True, True,
                               [b_qTl[q], kv_b[cur]], [pb])
                        for bk in range(2):
                            pt, pb = psc[bk]
                            TTo("dve", sf[w_][:, 2 * bk:2 * bk + 2, :], pt[:].rearrange("p (a c) -> p a c", a=2),
                                mask[:].unsqueeze(1).to_broadcast([128, 2, 256]), ALU.add, [pb, b_c], [b_sf[w_]])
                        if SWL < 3:
                            continue
                        mx = sm[:, w_, 0:4]
                        ngm = sm[:, w_, 4:8]
                        rs = sm[:, w_, 8:12]
                        t4 = sm[:, w_, 12:16]
                        rrs = sm[:, w_, 16:20]
                        sk = sinks[:, 4 * gq:4 * gq + 4]
                        P.dve(lambda e, w_=w_, mx=mx: e.tensor_reduce(out=mx, in_=sf[w_][:], axis=AX.X, op=ALU.max),
                              [b_sf[w_]], [b_sm[w_]])
                        STTo(mx, mx, SC, sk, ALU.mult, ALU.max, [b_sm[w_], b_c], [b_sm[w_]])
                        TSo("dve", ngm, mx, -1.0, None, ALU.mult, None, [b_sm[w_]], [b_sm[w_]])
                        for hh in range(4):
                            ACTF(sf[w_][:, hh, :], sf[w_][:, hh, :], AF.Exp, [b_sf[w_], b_sm[w_]], [b_sf[w_], b_sm[w_]],
                                 scale=SC, bias=ngm[:, hh:hh + 1], accum_out=rs[:, hh:hh + 1])
                        TTo("dve", t4, sk, ngm, ALU.add, [b_sm[w_], b_c], [b_sm[w_]])
                        ACTF(t4, t4, AF.Exp, [b_sm[w_]], [b_sm[w_]])
                        TTo("dve", rs, rs, t4, ALU.add, [b_sm[w_]], [b_sm[w_]])
                        P.dve(lambda e, rs=rs, rrs=rrs: e.reciprocal(out=rrs, in_=rs), [b_sm[w_]], [b_sm[w_]])
                        TTo("pool", pn[w_][:], sf[w_][:], rrs.unsqueeze(2).to_broadcast([128, 4, 256]), ALU.mult,
                            [b_sf[w_], b_sm[w_]], [b_pn[w_]])
                        pt, pb = PS()
                        ptb16 = pt[:].bitcast(BF16)
                        for hh in range(4):
                            for kc2 in range(2):
                                P.pe(lambda e, hh=hh, kc2=kc2, w_=w_, ptb16=ptb16: e.transpose(
                                    out=ptb16[:, (hh * 2 + kc2) * 128:(hh * 2 + kc2 + 1) * 128],
                                    in_=pn[w_][:, hh, kc2 * 128:(kc2 + 1) * 128], identity=ident_bf[:]), [b_pn[w_], b_c], [pb])
                        CPo("act", pT[w_][:], ptb16.rearrange("p (a t) -> p a t", a=8), [pb], [b_pT[w_]])
                        for hh in range(4):
                            h_ = 4 * gq + hh
                            po, pob = pso[h_ // 8]
                            col = (h_ % 8) * 64
                            MM(po[:, col:col + 64], pT[w_][:, hh * 2, :], vs[:, prv, gq * 64:(gq + 1) * 64], True, False,
                               [b_pT[w_], kv_b[prv]], [pob])
                            MM(po[:, col:col + 64], pT[w_][:, hh * 2 + 1, :], vs[:, cur, gq * 64:(gq + 1) * 64], False, True,
                               [b_pT[w_], kv_b[cur]], [pob])
                    if SWL < 4:
                        continue
                    CPo("act", obf[q][:, 0:512], pso[0][0][:], [pso[0][1]], [b_obf[q]])
                    CPo("dve", obf[q][:, 512:1024], pso[1][0][:], [pso[1][1]], [b_obf[q]])
                    pt, pb = PS()
                    ptb16 = pt[:].bitcast(BF16)
                    for kc in range(KC):
                        P.pe(lambda e, kc=kc, q=q, ptb16=ptb16: e.transpose(out=ptb16[:, kc * 128:(kc + 1) * 128],
                                                                           in_=obf[q][:, kc * 128:(kc + 1) * 128],
                                                                           identity=ident_bf[:]), [b_obf[q], b_c], [pb])
                    CPo("act", oT[q][:], ptb16.rearrange("p (a t) -> p a t", a=8), [pb], [b_oT[q]])
                    pA, pAb = PS()
                    pB, pBb = PS()
                    for kc in range(KC):
                        MM(pA[:], oT[q][:, kc, :], wo0[:, kc, :], kc == 0, kc == KC - 1, [b_oT[q], wo0b], [pAb])
                    for kc in range(KC):
                        MM(pB[:], oT[q][:, kc, :], wo1[:, kc, :], kc == 0, kc == KC - 1, [b_oT[q], wo1b], [pBb])
                    postnorm(pA, pAb, pB, pBb, gp, gp_b, j, pws)
                release(5)
                P.barrier()

        def load_x(loc0, nt):
            for j in range(nt):
                DMA("pool", h[:, j, :], xloc[(loc0 + j) * 128:(loc0 + j + 1) * 128, :], [], [h_b[j]])

        STOP = int(os.environ.get("MK_STOP", "99"))
        pcs = {}
        for nm in in_names:
            pcs[nm] = acquire(nm)
        ncast_grp = -(-len(cast_q) // max(1, len(tok_groups(0, NPRE))))
        for (j0, n) in tok_groups(0, NPRE):
            if STOP < 1:
                break
            load_x(j0, n)
            cast_some(ncast_grp)
            layer0_mix(n, [(0, n)], True, pcs)
        out_ops = []
        mbase = 0
        for bi, nt in enumerate(BLOCKS):
            if STOP < 2:
                break
            load_x(NPRE + mbase, nt)
            if bi > 0:
                pcs = {nm: acquire(nm) for nm in in_names}
            if mbase == 0 and nt > 1:
                layer0_mix(nt, [(0, 1), (1, nt - 1)], False, pcs, reset_before=1)
            elif mbase == 1:
                layer0_mix(nt, [(0, nt)], False, pcs, reset_before=0)
            else:
                layer0_mix(nt, [(0, nt)], False, pcs)
            dbg_dump(0, mbase, nt)
            if STOP >= 3:
                xattn(0, nt)
            dbg_dump(1, mbase, nt)
            if STOP >= 4:
                mlp(0, nt)
            dbg_dump(2, mbase, nt)
            if STOP >= 5:
                swa(nt, mbase)
            dbg_dump(3, mbase, nt)
            if STOP >= 6:
                xattn(1, nt)
            dbg_dump(4, mbase, nt)
            if STOP >= 7:
                mlp(1, nt)
            dbg_dump(5, mbase, nt)
            for j in range(nt):
                m = mbase + j
                if m >= 1:
                    out_ops.append(DMA("pool", outd[(m - 1) * 128:m * 128, :], h[:, j, :], [h_b[j]], []))
            mbase += nt
        P.emit(out_ops)
        nc._prog_stats = P.stats
    return nc


def _host_consts():
    r = np.arange(128)[:, None]
    c = np.arange(128)[None, :]
    mprev = np.where(c > r, 0.0, NEG).astype(np.float32)
    inv = np.power(10000.0, -np.arange(32, dtype=np.float32) * 2.0 / 64).astype(np.float32)
    invf = np.broadcast_to(inv[None, :], (128, 32)).copy()
    return mprev, invf


def run_module(inputs, NOWN, NPRE, BLOCKS, dbg=False):
    x = np.asarray(inputs["x"], dtype=np.float32)
    B = x.shape[0]
    ncores = 2 * B
    S = x.shape[1]
    assert S == 2 * NOWN * 128 and NPRE == NOWN - 1 and sum(BLOCKS) == NOWN + 1
    mprev, invf = _host_consts()
    nc = build_nc(NPRE, BLOCKS, dbg=dbg)
    in_maps = []
    NLOC = NPRE + 1 + NOWN
    pos = np.asarray(inputs["positions"]).astype(np.int32)
    for c in range(ncores):
        b, s = c // 2, c % 2
        xl = np.zeros((NLOC * 128, D), np.float32)
        pl = np.zeros((NOWN + 1, 128), np.int32)
        if s == 1:
            xl[:] = x[b]
            pl[:] = pos[b].reshape(2 * NOWN, 128)[NOWN - 1:]
        else:
            xl[(NPRE + 1) * 128:] = x[b, :NOWN * 128]
            pl[1:] = pos[b].reshape(2 * NOWN, 128)[:NOWN]
        m = {"xloc": xl, "mem": np.ascontiguousarray(inputs["mem"][b], dtype=np.float32), "pos": pl,
             "flag": np.full((128, 1), float(s), np.float32),
             "mask_first": mprev if s == 1 else np.full((128, 128), NEG, np.float32),
             "invf": invf}
        for n_ in W_NAMES:
            m[n_] = np.ascontiguousarray(inputs[n_], dtype=np.float32)
        in_maps.append(m)
    res = run_bass_kernel_spmd(nc, in_maps, core_ids=list(range(ncores)))
    if os.environ.get("MK_VERBOSE"):
        print("exec_time_ns", getattr(res, "exec_time_ns", None), flush=True)
    out = np.zeros((B, S, D), np.float32)
    dbgs = None
    for c in range(ncores):
        b, s = c // 2, c % 2
        out[b, s * NOWN * 128:(s + 1) * NOWN * 128] = res.results[c]["out"]
    if dbg:
        dbgs = [res.results[c]["dbg"] for c in range(ncores)]
    return out, dbgs, nc


def kernel(**inputs):
    out, _, _ = run_module(inputs, 32, 31, [1, 4, 4, 4, 4, 4, 4, 4, 4])
    return out
```

```python
import math
import os
from contextlib import ExitStack

import numpy as np
import concourse.bass as bass
import concourse.mybir as mybir
from concourse.bass_utils import run_bass_kernel_spmd

F32 = mybir.dt.float32
BF16 = mybir.dt.bfloat16
I32 = mybir.dt.int32
AF = mybir.ActivationFunctionType
ALU = mybir.AluOpType
AX = mybir.AxisListType


class Buf:
    __slots__ = ("name", "w", "r")

    def __init__(self, name):
        self.name = name
        self.w = None
        self.r = []


class Op:
    __slots__ = ("eng", "fn", "deps", "is_dma", "sem", "cnt", "has_dep", "idx", "pre")

    def __init__(self, eng, fn, is_dma):
        self.eng = eng
        self.fn = fn
        self.deps = []
        self.is_dma = is_dma
        self.sem = None
        self.cnt = None
        self.has_dep = False
        self.pre = None


class Prog:
    ENGS = ("pe", "act", "dve", "pool", "sp")
    NDMA = {"sp": 12, "pool": 12, "act": 4}

    def __init__(self, nc):
        self.nc = nc
        self.streams = {e: [] for e in self.ENGS}
        self.nbuf = 0

    def buf(self, name=None):
        self.nbuf += 1
        return Buf(name or f"b{self.nbuf}")

    def bufs(self, n, name="b"):
        return [self.buf(f"{name}{i}") for i in range(n)]

    def _add(self, o, reads, writes):
        deps = []
        for b in reads:
            if b.w is not None:
                deps.append(("raw", b.w))
            b.r.append(o)
        for b in writes:
            if b.w is not None:
                deps.append(("waw", b.w))
            for r in b.r:
                if r is not o:
                    deps.append(("war", r))
            b.r = []
            b.w = o
        seen = set()
        for kind, d in deps:
            if d is o or id(d) in seen:
                continue
            if (not d.is_dma) and d.eng == o.eng and not o.is_dma:
                if kind != "raw" or o.eng == "pe":
                    continue
            seen.add(id(d))
            o.deps.append(d)
            d.has_dep = True
        self.streams[o.eng].append(o)
        return o

    def op(self, eng, fn, reads=(), writes=()):
        return self._add(Op(eng, fn, False), list(reads), list(writes))

    def dma(self, eng, fn, reads=(), writes=()):
        return self._add(Op(eng, fn, True), list(reads), list(writes))

    def pe(self, fn, reads=(), writes=()):
        return self.op("pe", fn, reads, writes)

    def act(self, fn, reads=(), writes=()):
        return self.op("act", fn, reads, writes)

    def dve(self, fn, reads=(), writes=()):
        return self.op("dve", fn, reads, writes)

    def pool(self, fn, reads=(), writes=()):
        return self.op("pool", fn, reads, writes)

    def barrier(self):
        lasts = []
        for e in ("pe", "act", "dve", "pool"):
            for o in reversed(self.streams[e]):
                if not o.is_dma and o.fn is not None:
                    lasts.append(o)
                    break
        for e in ("pe", "act", "dve", "pool"):
            w = Op(e, None, False)
            for d in lasts:
                w.deps.append(d)
                d.has_dep = True
            self.streams[e].append(w)

    def emit(self, final_deps=()):
        nc = self.nc
        es = ExitStack()
        with es:
            csem = {e: es.enter_context(nc.semaphore(f"c_{e}")) for e in ("pe", "act", "dve", "pool")}
            dsem = {q: [es.enter_context(nc.semaphore(f"d_{q}{i}")) for i in range(n)]
                    for q, n in self.NDMA.items()}
            fin = Op("pool", None, False)
            for d in final_deps:
                fin.deps.append(d)
                d.has_dep = True
            for e in self.ENGS:
                if e != "pool":
                    for last in reversed(self.streams[e]):
                        if last.fn is not None:
                            fin.deps.append(last)
                            last.has_dep = True
                            break
            self.streams["pool"].append(fin)
            ccount = {e: 0 for e in csem}
            dcount = {q: 0 for q in dsem}
            for e in self.ENGS:
                for o in self.streams[e]:
                    if o.is_dma:
                        j = dcount[e]
                        dcount[e] += 1
                        k = len(dsem[e])
                        o.sem = dsem[e][j % k]
                        o.cnt = 16 * (j // k + 1)
                        o.pre = (o.sem, 16 * (j // k)) if j >= k else None
                    elif o.has_dep:
                        ccount[e] += 1
                        o.sem = csem[e]
                        o.cnt = ccount[e]
            self.stats = {"ccount": ccount, "dcount": dcount,
                          "n": {e: len(self.streams[e]) for e in self.ENGS}}
            engobj = {"pe": "tensor", "act": "scalar", "dve": "vector", "pool": "gpsimd", "sp": "sync"}

            def run_stream(e, eng):
                waited = {}
                nwait = 0
                for o in self.streams[e]:
                    need = {}
                    if o.pre is not None:
                        need[o.pre[0]] = o.pre[1]
                    for d in o.deps:
                        assert d.sem is not None, "dep without semaphore"
                        if need.get(d.sem, 0) < d.cnt:
                            need[d.sem] = d.cnt
                    for s, c in need.items():
                        if waited.get(s, 0) < c:
                            eng.wait_ge(s, c)
                            waited[s] = c
                            nwait += 1
                    if o.fn is None:
                        continue
                    ins = o.fn(eng)
                    if o.sem is not None:
                        ins.then_inc(o.sem, 16 if o.is_dma else 1)
                self.stats.setdefault("nwait", {})[e] = nwait

            with nc.Block() as block:
                for e in self.ENGS:
                    if not self.streams[e]:
                        continue

                    def f(eng, e=e):
                        run_stream(e, eng)

                    getattr(block, engobj[e])(f)


D = 1024
KC = 8
EPS = 1e-6
NEG = -1.0e9
PI = math.pi

W_NAMES = ["ev_w_in", "ev_b_if", "ev_conv_w", "ev_conv_b", "ev_rg_wa", "ev_rg_ba", "ev_rg_wx", "ev_rg_bx",
           "ev_rg_lambda", "ev_w_out", "od_w_qkv", "od_sinks", "od_w_o", "g_mix_pre", "g_mix_post",
           "g_x_pre", "g_x_post", "g_mem", "w_xq", "w_xkv", "w_xo", "g_ff_pre", "g_ff_post", "w_ff1", "w_ff2"]
W_SHAPES = {"ev_w_in": [1, 1024, 3080], "ev_b_if": [1, 8], "ev_conv_w": [1, 4, 512], "ev_conv_b": [1, 512],
            "ev_rg_wa": [1, 4, 128, 128], "ev_rg_ba": [1, 512], "ev_rg_wx": [1, 4, 128, 128], "ev_rg_bx": [1, 512],
            "ev_rg_lambda": [1, 512], "ev_w_out": [1, 1024, 1024], "od_w_qkv": [1, 1024, 1536],
            "od_sinks": [1, 16], "od_w_o": [1, 1024, 1024], "g_mix_pre": [2, 1024], "g_mix_post": [2, 1024],
            "g_x_pre": [2, 1024], "g_x_post": [2, 1024], "g_mem": [2, 1024], "w_xq": [2, 1024, 1024],
            "w_xkv": [2, 1024, 2048], "w_xo": [2, 1024, 1024], "g_ff_pre": [2, 1024], "g_ff_post": [2, 1024],
            "w_ff1": [2, 1024, 4096], "w_ff2": [2, 4096, 1024]}


def build_nc(NPRE, BLOCKS, dbg=False):
    nc = bass.Bass("TRN2", target_bir_lowering=False)
    NMAIN = sum(BLOCKS)
    NOWN = NMAIN - 1
    NLOC = NPRE + NMAIN
    NTB = max(BLOCKS)
    TB = NTB * 128

    def din(name, shape, dt=F32):
        return nc.dram_tensor(name, list(shape), dt, kind="ExternalInput").ap()

    xloc = din("xloc", [NLOC * 128, D])
    mem = din("mem", [256, D])
    posd = din("pos", [NMAIN, 128], I32)
    flagd = din("flag", [128, 1])
    maskfd = din("mask_first", [128, 128])
    invfd = din("invf", [128, 32])
    Wd = {n: din(n, W_SHAPES[n]) for n in W_NAMES}
    outd = nc.dram_tensor("out", [NOWN * 128, D], F32, kind="ExternalOutput").ap()
    dbgd = nc.dram_tensor("dbg", [6, NMAIN * 128, D], F32, kind="ExternalOutput").ap() if dbg else None

    P = Prog(nc)
    es = ExitStack()
    with es:
        ncd = es.enter_context(nc.allow_non_contiguous_dma(reason="small parameter loads"))

        sbn = [0]

        def sb(name, shape, dt, st=es):
            sbn[0] += 1
            return st.enter_context(nc.sbuf_tensor(f"{name}_{sbn[0]}", list(shape), dt))

        def MM(out, lhsT, rhs, start, stop, r, w):
            return P.pe(lambda e: e.matmul(out, lhsT=lhsT, rhs=rhs, start=start, stop=stop), r, w)

        def ACTF(out, in_, func, r, w, **kw):
            return P.act(lambda e: e.activation(out=out, in_=in_, func=func, **kw), r, w)

        def TTo(eng, out, in0, in1, op, r, w):
            return P.op(eng, lambda e: e.tensor_tensor(out=out, in0=in0, in1=in1, op=op), r, w)

        def TSo(eng, out, in0, s1, s2, op0, op1, r, w):
            if s2 is None:
                return P.op(eng, lambda e: e.tensor_scalar(out=out, in0=in0, scalar1=s1, scalar2=None, op0=op0), r, w)
            return P.op(eng, lambda e: e.tensor_scalar(out=out, in0=in0, scalar1=s1, scalar2=s2, op0=op0, op1=op1), r, w)

        def STTo(out, in0, scalar, in1, op0, op1, r, w):
            return P.dve(lambda e: e.scalar_tensor_tensor(out=out, in0=in0, scalar=scalar, in1=in1, op0=op0, op1=op1), r, w)

        def CPo(eng, out, in_, r, w):
            if eng == "act":
                return P.act(lambda e: e.activation(out=out, in_=in_, func=AF.Copy), r, w)
            return P.op(eng, lambda e: e.tensor_copy(out=out, in_=in_), r, w)

        def MSET(eng, ap, val, w):
            return P.op(eng, lambda e: e.memset(ap, val), [], w)

        def DMA(q, out, in_, r, w):
            return P.dma(q, lambda e: e.dma_start(out=out, in_=in_), r, w)

        ps_t = [es.enter_context(nc.psum_tensor(f"ps{i}", [128, 512], F32)) for i in range(8)]
        ps_b = P.bufs(8, "ps")
        ps_i = [0]

        def PS():
            i = ps_i[0]
            ps_i[0] = (i + 1) % 6
            return ps_t[i], ps_b[i]

        b_c = P.buf("const")
        ident_f = sb("ident_f", [128, 128], F32)
        ident_bf = sb("ident_bf", [128, 128], BF16)
        ones4 = sb("ones4", [4, 128], F32)
        onesrow = sb("onesrow", [4, TB], F32)
        tri = sb("tri", [128, 128], F32)
        mask_pc = sb("mask_pc", [128, 256], F32)
        mask_fc = sb("mask_fc", [128, 256], F32)
        invf = sb("invf_sb", [128, 32], F32)
        flag = sb("flag_sb", [128, 1], F32)
        MSET("pool", ident_f[:], 0.0, [b_c])
        P.pool(lambda e: e.affine_select(out=ident_f[:], in_=ident_f[:], pattern=[[-1, 128]], compare_op=ALU.not_equal,
                                         fill=1.0, base=0, channel_multiplier=1), [b_c], [b_c])
        CPo("dve", ident_bf[:], ident_f[:], [b_c], [b_c])
        MSET("pool", ones4[:], 1.0, [b_c])
        MSET("pool", onesrow[:], 1.0, [b_c])
        MSET("pool", tri[:], 1.0, [b_c])
        P.pool(lambda e: e.affine_select(out=tri[:], in_=tri[:], pattern=[[1, 128]], compare_op=ALU.is_ge,
                                         fill=0.0, base=0, channel_multiplier=-1), [b_c], [b_c])
        MSET("pool", mask_pc[:], 0.0, [b_c])
        P.pool(lambda e: e.affine_select(out=mask_pc[:, 0:128], in_=mask_pc[:, 0:128], pattern=[[1, 128]],
                                         compare_op=ALU.is_gt, fill=NEG, base=0, channel_multiplier=-1), [b_c], [b_c])
        P.pool(lambda e: e.affine_select(out=mask_pc[:, 128:256], in_=mask_pc[:, 128:256], pattern=[[-1, 128]],
                                         compare_op=ALU.is_ge, fill=NEG, base=0, channel_multiplier=1), [b_c], [b_c])
        DMA("pool", mask_fc[:, 0:128], maskfd, [], [b_c])
        CPo("pool", mask_fc[:, 128:256], mask_pc[:, 128:256], [b_c], [b_c])
        DMA("pool", invf[:], invfd, [], [b_c])
        DMA("pool", flag[:], flagd, [], [b_c])

        bif = sb("bif", [4, 2], F32)
        DMA("pool", bif[:, 0:1], Wd["ev_b_if"][0, 0:4].rearrange("(p o) -> p o", o=1), [], [b_c])
        DMA("pool", bif[:, 1:2], Wd["ev_b_if"][0, 4:8].rearrange("(p o) -> p o", o=1), [], [b_c])
        TSo("dve", bif[:, 1:2], bif[:, 1:2], -1.0, None, ALU.mult, None, [b_c], [b_c])
        cw = sb("cw", [128, 4, 4], F32)
        for j in range(4):
            DMA("pool", cw[:, j, :], Wd["ev_conv_w"][0, j, :].rearrange("(n p) -> p n", p=128), [], [b_c])
        cb = sb("cb", [128, 4], F32)
        rba = sb("rba", [128, 4], F32)
        rbx = sb("rbx", [128, 4], F32)
        lam = sb("lam", [128, 4], F32)
        cl = sb("cl", [128, 8], F32)
        for t_, n_ in ((cb, "ev_conv_b"), (rba, "ev_rg_ba"), (rbx, "ev_rg_bx"), (lam, "ev_rg_lambda")):
            DMA("pool", t_[:], Wd[n_][0, :].rearrange("(n p) -> p n", p=128), [], [b_c])
        ACTF(lam[:], lam[:], AF.Exp, [b_c], [b_c], scale=-1.0)
        ACTF(lam[:], lam[:], AF.Ln, [b_c], [b_c], bias=1.0)
        TSo("dve", cl[:, 0:4], lam[:], -8.0, None, ALU.mult, None, [b_c], [b_c])
        TSo("dve", cl[:, 4:8], lam[:], -16.0, None, ALU.mult, None, [b_c], [b_c])
        sinks = sb("sinks", [128, 16], F32)
        DMA("pool", sinks[:], Wd["od_sinks"][0:1, :].to_broadcast([128, 16]), [], [b_c])
        posi = sb("posi", [128, NMAIN], I32)
        posf = sb("posf", [128, NMAIN], F32)
        DMA("pool", posi[:], posd.rearrange("m p -> p m"), [], [b_c])
        CPo("dve", posf[:], posi[:], [b_c], [b_c])
        wgif = sb("wgif", [128, 8, 8], BF16)
        DMA("pool", wgif[:], Wd["ev_w_in"][0, :, 2048:2056].rearrange("(k p) n -> p k n", p=128), [], [b_c])
        wrg = sb("wrg", [128, 2, 4, 128], BF16)
        DMA("pool", wrg[:, 0], Wd["ev_rg_wa"][0].rearrange("n c d -> c n d"), [], [b_c])
        DMA("pool", wrg[:, 1], Wd["ev_rg_wx"][0].rearrange("n c d -> c n d"), [], [b_c])

        pieces = {}
        cast_q = []
        cast_done = {}
        cast_pos = [0]

        def cast_some(n):
            while n > 0 and cast_pos[0] < len(cast_q):
                name, t, src3, b = cast_q[cast_pos[0]]
                cast_pos[0] += 1
                DMA("pool", t, src3, [], [b])
                cast_done[name] = True
                n -= 1

        def mk_piece(name, src3):
            kk = src3.shape[1]
            nn = src3.shape[2]
            t = nc.dram_tensor("wsc_" + name, [128, kk, nn], BF16, kind="Internal").ap()
            b = P.buf("wsc_" + name)
            cast_q.append((name, t, src3, b))
            pieces[name] = (t, b, kk, nn)

        def std_pieces(prefix, w2d, ncols):
            names = []
            for c in range(ncols // 512):
                nm = f"{prefix}_{c}"
                mk_piece(nm, w2d[:, c * 512:(c + 1) * 512].rearrange("(k p) n -> p k n", p=128))
                names.append(nm)
            return names

        def mlp_pieces(l):
            names = []
            for half in range(2):
                for c in range(4 * half, 4 * half + 4):
                    nm = f"ff1_{l}_{c}"
                    mk_piece(nm, Wd["w_ff1"][l][:, c * 512:(c + 1) * 512].rearrange("(k p) n -> p k n", p=128))
                    names.append(nm)
                for c in range(4 * half, 4 * half + 4):
                    nm = f"ff2_{l}_{c}"
                    mk_piece(nm, Wd["w_ff2"][l][c * 512:(c + 1) * 512, :].rearrange("(k p) n -> p k n", p=128))
                    names.append(nm)
            return names

        seq = []
        xkv_names = []
        for l in range(2):
            xkv_names.append(std_pieces(f"xkv{l}", Wd["w_xkv"][l], 2048))
        win = Wd["ev_w_in"][0]
        in_names = []
        for nm, c0 in (("in_q", 0), ("in_k", 512), ("in_v", 1024), ("in_o", 1536), ("in_rx", 2056), ("in_ry", 2568)):
            mk_piece(nm, win[:, c0:c0 + 512].rearrange("(k p) n -> p k n", p=128))
            in_names.append(nm)
        L0 = list(in_names)
        L0 += std_pieces("out", Wd["ev_w_out"][0], 1024)
        L0 += std_pieces("xq0", Wd["w_xq"][0], 1024)
        L0 += std_pieces("xo0", Wd["w_xo"][0], 1024)
        L0 += mlp_pieces(0)
        L1 = std_pieces("qkv", Wd["od_w_qkv"][0], 1536)
        L1 += std_pieces("o1", Wd["od_w_o"][0], 1024)
        L1 += std_pieces("xq1", Wd["w_xq"][1], 1024)
        L1 += std_pieces("xo1", Wd["w_xo"][1], 1024)
        L1 += mlp_pieces(1)
        seq = xkv_names[0] + xkv_names[1]
        for _ in BLOCKS:
            seq += L0 + L1

        NSLOT = 8
        slot_t = [sb(f"slot{i}", [128, 4096], BF16) for i in range(NSLOT)]
        slot_b = P.bufs(NSLOT, "slot")
        ring = {"use": 0, "done": 0, "loaded": 0}

        def ring_load():
            i = ring["loaded"]
            if i >= len(seq):
                return
            t, b, kk, nn = pieces[seq[i]]
            while seq[i] not in cast_done:
                cast_some(1)
            s = i % NSLOT
            DMA("sp", slot_t[s][:].rearrange("p (k n) -> p k n", k=kk), t, [b], [slot_b[s]])
            ring["loaded"] += 1

        def acquire(expect=None):
            i = ring["use"]
            ring["use"] += 1
            assert i < ring["loaded"], "ring underflow"
            if expect is not None and not os.environ.get("MK_STOP"):
                assert seq[i].startswith(expect), (seq[i], expect)
            t, b, kk, nn = pieces[seq[i]]
            s = i % NSLOT
            return slot_t[s][:].rearrange("p (k n) -> p k n", k=kk), slot_b[s]

        def release(n=1):
            for _ in range(n):
                ring["done"] += 1
                ring_load()

        for _ in range(NSLOT):
            ring_load()

        h = sb("h", [128, NTB, D], F32)
        h_b = P.bufs(NTB, "h")
        grep = [sb(f"grep{i}", [128, D], F32) for i in range(2)]
        grep_b = P.bufs(2, "grep")
        gcnt = [0]

        def gload(name, l):
            i = gcnt[0] % 2
            gcnt[0] += 1
            DMA("pool", grep[i][:], Wd[name][l:l + 1, :].to_broadcast([128, D]), [], [grep_b[i]])
            return grep[i], grep_b[i]

        KT = sb("KT", [128, 2, 8, 256], BF16)
        VM = sb("VM", [128, 2, 2, D], BF16)
        b_kv = P.buf("kvmem")
        Cst = sb("Cst", [128, 4, 129], F32)
        b_C = P.buf("C")
        gcar = sb("gcar", [4, 2], F32)
        b_gc = P.buf("gcar")
        hst = sb("hst", [128, 4], F32)
        rxhist = sb("rxhist", [128, 4, 3], F32)
        b_rgs = P.bufs(4, "rgst")
        MSET("dve", Cst[:], 0.0, [b_C])
        MSET("dve", gcar[:], 0.0, [b_gc])
        MSET("dve", hst[:], 0.0, b_rgs)
        MSET("dve", rxhist[:], 0.0, b_rgs)
        kTs = sb("kTs", [128, 2, 8, 128], BF16)
        vs = sb("vs", [128, 2, 256], BF16)
        kv_b = P.bufs(2, "swakv")
        MSET("dve", kTs[:], 0.0, [kv_b[0], kv_b[1]])
        MSET("dve", vs[:], 0.0, [kv_b[0], kv_b[1]])

        def prenorm(src_fn, src_bufs, nt, g, g_b, uT, uT_b, ws, tag):
            ssq = sb(f"pn_ssq{tag}", [128, NTB], F32, ws)
            junk = sb(f"pn_junk{tag}", [128, D], BF16, ws)
            ubf = [sb(f"pn_u{tag}{i}", [128, D], BF16, ws) for i in range(2)]
            b_ssq = P.bufs(nt, "ssq")
            b_junk = P.buf()
            b_u = P.bufs(2, "u")
            for j in range(nt):
                src = src_fn(j)
                ACTF(junk[:], src, AF.Square, [src_bufs[j]], [b_junk, b_ssq[j]], accum_out=ssq[:, j:j + 1])
                ACTF(ssq[:, j:j + 1], ssq[:, j:j + 1], AF.Sqrt, [b_ssq[j]], [b_ssq[j]], scale=1.0 / D, bias=EPS)
                P.dve(lambda e, j=j: e.reciprocal(out=ssq[:, j:j + 1], in_=ssq[:, j:j + 1]), [b_ssq[j]], [b_ssq[j]])
                u = ubf[j % 2]
                STTo(u[:], src, ssq[:, j:j + 1], g[:], ALU.mult, ALU.mult, [src_bufs[j], b_ssq[j], g_b], [b_u[j % 2]])
                pt, pb = PS()
                ptb = pt[:].bitcast(BF16)
                for kc in range(KC):
                    P.pe(lambda e, kc=kc, u=u, ptb=ptb: e.transpose(out=ptb[:, kc * 128:(kc + 1) * 128],
                                                                   in_=u[:, kc * 128:(kc + 1) * 128],
                                                                   identity=ident_bf[:]),
                         [b_u[j % 2], b_c], [pb])
                CPo("act", uT[:, :, j * 128:(j + 1) * 128], ptb.rearrange("p (k t) -> p k t", k=KC), [pb], [uT_b[j]])

        def postnorm(psA, pbA, psB, pbB, g, g_b, j, ws_t):
            ss2, tmp, junk = ws_t["ss2"], ws_t["tmp"], ws_t["junk"]
            k = ws_t["cnt"] % 2
            ws_t["cnt"] += 1
            b_ss, b_tmp, b_jk = ws_t["b_ss"][k], ws_t["b_tmp"][k], ws_t["b_jk"]
            s2 = ss2[:, 4 * k:4 * k + 4]
            ACTF(junk[:, 0:512], psA[:], AF.Square, [pbA], [b_jk, b_ss], accum_out=s2[:, 0:1])
            ACTF(junk[:, 512:1024], psB[:], AF.Square, [pbB], [b_jk, b_ss], accum_out=s2[:, 1:2])
            TTo("dve", s2[:, 2:3], s2[:, 0:1], s2[:, 1:2], ALU.add, [b_ss], [b_ss])
            ACTF(s2[:, 2:3], s2[:, 2:3], AF.Sqrt, [b_ss], [b_ss], scale=1.0 / D, bias=EPS)
            P.dve(lambda e: e.reciprocal(out=s2[:, 3:4], in_=s2[:, 2:3]), [b_ss], [b_ss])
            tm = tmp[k]
            STTo(tm[:, 0:512], psA[:], s2[:, 3:4], g[:, 0:512], ALU.mult, ALU.mult, [pbA, b_ss, g_b], [b_tmp])
            STTo(tm[:, 512:1024], psB[:], s2[:, 3:4], g[:, 512:1024], ALU.mult, ALU.mult, [pbB, b_ss, g_b], [b_tmp])
            TTo("pool", h[:, j, :], h[:, j, :], tm[:], ALU.add, [h_b[j], b_tmp], [h_b[j]])

        def mk_post_ws(ws, tag):
            return {"ss2": sb(f"po_ss{tag}", [128, 8], F32, ws), "junk": sb(f"po_junk{tag}", [128, D], BF16, ws),
                    "tmp": [sb(f"po_tmp{tag}0", [128, D], F32, ws)] * 2,
                    "cnt": 0, "b_ss": P.bufs(2, "poss"), "b_tmp": [P.buf("potmp")] * 2, "b_jk": P.buf()}

        def proj_tm(uT, uT_b, j, wv, wb, c0, ncol):
            pt, pb = PS()
            for kc in range(KC):
                MM(pt[:, 0:ncol], uT[:, kc, j * 128:(j + 1) * 128], wv[:, kc, c0:c0 + ncol], kc == 0, kc == KC - 1,
                   [uT_b[j], wb], [pb])
            return pt, pb

        def tok_groups(t0, nt):
            out = []
            j = t0
            while j < t0 + nt:
                n = min(4, t0 + nt - j)
                out.append((j, n))
                j += n
            return out

        def proj_fm(uT, uT_b, t0, nt, wv, wb, c0, evac):
            for (j0, n) in tok_groups(t0, nt):
                pt, pb = PS()
                for kc in range(KC):
                    MM(pt[:, 0:n * 128], wv[:, kc, c0:c0 + 128], uT[:, kc, j0 * 128:(j0 + n) * 128], kc == 0, kc == KC - 1,
                       [uT_b[j] for j in range(j0, j0 + n)] + [wb], [pb])
                evac(pt, pb, j0, n)

        def interleave(gens):
            gens = list(gens)
            while gens:
                for g_ in list(gens):
                    try:
                        next(g_)
                    except StopIteration:
                        gens.remove(g_)

        def dbg_dump(k, mbase, nt):
            if dbgd is None:
                return
            for j in range(nt):
                m = mbase + j
                DMA("pool", dbgd[k, m * 128:(m + 1) * 128, :], h[:, j, :], [h_b[j]], [])

        with ExitStack() as ws:
            memsb = sb("memsb", [128, 2, D], F32, ws)
            b_mem = P.bufs(2, "mem")
            for j in range(2):
                DMA("pool", memsb[:, j, :], mem[j * 128:(j + 1) * 128, :], [], [b_mem[j]])
            memT = sb("memT", [128, KC, 256], BF16, ws)
            b_memT = P.bufs(2, "memT")
            for l in range(2):
                g, g_b = gload("g_mem", l)
                prenorm(lambda j: memsb[:, j, :], b_mem, 2, g, g_b, memT, b_memT, ws, f"m{l}")
                for pc in range(2):
                    wv, wb = acquire(f"xkv{l}")
                    for c in range(4):
                        pt, pb = PS()
                        for kc in range(KC):
                            MM(pt[:, 0:256], wv[:, kc, c * 128:(c + 1) * 128], memT[:, kc, :], kc == 0, kc == KC - 1,
                               b_memT + [wb], [pb])
                        CPo("act", KT[:, l, pc * 4 + c, :], pt[:, 0:256], [pb], [b_kv])
                    release()
                for pc in range(2):
                    wv, wb = acquire(f"xkv{l}")
                    for j in range(2):
                        pt, pb = proj_tm(memT, b_memT, j, wv, wb, 0, 512)
                        CPo("dve", VM[:, l, j, pc * 512:(pc + 1) * 512], pt[:], [pb], [b_kv])
                    release()
            P.barrier()

        def layer0_mix(nt, ranges, state_only, pcs, reset_before=None):
            with ExitStack() as ws:
                g, g_b = gload("g_mix_pre", 0)
                if not state_only:
                    gp, gp_b = gload("g_mix_post", 0)
                uT = sb("uT", [128, KC, TB], BF16, ws)
                uT_b = P.bufs(nt, "uT")
                prenorm(lambda j: h[:, j, :], h_b, nt, g, g_b, uT, uT_b, ws, "a")
                g1 = sb("g1", [4, TB], F32, ws)
                g2 = sb("g2", [4, TB], F32, ws)
                g3 = sb("g3", [4, TB], F32, ws)
                gsm = sb("gsm", [4, 4 * NTB + 8 + 4 * NTB], F32, ws)
                tmx = gsm[:, 0:NTB]
                MgE = gsm[:, NTB:2 * NTB + 1]
                wpv = gsm[:, 2 * NTB + 4:3 * NTB + 4]
                wpd = gsm[:, 3 * NTB + 8:3 * NTB + 8 + 4 * NTB]
                gtm = sb("gtm", [128, NTB, 8], F32, ws)
                wpr = sb("wpr", [128, NTB, 4], F32, ws)
                b_g = P.buf("gates")
                k_tm = sb("k_tm", [128, NTB, 512], BF16, ws)
                v_aug = sb("v_aug", [128, NTB, 4, 129], BF16, ws)
                b_kt = P.bufs(nt, "ktm")
                b_va = P.bufs(nt, "vaug")
                MSET("pool", v_aug[:, :, :, 128:129], 1.0, b_va)
                ktl = [sb(f"ktl{i}", [128, 4, 128], BF16, ws) for i in range(2)]
                b_ktl = P.bufs(2, "ktl")
                if not state_only:
                    qT = sb("qT", [128, 4, TB], BF16, ws)
                    kT = sb("kT", [128, 4, TB], BF16, ws)
                    b_qT = P.bufs(nt, "qT")
                    b_kT = P.bufs(nt, "kT")
                    sig_o = sb("sig_o", [128, NTB, 512], BF16, ws)
                    b_so = P.bufs(nt, "sigo")
                    yT = sb("yT", [128, KC, TB], BF16, ws)
                    b_yTa = P.bufs(nt, "yTa")
                    b_yTb = P.bufs(nt, "yTb")
                    Cbf = sb("Cbf", [128, 4, 129], BF16, ws)
                    b_Cbf = P.buf("Cbf")
                    ptl = [sb(f"ptl{i}", [128, 4, 128], BF16, ws) for i in range(2)]
                    b_ptl = P.bufs(2, "ptl")
                    yal = [sb(f"yal{i}", [128, 512], BF16, ws) for i in range(2)]
                    b_yal = P.bufs(2, "yal")
                    dn = sb("dn", [128, 8], F32, ws)
                    b_dn = P.bufs(2, "dn")
                rgs = []
                for i_ in range(2):
                    rgs.append(dict(rxn=sb("rxn", [128, TB + 3], F32, ws), xc=sb("xc", [128, TB], F32, ws),
                                    xcb=sb("xcb", [128, TB], BF16, ws), rr=sb("rr", [128, TB], F32, ws),
                                    ii=sb("ii", [128, TB], F32, ws), a2=sb("a2", [128, TB], F32, ws),
                                    hs=sb("hs", [128, TB], F32, ws), b=P.bufs(7, "rgw")))

                for ri, (t0, n) in enumerate(ranges):
                    if reset_before is not None and ri == reset_before:
                        TSo("dve", Cst[:], Cst[:], flag[:, 0:1], None, ALU.mult, None, [b_C, b_c], [b_C])
                        TSo("dve", gcar[:], gcar[:], flag[0:4, 0:1], None, ALU.mult, None, [b_gc, b_c], [b_gc])
                        TSo("dve", hst[:], hst[:], flag[:, 0:1], None, ALU.mult, None, b_rgs + [b_c], b_rgs)
                        TSo("dve", rxhist[:].rearrange("p a b -> p (a b)"), rxhist[:].rearrange("p a b -> p (a b)"),
                            flag[:, 0:1], None, ALU.mult, None, b_rgs + [b_c], b_rgs)
                    T_ = n * 128
                    c0 = t0 * 128
                    R = slice(c0, c0 + T_)
                    ub = [uT_b[j] for j in range(t0, t0 + n)]
                    wq, wqb = pcs["in_q"]
                    wk_, wkb = pcs["in_k"]
                    wv_, wvb = pcs["in_v"]
                    wo_, wob = pcs["in_o"]
                    wrx, wrxb = pcs["in_rx"]
                    wry, wryb = pcs["in_ry"]
                    for (j0, gn) in tok_groups(t0, n):
                        cs = slice(j0 * 128, (j0 + gn) * 128)
                        gub = [uT_b[j] for j in range(j0, j0 + gn)]
                        pti, pbi = PS()
                        for kc in range(KC):
                            MM(pti[0:4, 0:gn * 128], wgif[:, kc, 0:4], uT[:, kc, cs], kc == 0, kc == KC - 1, gub + [b_c], [pbi])
                        ACTF(g1[:, cs], pti[0:4, 0:gn * 128], AF.Identity, [pbi, b_c], [b_g], bias=bif[:, 0:1])
                        ptf, pbf = PS()
                        for kc in range(KC):
                            MM(ptf[0:4, 0:gn * 128], wgif[:, kc, 4:8], uT[:, kc, cs], kc == 0, kc == KC - 1, gub + [b_c], [pbf])
                        ACTF(g2[:, cs], ptf[0:4, 0:gn * 128], AF.Exp, [pbf, b_c], [b_g], scale=-1.0, bias=bif[:, 1:2])
                    ACTF(g2[:, R], g2[:, R], AF.Ln, [b_g], [b_g], bias=1.0)
                    P.dve(lambda e, R=R, T_=T_: e.tensor_tensor_scan(out=g3[:, R], data0=onesrow[:, 0:T_], data1=g2[:, R],
                                                                     initial=gcar[:, 0:1], op0=ALU.mult, op1=ALU.subtract),
                          [b_g, b_gc, b_c], [b_g])
                    CPo("dve", gcar[:, 0:1], g3[:, c0 + T_ - 1:c0 + T_], [b_g], [b_gc])
                    TTo("dve", g1[:, R], g1[:, R], g3[:, R], ALU.subtract, [b_g], [b_g])
                    P.dve(lambda e, R=R, n=n: e.tensor_reduce(out=tmx[:, 0:n], in_=g1[:, R].rearrange("p (j t) -> p j t", t=128),
                                                              axis=AX.X, op=ALU.max), [b_g], [b_g])
                    CPo("dve", MgE[:, 0:1], gcar[:, 1:2], [b_gc], [b_g])
                    P.dve(lambda e, n=n: e.tensor_tensor_scan(out=MgE[:, 1:n + 1], data0=onesrow[:, 0:n], data1=tmx[:, 0:n],
                                                              initial=MgE[:, 0:1], op0=ALU.mult, op1=ALU.max),
                          [b_g, b_c], [b_g])
                    CPo("dve", gcar[:, 1:2], MgE[:, n:n + 1], [b_g], [b_gc])
                    TTo("dve", wpv[:, 0:n], MgE[:, 0:n], MgE[:, 1:n + 1], ALU.subtract, [b_g], [b_g])
                    ACTF(wpv[:, 0:n], wpv[:, 0:n], AF.Exp, [b_g], [b_g])
                    Mb = MgE[:, 1:n + 1].unsqueeze(2).to_broadcast([4, n, 128])
                    g1v = g1[:, R].rearrange("p (j t) -> p j t", t=128)
                    g2v = g2[:, R].rearrange("p (j t) -> p j t", t=128)
                    g3v = g3[:, R].rearrange("p (j t) -> p j t", t=128)
                    TTo("dve", g2v, g1v, Mb, ALU.subtract, [b_g], [b_g])
                    ACTF(g2[:, R], g2[:, R], AF.Exp, [b_g], [b_g], bias=math.log(128 ** -0.5))
                    TTo("dve", g3v, g3v, Mb, ALU.add, [b_g], [b_g])
                    ACTF(g3[:, R], g3[:, R], AF.Exp, [b_g], [b_g], scale=-1.0)
                    if not state_only:
                        for c in range(4):
                            proj_fm(uT, uT_b, t0, n, wq, wqb, c * 128,
                                    lambda pt, pb, j0, gn, c=c: CPo("act", qT[:, c, j0 * 128:(j0 + gn) * 128], pt[:, 0:gn * 128],
                                                                    [pb], [b_qT[j] for j in range(j0, j0 + gn)]))
                            proj_fm(uT, uT_b, t0, n, wk_, wkb, c * 128,
                                    lambda pt, pb, j0, gn, c=c: CPo("dve", kT[:, c, j0 * 128:(j0 + gn) * 128], pt[:, 0:gn * 128],
                                                                    [pb], [b_kT[j] for j in range(j0, j0 + gn)]))
                    for j in range(t0, t0 + n):
                        pt, pb = proj_tm(uT, uT_b, j, wk_, wkb, 0, 512)
                        CPo("act", k_tm[:, j, :], pt[:], [pb], [b_kt[j]])
                        pt, pb = proj_tm(uT, uT_b, j, wv_, wvb, 0, 512)
                        CPo("dve", v_aug[:, j, :, 0:128], pt[:].rearrange("p (h d) -> p h d", h=4), [pb], [b_va[j]])
                        if not state_only:
                            pt, pb = proj_tm(uT, uT_b, j, wo_, wob, 0, 512)
                            ACTF(sig_o[:, j, :], pt[:], AF.Sigmoid, [pb], [b_so[j]])

                    wpdv = wpd[:, 0:4 * n].rearrange("p (j h) -> p j h", h=4)
                    TTo("dve", wpdv, wpv[:, 0:n].unsqueeze(2).to_broadcast([4, n, 4]),
                        ident_f[0:4, 0:4].unsqueeze(1).to_broadcast([4, n, 4]), ALU.mult, [b_g, b_c], [b_g])
                    pt, pb = PS()
                    MM(pt[:, 0:4 * n], ones4[:], wpd[:, 0:4 * n], True, True, [b_g, b_c], [pb])
                    CPo("dve", wpr[:, t0:t0 + n, :], pt[:, 0:4 * n].rearrange("p (j h) -> p j h", h=4), [pb], [b_g])
                    pt, pb = PS()
                    for jj in range(n):
                        cj = slice(c0 + jj * 128, c0 + (jj + 1) * 128)
                        MM(pt[:, jj * 8:jj * 8 + 4], g2[:, cj], ident_f[0:4, 0:4], True, True, [b_g, b_c], [pb])
                        MM(pt[:, jj * 8 + 4:jj * 8 + 8], g3[:, cj], ident_f[0:4, 0:4], True, True, [b_g, b_c], [pb])
                    CPo("dve", gtm[:, t0:t0 + n, :], pt[:, 0:8 * n].rearrange("p (j h) -> p j h", h=8), [pb], [b_g])

                    for j in range(t0, t0 + n):
                        cj = slice(j * 128, (j + 1) * 128)
                        wk4 = gtm[:, j, 0:4]
                        kt = ktl[j % 2]
                        TTo("pool", kt[:], k_tm[:, j, :].rearrange("p (h d) -> p h d", h=4),
                            wk4.unsqueeze(2).to_broadcast([128, 4, 128]), ALU.mult, [b_kt[j], b_g], [b_ktl[j % 2]])
                        TTo("dve", Cst[:], Cst[:], wpr[:, j, :].unsqueeze(2).to_broadcast([128, 4, 129]), ALU.mult,
                            [b_C, b_g], [b_C])
                        if not state_only:
                            CPo("act", Cbf[:], Cst[:], [b_C], [b_Cbf])
                            pss, pssb = PS()
                            for h_ in range(4):
                                MM(pss[:, h_ * 128:(h_ + 1) * 128], kT[:, h_, cj], qT[:, h_, cj], True, True,
                                   [b_kT[j], b_qT[j]], [pssb])
                            ptb_ = ptl[j % 2]
                            for h_ in range(4):
                                STTo(ptb_[:, h_, :], pss[:, h_ * 128:(h_ + 1) * 128], gtm[:, j, h_:h_ + 1], tri[:],
                                     ALU.mult, ALU.mult, [pssb, b_g, b_c], [b_ptl[j % 2]])
                            psn = [PS(), PS()]
                            for h_ in range(4):
                                pn_, pnb = psn[h_ // 2]
                                reg = pn_[:, (h_ % 2) * 129:(h_ % 2) * 129 + 129]
                                MM(reg, ptb_[:, h_, :], v_aug[:, j, h_, :], True, False, [b_ptl[j % 2], b_va[j]], [pnb])
                                MM(reg, qT[:, h_, cj], Cbf[:, h_, :], False, True, [b_qT[j], b_Cbf], [pnb])
                            d_ = dn[:, 4 * (j % 2):4 * (j % 2) + 4]
                            for bk in range(2):
                                pn_, pnb = psn[bk]
                                ACTF(d_[:, 2 * bk:2 * bk + 2], pn_[:, 0:258].rearrange("p (h e) -> p h e", e=129)[:, :, 128],
                                     AF.Abs, [pnb], [b_dn[j % 2]])
                            TTo("dve", d_, d_, gtm[:, j, 4:8], ALU.max, [b_dn[j % 2], b_g], [b_dn[j % 2]])
                            P.dve(lambda e, d_=d_: e.reciprocal(out=d_, in_=d_), [b_dn[j % 2]], [b_dn[j % 2]])
                            ya = yal[j % 2]
                            for h_ in range(4):
                                pn_, pnb = psn[h_ // 2]
                                STTo(ya[:, h_ * 128:(h_ + 1) * 128], pn_[:, (h_ % 2) * 129:(h_ % 2) * 129 + 128], d_[:, h_:h_ + 1],
                                     sig_o[:, j, h_ * 128:(h_ + 1) * 128], ALU.mult, ALU.mult,
                                     [pnb, b_dn[j % 2], b_so[j]], [b_yal[j % 2]])
                            pt, pb = PS()
                            ptb16 = pt[:].bitcast(BF16)
                            for h_ in range(4):
                                P.pe(lambda e, h_=h_, ya=ya, ptb16=ptb16: e.transpose(out=ptb16[:, h_ * 128:(h_ + 1) * 128],
                                                                                      in_=ya[:, h_ * 128:(h_ + 1) * 128],
                                                                                      identity=ident_bf[:]),
                                     [b_yal[j % 2], b_c], [pb])
                            CPo("act", yT[:, 0:4, cj], ptb16[:, 0:512].rearrange("p (h t) -> p h t", h=4), [pb], [b_yTa[j]])
                        psk = [PS(), PS()]
                        for h_ in range(4):
                            pk_, pkb = psk[h_ // 2]
                            MM(pk_[:, (h_ % 2) * 129:(h_ % 2) * 129 + 129], kt[:, h_, :], v_aug[:, j, h_, :], True, True,
                               [b_ktl[j % 2], b_va[j]], [pkb])
                        for bk in range(2):
                            pk_, pkb = psk[bk]
                            TTo("dve", Cst[:, 2 * bk:2 * bk + 2, :], Cst[:, 2 * bk:2 * bk + 2, :],
                                pk_[:, 0:258].rearrange("p (h e) -> p h e", e=129), ALU.add, [pkb, b_C], [b_C])

                    def rg_chain(n_):
                        rg_ = rgs[n_ % 2]
                        b_rg = b_rgs[n_]
                        rxn, xc, xcb, rr, ii, a2, hs = (rg_["rxn"], rg_["xc"], rg_["xcb"], rg_["rr"], rg_["ii"], rg_["a2"], rg_["hs"])
                        b_rxn, b_xc, b_xcb, b_rr, b_ii, b_a2, b_hs = rg_["b"]
                        proj_fm(uT, uT_b, t0, n, wrx, wrxb, n_ * 128,
                                lambda pt, pb, j0, gn: CPo("act", rxn[:, 3 + (j0 - t0) * 128:3 + (j0 - t0 + gn) * 128],
                                                           pt[:, 0:gn * 128], [pb], [b_rxn]))
                        CPo("pool", rxn[:, 0:3], rxhist[:, n_, :], [b_rg], [b_rxn])
                        yield
                        TSo("dve", xc[:, 0:T_], rxn[:, 0:T_], cw[:, 0, n_:n_ + 1], cb[:, n_:n_ + 1], ALU.mult, ALU.add,
                            [b_rxn, b_c], [b_xc])
                        for jj in range(1, 4):
                            STTo(xc[:, 0:T_], rxn[:, jj:jj + T_], cw[:, jj, n_:n_ + 1], xc[:, 0:T_], ALU.mult, ALU.add,
                                 [b_rxn, b_c, b_xc], [b_xc])
                        CPo("pool", rxhist[:, n_, :], rxn[:, T_:T_ + 3], [b_rxn], [b_rg])
                        CPo("pool", xcb[:, 0:T_], xc[:, 0:T_], [b_xc], [b_xcb])
                        yield
                        for (j0, gn) in tok_groups(0, n):
                            cs = slice(j0 * 128, (j0 + gn) * 128)
                            pr, prb = PS()
                            MM(pr[:, 0:gn * 128], wrg[:, 0, n_, :], xcb[:, cs], True, True, [b_xcb, b_c], [prb])
                            ACTF(rr[:, cs], pr[:, 0:gn * 128], AF.Sigmoid, [prb, b_c], [b_rr], bias=rba[:, n_:n_ + 1])
                            pi_, pib = PS()
                            MM(pi_[:, 0:gn * 128], wrg[:, 1, n_, :], xcb[:, cs], True, True, [b_xcb, b_c], [pib])
                            ACTF(ii[:, cs], pi_[:, 0:gn * 128], AF.Sigmoid, [pib, b_c], [b_ii], bias=rbx[:, n_:n_ + 1])
                        yield
                        ACTF(a2[:, 0:T_], rr[:, 0:T_], AF.Exp, [b_rr, b_c], [b_a2], scale=cl[:, 4 + n_:5 + n_])
                        ACTF(rr[:, 0:T_], rr[:, 0:T_], AF.Exp, [b_rr, b_c], [b_rr], scale=cl[:, n_:n_ + 1])
                        TSo("dve", a2[:, 0:T_], a2[:, 0:T_], 1.0, None, ALU.min, None, [b_a2], [b_a2])
                        ACTF(a2[:, 0:T_], a2[:, 0:T_], AF.Sqrt, [b_a2], [b_a2], scale=-1.0, bias=1.0)
                        yield
                        TTo("pool", ii[:, 0:T_], ii[:, 0:T_], xc[:, 0:T_], ALU.mult, [b_ii, b_xc], [b_ii])
                        TTo("pool", a2[:, 0:T_], a2[:, 0:T_], ii[:, 0:T_], ALU.mult, [b_a2, b_ii], [b_a2])
                        yield
                        P.dve(lambda e, n_=n_, T_=T_, hs=hs, rr=rr, a2=a2: e.tensor_tensor_scan(out=hs[:, 0:T_], data0=rr[:, 0:T_], data1=a2[:, 0:T_],
                                                                           initial=hst[:, n_:n_ + 1], op0=ALU.mult, op1=ALU.add),
                              [b_rr, b_a2, b_rg], [b_hs])
                        CPo("pool", hst[:, n_:n_ + 1], hs[:, T_ - 1:T_], [b_hs], [b_rg])
                        yield
                        if not state_only:
                            proj_fm(uT, uT_b, t0, n, wry, wryb, n_ * 128,
                                    lambda pt, pb, j0, gn: ACTF(yT[:, 4 + n_, j0 * 128:(j0 + gn) * 128], pt[:, 0:gn * 128],
                                                                AF.Gelu_apprx_tanh, [pb],
                                                                [b_yTb[j] for j in range(j0, j0 + gn)]))
                            ybs = [b_yTb[j] for j in range(t0, t0 + n)]
                            TTo("dve", yT[:, 4 + n_, R], yT[:, 4 + n_, R], hs[:, 0:T_], ALU.mult, ybs + [b_hs], ybs)
                    for pair in ((0, 1), (2, 3)):
                        interleave([rg_chain(a_) for a_ in pair])
                if not state_only:
                    release(6)
                    wo0, wo0b = acquire("out")
                    wo1, wo1b = acquire("out")
                    pws = mk_post_ws(ws, "a")
                    for j in range(nt):
                        yb_ = [b_yTa[j], b_yTb[j]]
                        pA, pAb = PS()
                        pB, pBb = PS()
                        for kc in range(KC):
                            MM(pA[:], yT[:, kc, j * 128:(j + 1) * 128], wo0[:, kc, :], kc == 0, kc == KC - 1, yb_ + [wo0b], [pAb])
                        for kc in range(KC):
                            MM(pB[:], yT[:, kc, j * 128:(j + 1) * 128], wo1[:, kc, :], kc == 0, kc == KC - 1, yb_ + [wo1b], [pBb])
                        postnorm(pA, pAb, pB, pBb, gp, gp_b, j, pws)
                    release(2)
                P.barrier()

        def xattn(l, nt):
            SC = 256 ** -0.5
            with ExitStack() as ws:
                g, g_b = gload("g_x_pre", l)
                gp, gp_b = gload("g_x_post", l)
                uT = sb("uT", [128, KC, TB], BF16, ws)
                uT_b = P.bufs(nt, "uT")
                prenorm(lambda j: h[:, j, :], h_b, nt, g, g_b, uT, uT_b, ws, "x")
                xqT = sb("xqT", [128, 8, TB], BF16, ws)
                b_xq = P.bufs(nt, "xq")
                for pc in range(2):
                    wv, wb = acquire(f"xq{l}")
                    for c in range(4):
                        proj_fm(uT, uT_b, 0, nt, wv, wb, c * 128,
                                lambda pt, pb, j0, gn, cc=pc * 4 + c: CPo("act", xqT[:, cc, j0 * 128:(j0 + gn) * 128],
                                                                          pt[:, 0:gn * 128], [pb],
                                                                          [b_xq[j] for j in range(j0, j0 + gn)]))
                    release()
                wo0, wo0b = acquire(f"xo{l}")
                wo1, wo1b = acquire(f"xo{l}")
                pf = [sb(f"xpf{i}", [128, 4, 256], F32, ws) for i in range(2)]
                pn = [sb(f"xpn{i}", [128, 4, 256], BF16, ws) for i in range(2)]
                pT = [sb(f"xpT{i}", [128, 8, 128], BF16, ws) for i in range(2)]
                xoT = [sb(f"xoT{i}", [128, 8, 128], BF16, ws) for i in range(2)]
                sm = sb("xsm", [128, 2, 12], F32, ws)
                b_pf, b_pn, b_pT, b_xo, b_sm = (P.bufs(2, "xpf"), P.bufs(2, "xpn"), P.bufs(2, "xpT"),
                                                 P.bufs(2, "xoT"), P.bufs(2, "xsm"))
                pws = mk_post_ws(ws, "x")
                for j in range(nt):
                    q = j % 2
                    cj = slice(j * 128, (j + 1) * 128)
                    psc = [PS(), PS()]
                    for h_ in range(4):
                        pt, pb = psc[h_ // 2]
                        for dc in range(2):
                            MM(pt[:, (h_ % 2) * 256:(h_ % 2) * 256 + 256], xqT[:, 2 * h_ + dc, cj], KT[:, l, 2 * h_ + dc, :],
                               dc == 0, dc == 1, [b_xq[j], b_kv], [pb])
                    mx = sm[:, q, 0:4]
                    rs = sm[:, q, 4:8]
                    rrs = sm[:, q, 8:12]
                    for bk in range(2):
                        pt, pb = psc[bk]
                        P.dve(lambda e, pt=pt, mx=mx, bk=bk: e.tensor_reduce(out=mx[:, 2 * bk:2 * bk + 2],
                                                                             in_=pt[:].rearrange("p (h m) -> p h m", h=2),
                                                                             axis=AX.X, op=ALU.max), [pb], [b_sm[q]])
                    TSo("dve", mx, mx, -SC, None, ALU.mult, None, [b_sm[q]], [b_sm[q]])
                    for h_ in range(4):
                        pt, pb = psc[h_ // 2]
                        ACTF(pf[q][:, h_, :], pt[:, (h_ % 2) * 256:(h_ % 2) * 256 + 256], AF.Exp, [pb, b_sm[q]], [b_pf[q], b_sm[q]],
                             scale=SC, bias=mx[:, h_:h_ + 1], accum_out=rs[:, h_:h_ + 1])
                    P.dve(lambda e, rs=rs, rrs=rrs: e.reciprocal(out=rrs, in_=rs), [b_sm[q]], [b_sm[q]])
                    TTo("pool", pn[q][:], pf[q][:], rrs.unsqueeze(2).to_broadcast([128, 4, 256]), ALU.mult,
                        [b_pf[q], b_sm[q]], [b_pn[q]])
                    pt, pb = PS()
                    ptb16 = pt[:].bitcast(BF16)
                    for h_ in range(4):
                        for mc in range(2):
                            P.pe(lambda e, h_=h_, mc=mc, q=q, ptb16=ptb16: e.transpose(
                                out=ptb16[:, (h_ * 2 + mc) * 128:(h_ * 2 + mc + 1) * 128],
                                in_=pn[q][:, h_, mc * 128:(mc + 1) * 128], identity=ident_bf[:]), [b_pn[q], b_c], [pb])
                    CPo("act", pT[q][:], ptb16.rearrange("p (a t) -> p a t", a=8), [pb], [b_pT[q]])
                    pso = [PS(), PS()]
                    for h_ in range(4):
                        for dc in range(2):
                            idx = 2 * h_ + dc
                            pt, pb = pso[idx // 4]
                            for mc in range(2):
                                MM(pt[:, (idx % 4) * 128:(idx % 4 + 1) * 128],
                                   VM[:, l, mc, h_ * 256 + dc * 128:h_ * 256 + (dc + 1) * 128], pT[q][:, h_ * 2 + mc, :],
                                   mc == 0, mc == 1, [b_kv, b_pT[q]], [pb])
                    for bk in range(2):
                        pt, pb = pso[bk]
                        CPo("dve", xoT[q][:, 4 * bk:4 * bk + 4, :], pt[:].rearrange("p (a t) -> p a t", a=4), [pb], [b_xo[q]])
                    pA, pAb = PS()
                    pB, pBb = PS()
                    for kc in range(KC):
                        MM(pA[:], xoT[q][:, kc, :], wo0[:, kc, :], kc == 0, kc == KC - 1, [b_xo[q], wo0b], [pAb])
                    for kc in range(KC):
                        MM(pB[:], xoT[q][:, kc, :], wo1[:, kc, :], kc == 0, kc == KC - 1, [b_xo[q], wo1b], [pBb])
                    postnorm(pA, pAb, pB, pBb, gp, gp_b, j, pws)
                release(2)
                P.barrier()

        def mlp(l, nt):
            with ExitStack() as ws:
                g, g_b = gload("g_ff_pre", l)
                gp, gp_b = gload("g_ff_post", l)
                uT = sb("uT", [128, KC, TB], BF16, ws)
                uT_b = P.bufs(nt, "uT")
                prenorm(lambda j: h[:, j, :], h_b, nt, g, g_b, uT, uT_b, ws, "f")
                hid = sb("hid", [128, 16, TB], BF16, ws)
                acc = sb("acc", [128, NTB, D], F32, ws)
                b_acc = P.bufs(nt, "acc")
                rl = [sb(f"rl{i}", [128, 512], BF16, ws) for i in range(2)]
                b_rl = P.bufs(2, "rl")
                cnt = [0]
                pws = mk_post_ws(ws, "f")
                b_hid = P.bufs(nt, "hid")
                for half in range(2):
                    for pc in range(4):
                        wv, wb = acquire(f"ff1_{l}")
                        for c in range(4):
                            ch = pc * 4 + c

                            def ev(pt, pb, j0, gn, ch=ch, b_hid=b_hid):
                                k = cnt[0] % 2
                                cnt[0] += 1
                                ACTF(rl[k][:, 0:gn * 128], pt[:, 0:gn * 128], AF.Relu, [pb], [b_rl[k]])
                                TTo("pool", hid[:, ch, j0 * 128:(j0 + gn) * 128], rl[k][:, 0:gn * 128], rl[k][:, 0:gn * 128],
                                    ALU.mult, [b_rl[k]], [b_hid[j] for j in range(j0, j0 + gn)])
                            proj_fm(uT, uT_b, 0, nt, wv, wb, c * 128, ev)
                        release()
                    w2 = [acquire(f"ff2_{l}") for _ in range(4)]
                    for j in range(nt):
                        pA, pAb = PS()
                        pB, pBb = PS()
                        for (pp, ppb, c0) in ((pA, pAb, 0), (pB, pBb, 512)):
                            for k in range(16):
                                wv, wb = w2[k // 4]
                                MM(pp[:], hid[:, k, j * 128:(j + 1) * 128], wv[:, k % 4, c0:c0 + 512], k == 0, k == 15,
                                   [b_hid[j], wb], [ppb])
                        if half == 0:
                            CPo("act", acc[:, j, 0:512], pA[:], [pAb], [b_acc[j]])
                            CPo("dve", acc[:, j, 512:1024], pB[:], [pBb], [b_acc[j]])
                        else:
                            TTo("dve", acc[:, j, 0:512], acc[:, j, 0:512], pA[:], ALU.add, [pAb, b_acc[j]], [b_acc[j]])
                            TTo("dve", acc[:, j, 512:1024], acc[:, j, 512:1024], pB[:], ALU.add, [pBb, b_acc[j]], [b_acc[j]])
                            postnorm(acc[:, j, 0:512], b_acc[j], acc[:, j, 512:1024], b_acc[j], gp, gp_b, j, pws)
                    release(4)
                P.barrier()

        def swa(nt, mbase):
            SC = 64 ** -0.5
            PIB = 3.1415925
            with ExitStack() as ws:
                g, g_b = gload("g_mix_pre", 1)
                gp, gp_b = gload("g_mix_post", 1)
                uT = sb("uT", [128, KC, TB], BF16, ws)
                uT_b = P.bufs(nt, "uT")
                prenorm(lambda j: h[:, j, :], h_b, nt, g, g_b, uT, uT_b, ws, "s")
                wq0, wq0b = acquire("qkv")
                wq1, wq1b = acquire("qkv")
                wkv, wkvb = acquire("qkv")
                wo0, wo0b = acquire("o1")
                wo1, wo1b = acquire("o1")
                ang = sb("ang", [128, NTB, 32], F32, ws)
                tmd = sb("tmd", [128, NTB, 32], F32, ws)
                cosv = sb("cosv", [128, NTB, 32], F32, ws)
                sinv = sb("sinv", [128, NTB, 32], F32, ws)
                b_rp = P.buf("rope")
                A_ = ang[:, 0:nt, :]
                TTo("dve", A_, posf[:, mbase:mbase + nt].unsqueeze(2).to_broadcast([128, nt, 32]),
                    invf[:].unsqueeze(1).to_broadcast([128, nt, 32]), ALU.mult, [b_c], [b_rp])
                kf = sb("kf", [128, NTB, 32], F32, ws)

                def red_sin(dst, shift):
                    T_ = tmd[:, 0:nt, :]
                    K_ = kf[:, 0:nt, :]
                    TSo("dve", T_, A_, shift, None, ALU.add, None, [b_rp], [b_rp])
                    TSo("dve", K_, T_, 1.0 / (2 * PI), None, ALU.mult, None, [b_rp], [b_rp])
                    TSo("dve", K_, K_, 12582912.0, None, ALU.add, None, [b_rp], [b_rp])
                    TSo("dve", K_, K_, 12582912.0, None, ALU.subtract, None, [b_rp], [b_rp])
                    STTo(T_, K_, -2 * PI, T_, ALU.mult, ALU.add, [b_rp], [b_rp])
                    TSo("dve", K_, T_, PI, 2 * PI, ALU.is_gt, ALU.mult, [b_rp], [b_rp])
                    TTo("dve", T_, T_, K_, ALU.subtract, [b_rp], [b_rp])
                    TSo("dve", K_, T_, -PI, 2 * PI, ALU.is_lt, ALU.mult, [b_rp], [b_rp])
                    TTo("dve", T_, T_, K_, ALU.add, [b_rp], [b_rp])
                    TSo("dve", T_, T_, PIB, -PIB, ALU.min, ALU.max, [b_rp], [b_rp])
                    ACTF(dst, T_, AF.Sin, [b_rp], [b_rp])

                red_sin(sinv[:, 0:nt, :], 0.0)
                red_sin(cosv[:, 0:nt, :], PI / 2)
                qkf = [sb(f"qkf{i}", [128, 20, 64], F32, ws) for i in range(2)]
                rt = [sb(f"rt{i}", [128, 20, 32], F32, ws) for i in range(4)]
                qkr = [sb(f"qkr{i}", [128, 24, 64], BF16, ws) for i in range(2)]
                qTl = [sb(f"qTl{i}", [128, 8, 128], BF16, ws) for i in range(2)]
                sf = [sb(f"sf{i}", [128, 4, 256], F32, ws) for i in range(2)]
                pn = [sb(f"spn{i}", [128, 4, 256], BF16, ws) for i in range(2)]
                pT = [sb(f"spT{i}", [128, 8, 128], BF16, ws) for i in range(2)]
                obf = [sb(f"obf{i}", [128, D], BF16, ws) for i in range(2)]
                oT = [sb(f"oT{i}", [128, 8, 128], BF16, ws) for i in range(2)]
                sm = sb("ssm", [128, 2, 20], F32, ws)
                b_qkf, b_qkr, b_qTl, b_sf, b_pn, b_pT, b_obf, b_oT, b_sm = [P.bufs(2, "sw") for _ in range(9)]
                b_rt = P.bufs(4, "rt")
                pws = mk_post_ws(ws, "s")
                gi = [0]
                for j in range(nt):
                    m = mbase + j
                    q = j % 2
                    cur, prv = m % 2, (m - 1) % 2
                    pq0, pq0b = proj_tm(uT, uT_b, j, wq0, wq0b, 0, 512)
                    pq1, pq1b = proj_tm(uT, uT_b, j, wq1, wq1b, 0, 512)
                    pkv, pkvb = proj_tm(uT, uT_b, j, wkv, wkvb, 0, 512)
                    CPo("act", qkf[q][:, 0:8, :], pq0[:].rearrange("p (a d) -> p a d", d=64), [pq0b], [b_qkf[q]])
                    CPo("act", qkf[q][:, 8:16, :], pq1[:].rearrange("p (a d) -> p a d", d=64), [pq1b], [b_qkf[q]])
                    CPo("dve", qkf[q][:, 16:20, :], pkv[:, 0:256].rearrange("p (a d) -> p a d", d=64), [pkvb], [b_qkf[q]])
                    CPo("dve", vs[:, cur, :], pkv[:, 256:512], [pkvb], [kv_b[cur]])
                    t1 = qkf[q][:, :, 0:32]
                    t2 = qkf[q][:, :, 32:64]
                    cbb = cosv[:, j, :].unsqueeze(1).to_broadcast([128, 20, 32])
                    sbb = sinv[:, j, :].unsqueeze(1).to_broadcast([128, 20, 32])
                    TTo("dve", rt[0][:], t1, cbb, ALU.mult, [b_qkf[q], b_rp], [b_rt[0]])
                    TTo("dve", rt[1][:], t2, sbb, ALU.mult, [b_qkf[q], b_rp], [b_rt[1]])
                    TTo("pool", rt[2][:], t2, cbb, ALU.mult, [b_qkf[q], b_rp], [b_rt[2]])
                    TTo("pool", rt[3][:], t1, sbb, ALU.mult, [b_qkf[q], b_rp], [b_rt[3]])
                    TTo("dve", qkr[q][:, 0:20, 0:32], rt[0][:], rt[1][:], ALU.subtract, [b_rt[0], b_rt[1]], [b_qkr[q]])
                    TTo("pool", qkr[q][:, 0:20, 32:64], rt[2][:], rt[3][:], ALU.add, [b_rt[2], b_rt[3]], [b_qkr[q]])
                    for dst, src in ((20, 17), (21, 16), (22, 19), (23, 18)):
                        CPo("pool", qkr[q][:, dst, :], qkr[q][:, src, :], [b_qkr[q]], [b_qkr[q]])
                    pt, pb = PS()
                    ptb16 = pt[:].bitcast(BF16)
                    for p_ in range(8):
                        P.pe(lambda e, p_=p_, q=q, ptb16=ptb16: e.transpose(
                            out=ptb16[:, p_ * 128:(p_ + 1) * 128],
                            in_=qkr[q][:, 2 * p_:2 * p_ + 2, :].rearrange("p a d -> p (a d)"), identity=ident_bf[:]),
                            [b_qkr[q], b_c], [pb])
                    CPo("act", qTl[q][:], ptb16.rearrange("p (a t) -> p a t", a=8), [pb], [b_qTl[q]])
                    pt, pb = PS()
                    ptb16 = pt[:].bitcast(BF16)
                    for i_ in range(4):
                        P.pe(lambda e, i_=i_, q=q, ptb16=ptb16: e.transpose(
                            out=ptb16[:, i_ * 128:(i_ + 1) * 128],
                            in_=qkr[q][:, 16 + 2 * i_:18 + 2 * i_, :].rearrange("p a d -> p (a d)"), identity=ident_bf[:]),
                            [b_qkr[q], b_c], [pb])
                    for i_, (ga, gb) in enumerate(((0, 1), (2, 3), (1, 0), (3, 2))):
                        CPo("dve", kTs[0:64, cur, ga * 2, :], ptb16[0:64, i_ * 128:(i_ + 1) * 128], [pb], [kv_b[cur]])
                        CPo("act", kTs[64:128, cur, gb * 2 + 1, :], ptb16[64:128, i_ * 128:(i_ + 1) * 128], [pb], [kv_b[cur]])
                    mask = mask_fc if m == 1 else mask_pc
                    pso = [(ps_t[6], ps_b[6]), (ps_t[7], ps_b[7])]
                    SWL = int(os.environ.get("MK_SWA", "9"))
                    if SWL < 2:
                        continue
                    for gq in range(4):
                        w_ = gi[0] % 2
                        gi[0] += 1
                        psc = [PS(), PS()]
                        for hh in range(4):
                            h_ = 4 * gq + hh
                            p_ = h_ // 2
                            o_ = (h_ % 2) * 64
                            var = gq * 2 + (1 if o_ else 0)
                            pt, pb = psc[hh // 2]
                            base = (hh % 2) * 256
                            MM(pt[:, base:base + 128], qTl[q][:, p_, :], kTs[:, prv, var, :], True, True,
                               [b_qTl[q], kv_b[prv]], [pb])
                            MM(pt[:, base + 128:base + 256], qTl[q][:, p_, :], kTs[:, cur, var, :], True, True,
                               [b_qTl[q], kv_b[cur]], [pb])
                        for bk in range(2):
                            pt, pb = psc[bk]
                            TTo("dve", sf[w_][:, 2 * bk:2 * bk + 2, :], pt[:].rearrange("p (a c) -> p a c", a=2),
                                mask[:].unsqueeze(1).to_broadcast([128, 2, 256]), ALU.add, [pb, b_c], [b_sf[w_]])
                        if SWL < 3:
                            continue
                        mx = sm[:, w_, 0:4]
                        ngm = sm[:, w_, 4:8]
                        rs = sm[:, w_, 8:12]
                        t4 = sm[:, w_, 12:16]
                        rrs = sm[:, w_, 16:20]
                        sk = sinks[:, 4 * gq:4 * gq + 4]
                        P.dve(lambda e, w_=w_, mx=mx: e.tensor_reduce(out=mx, in_=sf[w_][:], axis=AX.X, op=ALU.max),
                              [b_sf[w_]], [b_sm[w_]])
                        STTo(mx, mx, SC, sk, ALU.mult, ALU.max, [b_sm[w_], b_c], [b_sm[w_]])
                        TSo("dve", ngm, mx, -1.0, None, ALU.mult, None, [b_sm[w_]], [b_sm[w_]])
                        for hh in range(4):
                            ACTF(sf[w_][:, hh, :], sf[w_][:, hh, :], AF.Exp, [b_sf[w_], b_sm[w_]], [b_sf[w_], b_sm[w_]],
                                 scale=SC, bias=ngm[:, hh:hh + 1], accum_out=rs[:, hh:hh + 1])
                        TTo("dve", t4, sk, ngm, ALU.add, [b_sm[w_], b_c], [b_sm[w_]])
                        ACTF(t4, t4, AF.Exp, [b_sm[w_]], [b_sm[w_]])
                        TTo("dve", rs, rs, t4, ALU.add, [b_sm[w_]], [b_sm[w_]])
                        P.dve(lambda e, rs=rs, rrs=rrs: e.reciprocal(out=rrs, in_=rs), [b_sm[w_]], [b_sm[w_]])
                        TTo("pool", pn[w_][:], sf[w_][:], rrs.unsqueeze(2).to_broadcast([128, 4, 256]), ALU.mult,
                            [b_sf[w_], b_sm[w_]], [b_pn[w_]])
                        pt, pb = PS()
                        ptb16 = pt[:].bitcast(BF16)
                        for hh in range(4):
                            for kc2 in range(2):
                                P.pe(lambda e, hh=hh, kc2=kc2, w_=w_, ptb16=ptb16: e.transpose(
                                    out=ptb16[:, (hh * 2 + kc2) * 128:(hh * 2 + kc2 + 1) * 128],
                                    in_=pn[w_][:, hh, kc2 * 128:(kc2 + 1) * 128], identity=ident_bf[:]), [b_pn[w_], b_c], [pb])
                        CPo("act", pT[w_][:], ptb16.rearrange("p (a t) -> p a t", a=8), [pb], [b_pT[w_]])
                        for hh in range(4):
                            h_ = 4 * gq + hh
                            po, pob = pso[h_ // 8]
                            col = (h_ % 8) * 64
                            MM(po[:, col:col + 64], pT[w_][:, hh * 2, :], vs[:, prv, gq * 64:(gq + 1) * 64], True, False,
                               [b_pT[w_], kv_b[prv]], [pob])
                            MM(po[:, col:col + 64], pT[w_][:, hh * 2 + 1, :], vs[:, cur, gq * 64:(gq + 1) * 64], False, True,
                               [b_pT[w_], kv_b[cur]], [pob])
                    if SWL < 4:
                        continue
                    CPo("act", obf[q][:, 0:512], pso[0][0][:], [pso[0][1]], [b_obf[q]])
                    CPo("dve", obf[q][:, 512:1024], pso[1][0][:], [pso[1][1]], [b_obf[q]])
                    pt, pb = PS()
                    ptb16 = pt[:].bitcast(BF16)
                    for kc in range(KC):
                        P.pe(lambda e, kc=kc, q=q, ptb16=ptb16: e.transpose(out=ptb16[:, kc * 128:(kc + 1) * 128],
                                                                           in_=obf[q][:, kc * 128:(kc + 1) * 128],
                                                                           identity=ident_bf[:]), [b_obf[q], b_c], [pb])
                    CPo("act", oT[q][:], ptb16.rearrange("p (a t) -> p a t", a=8), [pb], [b_oT[q]])
                    pA, pAb = PS()
                    pB, pBb = PS()
                    for kc in range(KC):
                        MM(pA[:], oT[q][:, kc, :], wo0[:, kc, :], kc == 0, kc == KC - 1, [b_oT[q], wo0b], [pAb])
                    for kc in range(KC):
                        MM(pB[:], oT[q][:, kc, :], wo1[:, kc, :], kc == 0, kc == KC - 1, [b_oT[q], wo1b], [pBb])
                    postnorm(pA, pAb, pB, pBb, gp, gp_b, j, pws)
                release(5)
                P.barrier()

        def load_x(loc0, nt):
            for j in range(nt):
                DMA("pool", h[:, j, :], xloc[(loc0 + j) * 128:(loc0 + j + 1) * 128, :], [], [h_b[j]])

        STOP = int(os.environ.get("MK_STOP", "99"))
        pcs = {}
        for nm in in_names:
            pcs[nm] = acquire(nm)
        ncast_grp = -(-len(cast_q) // max(1, len(tok_groups(0, NPRE))))
        for (j0, n) in tok_groups(0, NPRE):
            if STOP < 1:
                break
            load_x(j0, n)
            cast_some(ncast_grp)
            layer0_mix(n, [(0, n)], True, pcs)
        out_ops = []
        mbase = 0
        for bi, nt in enumerate(BLOCKS):
            if STOP < 2:
                break
            load_x(NPRE + mbase, nt)
            if bi > 0:
                pcs = {nm: acquire(nm) for nm in in_names}
            if mbase == 0 and nt > 1:
                layer0_mix(nt, [(0, 1), (1, nt - 1)], False, pcs, reset_before=1)
            elif mbase == 1:
                layer0_mix(nt, [(0, nt)], False, pcs, reset_before=0)
            else:
                layer0_mix(nt, [(0, nt)], False, pcs)
            dbg_dump(0, mbase, nt)
            if STOP >= 3:
                xattn(0, nt)
            dbg_dump(1, mbase, nt)
            if STOP >= 4:
                mlp(0, nt)
            dbg_dump(2, mbase, nt)
            if STOP >= 5:
                swa(nt, mbase)
            dbg_dump(3, mbase, nt)
            if STOP >= 6:
                xattn(1, nt)
            dbg_dump(4, mbase, nt)
            if STOP >= 7:
                mlp(1, nt)
            dbg_dump(5, mbase, nt)
            for j in range(nt):
                m = mbase + j
                if m >= 1:
                    out_ops.append(DMA("pool", outd[(m - 1) * 128:m * 128, :], h[:, j, :], [h_b[j]], []))
            mbase += nt
        P.emit(out_ops)
        nc._prog_stats = P.stats
    return nc


def _host_consts():
    r = np.arange(128)[:, None]
    c = np.arange(128)[None, :]
    mprev = np.where(c > r, 0.0, NEG).astype(np.float32)
    inv = np.power(10000.0, -np.arange(32, dtype=np.float32) * 2.0 / 64).astype(np.float32)
    invf = np.broadcast_to(inv[None, :], (128, 32)).copy()
    return mprev, invf


def run_module(inputs, NOWN, NPRE, BLOCKS, dbg=False):
    x = np.asarray(inputs["x"], dtype=np.float32)
    B = x.shape[0]
    ncores = 2 * B
    S = x.shape[1]
    assert S == 2 * NOWN * 128 and NPRE == NOWN - 1 and sum(BLOCKS) == NOWN + 1
    mprev, invf = _host_consts()
    nc = build_nc(NPRE, BLOCKS, dbg=dbg)
    in_maps = []
    NLOC = NPRE + 1 + NOWN
    pos = np.asarray(inputs["positions"]).astype(np.int32)
    for c in range(ncores):
        b, s = c // 2, c % 2
        xl = np.zeros((NLOC * 128, D), np.float32)
        pl = np.zeros((NOWN + 1, 128), np.int32)
        if s == 1:
            xl[:] = x[b]
            pl[:] = pos[b].reshape(2 * NOWN, 128)[NOWN - 1:]
        else:
            xl[(NPRE + 1) * 128:] = x[b, :NOWN * 128]
            pl[1:] = pos[b].reshape(2 * NOWN, 128)[:NOWN]
        m = {"xloc": xl, "mem": np.ascontiguousarray(inputs["mem"][b], dtype=np.float32), "pos": pl,
             "flag": np.full((128, 1), float(s), np.float32),
             "mask_first": mprev if s == 1 else np.full((128, 128), NEG, np.float32),
             "invf": invf}
        for n_ in W_NAMES:
            m[n_] = np.ascontiguousarray(inputs[n_], dtype=np.float32)
        in_maps.append(m)
    res = run_bass_kernel_spmd(nc, in_maps, core_ids=list(range(ncores)))
    if os.environ.get("MK_VERBOSE"):
        print("exec_time_ns", getattr(res, "exec_time_ns", None), flush=True)
    out = np.zeros((B, S, D), np.float32)
    dbgs = None
    for c in range(ncores):
        b, s = c // 2, c % 2
        out[b, s * NOWN * 128:(s + 1) * NOWN * 128] = res.results[c]["out"]
    if dbg:
        dbgs = [res.results[c]["dbg"] for c in range(ncores)]
    return out, dbgs, nc


def kernel(**inputs):
    out, _, _ = run_module(inputs, 32, 31, [1, 4, 4, 4, 4, 4, 4, 4, 4])
    return out
```

```python
import math
import os
from contextlib import ExitStack

import numpy as np
import concourse.bass as bass
import concourse.mybir as mybir
from concourse.bass_utils import run_bass_kernel_spmd

F32 = mybir.dt.float32
BF16 = mybir.dt.bfloat16
I32 = mybir.dt.int32
AF = mybir.ActivationFunctionType
ALU = mybir.AluOpType
AX = mybir.AxisListType


class Buf:
    __slots__ = ("name", "w", "r")

    def __init__(self, name):
        self.name = name
        self.w = None
        self.r = []


class Op:
    __slots__ = ("eng", "fn", "deps", "is_dma", "sem", "cnt", "has_dep", "idx", "pre")

    def __init__(self, eng, fn, is_dma):
        self.eng = eng
        self.fn = fn
        self.deps = []
        self.is_dma = is_dma
        self.sem = None
        self.cnt = None
        self.has_dep = False
        self.pre = None


class Prog:
    ENGS = ("pe", "act", "dve", "pool", "sp")
    NDMA = {"sp": 12, "pool": 12, "act": 4}

    def __init__(self, nc):
        self.nc = nc
        self.streams = {e: [] for e in self.ENGS}
        self.nbuf = 0

    def buf(self, name=None):
        self.nbuf += 1
        return Buf(name or f"b{self.nbuf}")

    def bufs(self, n, name="b"):
        return [self.buf(f"{name}{i}") for i in range(n)]

    def _add(self, o, reads, writes):
        deps = []
        for b in reads:
            if b.w is not None:
                deps.append(("raw", b.w))
            b.r.append(o)
        for b in writes:
            if b.w is not None:
                deps.append(("waw", b.w))
            for r in b.r:
                if r is not o:
                    deps.append(("war", r))
            b.r = []
            b.w = o
        seen = set()
        for kind, d in deps:
            if d is o or id(d) in seen:
                continue
            if (not d.is_dma) and d.eng == o.eng and not o.is_dma:
                if kind != "raw" or o.eng == "pe":
                    continue
            seen.add(id(d))
            o.deps.append(d)
            d.has_dep = True
        self.streams[o.eng].append(o)
        return o

    def op(self, eng, fn, reads=(), writes=()):
        return self._add(Op(eng, fn, False), list(reads), list(writes))

    def dma(self, eng, fn, reads=(), writes=()):
        return self._add(Op(eng, fn, True), list(reads), list(writes))

    def pe(self, fn, reads=(), writes=()):
        return self.op("pe", fn, reads, writes)

    def act(self, fn, reads=(), writes=()):
        return self.op("act", fn, reads, writes)

    def dve(self, fn, reads=(), writes=()):
        return self.op("dve", fn, reads, writes)

    def pool(self, fn, reads=(), writes=()):
        return self.op("pool", fn, reads, writes)

    def barrier(self):
        lasts = []
        for e in ("pe", "act", "dve", "pool"):
            for o in reversed(self.streams[e]):
                if not o.is_dma and o.fn is not None:
                    lasts.append(o)
                    break
        for e in ("pe", "act", "dve", "pool"):
            w = Op(e, None, False)
            for d in lasts:
                w.deps.append(d)
                d.has_dep = True
            self.streams[e].append(w)

    def emit(self, final_deps=()):
        nc = self.nc
        es = ExitStack()
        with es:
            csem = {e: es.enter_context(nc.semaphore(f"c_{e}")) for e in ("pe", "act", "dve", "pool")}
            dsem = {q: [es.enter_context(nc.semaphore(f"d_{q}{i}")) for i in range(n)]
                    for q, n in self.NDMA.items()}
            fin = Op("pool", None, False)
            for d in final_deps:
                fin.deps.append(d)
                d.has_dep = True
            for e in self.ENGS:
                if e != "pool":
                    for last in reversed(self.streams[e]):
                        if last.fn is not None:
                            fin.deps.append(last)
                            last.has_dep = True
                            break
            self.streams["pool"].append(fin)
            ccount = {e: 0 for e in csem}
            dcount = {q: 0 for q in dsem}
            for e in self.ENGS:
                for o in self.streams[e]:
                    if o.is_dma:
                        j = dcount[e]
                        dcount[e] += 1
                        k = len(dsem[e])
                        o.sem = dsem[e][j % k]
                        o.cnt = 16 * (j // k + 1)
                        o.pre = (o.sem, 16 * (j // k)) if j >= k else None
                    elif o.has_dep:
                        ccount[e] += 1
                        o.sem = csem[e]
                        o.cnt = ccount[e]
            self.stats = {"ccount": ccount, "dcount": dcount,
                          "n": {e: len(self.streams[e]) for e in self.ENGS}}
            engobj = {"pe": "tensor", "act": "scalar", "dve": "vector", "pool": "gpsimd", "sp": "sync"}

            def run_stream(e, eng):
                waited = {}
                nwait = 0
                for o in self.streams[e]:
                    need = {}
                    if o.pre is not None:
                        need[o.pre[0]] = o.pre[1]
                    for d in o.deps:
                        assert d.sem is not None, "dep without semaphore"
                        if need.get(d.sem, 0) < d.cnt:
                            need[d.sem] = d.cnt
                    for s, c in need.items():
                        if waited.get(s, 0) < c:
                            eng.wait_ge(s, c)
                            waited[s] = c
                            nwait += 1
                    if o.fn is None:
                        continue
                    ins = o.fn(eng)
                    if o.sem is not None:
                        ins.then_inc(o.sem, 16 if o.is_dma else 1)
                self.stats.setdefault("nwait", {})[e] = nwait

            with nc.Block() as block:
                for e in self.ENGS:
                    if not self.streams[e]:
                        continue

                    def f(eng, e=e):
                        run_stream(e, eng)

                    getattr(block, engobj[e])(f)


D = 1024
KC = 8
EPS = 1e-6
NEG = -1.0e9
PI = math.pi

W_NAMES = ["ev_w_in", "ev_b_if", "ev_conv_w", "ev_conv_b", "ev_rg_wa", "ev_rg_ba", "ev_rg_wx", "ev_rg_bx",
           "ev_rg_lambda", "ev_w_out", "od_w_qkv", "od_sinks", "od_w_o", "g_mix_pre", "g_mix_post",
           "g_x_pre", "g_x_post", "g_mem", "w_xq", "w_xkv", "w_xo", "g_ff_pre", "g_ff_post", "w_ff1", "w_ff2"]
W_SHAPES = {"ev_w_in": [1, 1024, 3080], "ev_b_if": [1, 8], "ev_conv_w": [1, 4, 512], "ev_conv_b": [1, 512],
            "ev_rg_wa": [1, 4, 128, 128], "ev_rg_ba": [1, 512], "ev_rg_wx": [1, 4, 128, 128], "ev_rg_bx": [1, 512],
            "ev_rg_lambda": [1, 512], "ev_w_out": [1, 1024, 1024], "od_w_qkv": [1, 1024, 1536],
            "od_sinks": [1, 16], "od_w_o": [1, 1024, 1024], "g_mix_pre": [2, 1024], "g_mix_post": [2, 1024],
            "g_x_pre": [2, 1024], "g_x_post": [2, 1024], "g_mem": [2, 1024], "w_xq": [2, 1024, 1024],
            "w_xkv": [2, 1024, 2048], "w_xo": [2, 1024, 1024], "g_ff_pre": [2, 1024], "g_ff_post": [2, 1024],
            "w_ff1": [2, 1024, 4096], "w_ff2": [2, 4096, 1024]}


def build_nc(NPRE, BLOCKS, dbg=False):
    nc = bass.Bass("TRN2", target_bir_lowering=False)
    NMAIN = sum(BLOCKS)
    NOWN = NMAIN - 1
    NLOC = NPRE + NMAIN
    NTB = max(BLOCKS)
    TB = NTB * 128

    def din(name, shape, dt=F32):
        return nc.dram_tensor(name, list(shape), dt, kind="ExternalInput").ap()

    xloc = din("xloc", [NLOC * 128, D])
    mem = din("mem", [256, D])
    posd = din("pos", [NMAIN, 128], I32)
    flagd = din("flag", [128, 1])
    maskfd = din("mask_first", [128, 128])
    invfd = din("invf", [128, 32])
    Wd = {n: din(n, W_SHAPES[n]) for n in W_NAMES}
    outd = nc.dram_tensor("out", [NOWN * 128, D], F32, kind="ExternalOutput").ap()
    dbgd = nc.dram_tensor("dbg", [6, NMAIN * 128, D], F32, kind="ExternalOutput").ap() if dbg else None

    P = Prog(nc)
    es = ExitStack()
    with es:
        ncd = es.enter_context(nc.allow_non_contiguous_dma(reason="small parameter loads"))

        sbn = [0]

        def sb(name, shape, dt, st=es):
            sbn[0] += 1
            return st.enter_context(nc.sbuf_tensor(f"{name}_{sbn[0]}", list(shape), dt))

        def MM(out, lhsT, rhs, start, stop, r, w):
            return P.pe(lambda e: e.matmul(out, lhsT=lhsT, rhs=rhs, start=start, stop=stop), r, w)

        def ACTF(out, in_, func, r, w, **kw):
            return P.act(lambda e: e.activation(out=out, in_=in_, func=func, **kw), r, w)

        def TTo(eng, out, in0, in1, op, r, w):
            return P.op(eng, lambda e: e.tensor_tensor(out=out, in0=in0, in1=in1, op=op), r, w)

        def TSo(eng, out, in0, s1, s2, op0, op1, r, w):
            if s2 is None:
                return P.op(eng, lambda e: e.tensor_scalar(out=out, in0=in0, scalar1=s1, scalar2=None, op0=op0), r, w)
            return P.op(eng, lambda e: e.tensor_scalar(out=out, in0=in0, scalar1=s1, scalar2=s2, op0=op0, op1=op1), r, w)

        def STTo(out, in0, scalar, in1, op0, op1, r, w):
            return P.dve(lambda e: e.scalar_tensor_tensor(out=out, in0=in0, scalar=scalar, in1=in1, op0=op0, op1=op1), r, w)

        def CPo(eng, out, in_, r, w):
            if eng == "act":
                return P.act(lambda e: e.activation(out=out, in_=in_, func=AF.Copy), r, w)
            return P.op(eng, lambda e: e.tensor_copy(out=out, in_=in_), r, w)

        def MSET(eng, ap, val, w):
            return P.op(eng, lambda e: e.memset(ap, val), [], w)

        def DMA(q, out, in_, r, w):
            return P.dma(q, lambda e: e.dma_start(out=out, in_=in_), r, w)

        ps_t = [es.enter_context(nc.psum_tensor(f"ps{i}", [128, 512], F32)) for i in range(8)]
        ps_b = P.bufs(8, "ps")
        ps_i = [0]

        def PS():
            i = ps_i[0]
            ps_i[0] = (i + 1) % 6
            return ps_t[i], ps_b[i]

        b_c = P.buf("const")
        ident_f = sb("ident_f", [128, 128], F32)
        ident_bf = sb("ident_bf", [128, 128], BF16)
        ones4 = sb("ones4", [4, 128], F32)
        onesrow = sb("onesrow", [4, TB], F32)
        tri = sb("tri", [128, 128], F32)
        mask_pc = sb("mask_pc", [128, 256], F32)
        mask_fc = sb("mask_fc", [128, 256], F32)
        invf = sb("invf_sb", [128, 32], F32)
        flag = sb("flag_sb", [128, 1], F32)
        MSET("pool", ident_f[:], 0.0, [b_c])
        P.pool(lambda e: e.affine_select(out=ident_f[:], in_=ident_f[:], pattern=[[-1, 128]], compare_op=ALU.not_equal,
                                         fill=1.0, base=0, channel_multiplier=1), [b_c], [b_c])
        CPo("dve", ident_bf[:], ident_f[:], [b_c], [b_c])
        MSET("pool", ones4[:], 1.0, [b_c])
        MSET("pool", onesrow[:], 1.0, [b_c])
        MSET("pool", tri[:], 1.0, [b_c])
        P.pool(lambda e: e.affine_select(out=tri[:], in_=tri[:], pattern=[[1, 128]], compare_op=ALU.is_ge,
                                         fill=0.0, base=0, channel_multiplier=-1), [b_c], [b_c])
        MSET("pool", mask_pc[:], 0.0, [b_c])
        P.pool(lambda e: e.affine_select(out=mask_pc[:, 0:128], in_=mask_pc[:, 0:128], pattern=[[1, 128]],
                                         compare_op=ALU.is_gt, fill=NEG, base=0, channel_multiplier=-1), [b_c], [b_c])
        P.pool(lambda e: e.affine_select(out=mask_pc[:, 128:256], in_=mask_pc[:, 128:256], pattern=[[-1, 128]],
                                         compare_op=ALU.is_ge, fill=NEG, base=0, channel_multiplier=1), [b_c], [b_c])
        DMA("pool", mask_fc[:, 0:128], maskfd, [], [b_c])
        CPo("pool", mask_fc[:, 128:256], mask_pc[:, 128:256], [b_c], [b_c])
        DMA("pool", invf[:], invfd, [], [b_c])
        DMA("pool", flag[:], flagd, [], [b_c])

        bif = sb("bif", [4, 2], F32)
        DMA("pool", bif[:, 0:1], Wd["ev_b_if"][0, 0:4].rearrange("(p o) -> p o", o=1), [], [b_c])
        DMA("pool", bif[:, 1:2], Wd["ev_b_if"][0, 4:8].rearrange("(p o) -> p o", o=1), [], [b_c])
        TSo("dve", bif[:, 1:2], bif[:, 1:2], -1.0, None, ALU.mult, None, [b_c], [b_c])
        cw = sb("cw", [128, 4, 4], F32)
        for j in range(4):
            DMA("pool", cw[:, j, :], Wd["ev_conv_w"][0, j, :].rearrange("(n p) -> p n", p=128), [], [b_c])
        cb = sb("cb", [128, 4], F32)
        rba = sb("rba", [128, 4], F32)
        rbx = sb("rbx", [128, 4], F32)
        lam = sb("lam", [128, 4], F32)
        cl = sb("cl", [128, 8], F32)
        for t_, n_ in ((cb, "ev_conv_b"), (rba, "ev_rg_ba"), (rbx, "ev_rg_bx"), (lam, "ev_rg_lambda")):
            DMA("pool", t_[:], Wd[n_][0, :].rearrange("(n p) -> p n", p=128), [], [b_c])
        ACTF(lam[:], lam[:], AF.Exp, [b_c], [b_c], scale=-1.0)
        ACTF(lam[:], lam[:], AF.Ln, [b_c], [b_c], bias=1.0)
        TSo("dve", cl[:, 0:4], lam[:], -8.0, None, ALU.mult, None, [b_c], [b_c])
        TSo("dve", cl[:, 4:8], lam[:], -16.0, None, ALU.mult, None, [b_c], [b_c])
        sinks = sb("sinks", [128, 16], F32)
        DMA("pool", sinks[:], Wd["od_sinks"][0:1, :].to_broadcast([128, 16]), [], [b_c])
        posi = sb("posi", [128, NMAIN], I32)
        posf = sb("posf", [128, NMAIN], F32)
        DMA("pool", posi[:], posd.rearrange("m p -> p m"), [], [b_c])
        CPo("dve", posf[:], posi[:], [b_c], [b_c])
        wgif = sb("wgif", [128, 8, 8], BF16)
        DMA("pool", wgif[:], Wd["ev_w_in"][0, :, 2048:2056].rearrange("(k p) n -> p k n", p=128), [], [b_c])
        wrg = sb("wrg", [128, 2, 4, 128], BF16)
        DMA("pool", wrg[:, 0], Wd["ev_rg_wa"][0].rearrange("n c d -> c n d"), [], [b_c])
        DMA("pool", wrg[:, 1], Wd["ev_rg_wx"][0].rearrange("n c d -> c n d"), [], [b_c])

        pieces = {}
        cast_q = []
        cast_done = {}
        cast_pos = [0]

        def cast_some(n):
            while n > 0 and cast_pos[0] < len(cast_q):
                name, t, src3, b = cast_q[cast_pos[0]]
                cast_pos[0] += 1
                DMA("pool", t, src3, [], [b])
                cast_done[name] = True
                n -= 1

        def mk_piece(name, src3):
            kk = src3.shape[1]
            nn = src3.shape[2]
            t = nc.dram_tensor("wsc_" + name, [128, kk, nn], BF16, kind="Internal").ap()
            b = P.buf("wsc_" + name)
            cast_q.append((name, t, src3, b))
            pieces[name] = (t, b, kk, nn)

        def std_pieces(prefix, w2d, ncols):
            names = []
            for c in range(ncols // 512):
                nm = f"{prefix}_{c}"
                mk_piece(nm, w2d[:, c * 512:(c + 1) * 512].rearrange("(k p) n -> p k n", p=128))
                names.append(nm)
            return names

        def mlp_pieces(l):
            names = []
            for half in range(2):
                for c in range(4 * half, 4 * half + 4):
                    nm = f"ff1_{l}_{c}"
                    mk_piece(nm, Wd["w_ff1"][l][:, c * 512:(c + 1) * 512].rearrange("(k p) n -> p k n", p=128))
                    names.append(nm)
                for c in range(4 * half, 4 * half + 4):
                    nm = f"ff2_{l}_{c}"
                    mk_piece(nm, Wd["w_ff2"][l][c * 512:(c + 1) * 512, :].rearrange("(k p) n -> p k n", p=128))
                    names.append(nm)
            return names

        seq = []
        xkv_names = []
        for l in range(2):
            xkv_names.append(std_pieces(f"xkv{l}", Wd["w_xkv"][l], 2048))
        win = Wd["ev_w_in"][0]
        in_names = []
        for nm, c0 in (("in_q", 0), ("in_k", 512), ("in_v", 1024), ("in_o", 1536), ("in_rx", 2056), ("in_ry", 2568)):
            mk_piece(nm, win[:, c0:c0 + 512].rearrange("(k p) n -> p k n", p=128))
            in_names.append(nm)
        L0 = list(in_names)
        L0 += std_pieces("out", Wd["ev_w_out"][0], 1024)
        L0 += std_pieces("xq0", Wd["w_xq"][0], 1024)
        L0 += std_pieces("xo0", Wd["w_xo"][0], 1024)
        L0 += mlp_pieces(0)
        L1 = std_pieces("qkv", Wd["od_w_qkv"][0], 1536)
        L1 += std_pieces("o1", Wd["od_w_o"][0], 1024)
        L1 += std_pieces("xq1", Wd["w_xq"][1], 1024)
        L1 += std_pieces("xo1", Wd["w_xo"][1], 1024)
        L1 += mlp_pieces(1)
        seq = xkv_names[0] + xkv_names[1]
        for _ in BLOCKS:
            seq += L0 + L1

        NSLOT = 8
        slot_t = [sb(f"slot{i}", [128, 4096], BF16) for i in range(NSLOT)]
        slot_b = P.bufs(NSLOT, "slot")
        ring = {"use": 0, "done": 0, "loaded": 0}

        def ring_load():
            i = ring["loaded"]
            if i >= len(seq):
                return
            t, b, kk, nn = pieces[seq[i]]
            while seq[i] not in cast_done:
                cast_some(1)
            s = i % NSLOT
            DMA("sp", slot_t[s][:].rearrange("p (k n) -> p k n", k=kk), t, [b], [slot_b[s]])
            ring["loaded"] += 1

        def acquire(expect=None):
            i = ring["use"]
            ring["use"] += 1
            assert i < ring["loaded"], "ring underflow"
            if expect is not None and not os.environ.get("MK_STOP"):
                assert seq[i].startswith(expect), (seq[i], expect)
            t, b, kk, nn = pieces[seq[i]]
            s = i % NSLOT
            return slot_t[s][:].rearrange("p (k n) -> p k n", k=kk), slot_b[s]

        def release(n=1):
            for _ in range(n):
                ring["done"] += 1
                ring_load()

        for _ in range(NSLOT):
            ring_load()

        h = sb("h", [128, NTB, D], F32)
        h_b = P.bufs(NTB, "h")
        grep = [sb(f"grep{i}", [128, D], F32) for i in range(2)]
        grep_b = P.bufs(2, "grep")
        gcnt = [0]

        def gload(name, l):
            i = gcnt[0] % 2
            gcnt[0] += 1
            DMA("pool", grep[i][:], Wd[name][l:l + 1, :].to_broadcast([128, D]), [], [grep_b[i]])
            return grep[i], grep_b[i]

        KT = sb("KT", [128, 2, 8, 256], BF16)
        VM = sb("VM", [128, 2, 2, D], BF16)
        b_kv = P.buf("kvmem")
        Cst = sb("Cst", [128, 4, 129], F32)
        b_C = P.buf("C")
        gcar = sb("gcar", [4, 2], F32)
        b_gc = P.buf("gcar")
        hst = sb("hst", [128, 4], F32)
        rxhist = sb("rxhist", [128, 4, 3], F32)
        b_rgs = P.bufs(4, "rgst")
        MSET("dve", Cst[:], 0.0, [b_C])
        MSET("dve", gcar[:], 0.0, [b_gc])
        MSET("dve", hst[:], 0.0, b_rgs)
        MSET("dve", rxhist[:], 0.0, b_rgs)
        kTs = sb("kTs", [128, 2, 8, 128], BF16)
        vs = sb("vs", [128, 2, 256], BF16)
        kv_b = P.bufs(2, "swakv")
        MSET("dve", kTs[:], 0.0, [kv_b[0], kv_b[1]])
        MSET("dve", vs[:], 0.0, [kv_b[0], kv_b[1]])

        def prenorm(src_fn, src_bufs, nt, g, g_b, uT, uT_b, ws, tag):
            ssq = sb(f"pn_ssq{tag}", [128, NTB], F32, ws)
            junk = sb(f"pn_junk{tag}", [128, D], BF16, ws)
            ubf = [sb(f"pn_u{tag}{i}", [128, D], BF16, ws) for i in range(2)]
            b_ssq = P.bufs(nt, "ssq")
            b_junk = P.buf()
            b_u = P.bufs(2, "u")
            for j in range(nt):
                src = src_fn(j)
                ACTF(junk[:], src, AF.Square, [src_bufs[j]], [b_junk, b_ssq[j]], accum_out=ssq[:, j:j + 1])
                ACTF(ssq[:, j:j + 1], ssq[:, j:j + 1], AF.Sqrt, [b_ssq[j]], [b_ssq[j]], scale=1.0 / D, bias=EPS)
                P.dve(lambda e, j=j: e.reciprocal(out=ssq[:, j:j + 1], in_=ssq[:, j:j + 1]), [b_ssq[j]], [b_ssq[j]])
                u = ubf[j % 2]
                STTo(u[:], src, ssq[:, j:j + 1], g[:], ALU.mult, ALU.mult, [src_bufs[j], b_ssq[j], g_b], [b_u[j % 2]])
                pt, pb = PS()
                ptb = pt[:].bitcast(BF16)
                for kc in range(KC):
                    P.pe(lambda e, kc=kc, u=u, ptb=ptb: e.transpose(out=ptb[:, kc * 128:(kc + 1) * 128],
                                                                   in_=u[:, kc * 128:(kc + 1) * 128],
                                                                   identity=ident_bf[:]),
                         [b_u[j % 2], b_c], [pb])
                CPo("act", uT[:, :, j * 128:(j + 1) * 128], ptb.rearrange("p (k t) -> p k t", k=KC), [pb], [uT_b[j]])

        def postnorm(psA, pbA, psB, pbB, g, g_b, j, ws_t):
            ss2, tmp, junk = ws_t["ss2"], ws_t["tmp"], ws_t["junk"]
            k = ws_t["cnt"] % 2
            ws_t["cnt"] += 1
            b_ss, b_tmp, b_jk = ws_t["b_ss"][k], ws_t["b_tmp"][k], ws_t["b_jk"]
            s2 = ss2[:, 4 * k:4 * k + 4]
            ACTF(junk[:, 0:512], psA[:], AF.Square, [pbA], [b_jk, b_ss], accum_out=s2[:, 0:1])
            ACTF(junk[:, 512:1024], psB[:], AF.Square, [pbB], [b_jk, b_ss], accum_out=s2[:, 1:2])
            TTo("dve", s2[:, 2:3], s2[:, 0:1], s2[:, 1:2], ALU.add, [b_ss], [b_ss])
            ACTF(s2[:, 2:3], s2[:, 2:3], AF.Sqrt, [b_ss], [b_ss], scale=1.0 / D, bias=EPS)
            P.dve(lambda e: e.reciprocal(out=s2[:, 3:4], in_=s2[:, 2:3]), [b_ss], [b_ss])
            tm = tmp[k]
            STTo(tm[:, 0:512], psA[:], s2[:, 3:4], g[:, 0:512], ALU.mult, ALU.mult, [pbA, b_ss, g_b], [b_tmp])
            STTo(tm[:, 512:1024], psB[:], s2[:, 3:4], g[:, 512:1024], ALU.mult, ALU.mult, [pbB, b_ss, g_b], [b_tmp])
            TTo("pool", h[:, j, :], h[:, j, :], tm[:], ALU.add, [h_b[j], b_tmp], [h_b[j]])

        def mk_post_ws(ws, tag):
            return {"ss2": sb(f"po_ss{tag}", [128, 8], F32, ws), "junk": sb(f"po_junk{tag}", [128, D], BF16, ws),
                    "tmp": [sb(f"po_tmp{tag}0", [128, D], F32, ws)] * 2,
                    "cnt": 0, "b_ss": P.bufs(2, "poss"), "b_tmp": [P.buf("potmp")] * 2, "b_jk": P.buf()}

        def proj_tm(uT, uT_b, j, wv, wb, c0, ncol):
            pt, pb = PS()
            for kc in range(KC):
                MM(pt[:, 0:ncol], uT[:, kc, j * 128:(j + 1) * 128], wv[:, kc, c0:c0 + ncol], kc == 0, kc == KC - 1,
                   [uT_b[j], wb], [pb])
            return pt, pb

        def tok_groups(t0, nt):
            out = []
            j = t0
            while j < t0 + nt:
                n = min(4, t0 + nt - j)
                out.append((j, n))
                j += n
            return out

        def proj_fm(uT, uT_b, t0, nt, wv, wb, c0, evac):
            for (j0, n) in tok_groups(t0, nt):
                pt, pb = PS()
                for kc in range(KC):
                    MM(pt[:, 0:n * 128], wv[:, kc, c0:c0 + 128], uT[:, kc, j0 * 128:(j0 + n) * 128], kc == 0, kc == KC - 1,
                       [uT_b[j] for j in range(j0, j0 + n)] + [wb], [pb])
                evac(pt, pb, j0, n)

        def interleave(gens):
            gens = list(gens)
            while gens:
                for g_ in list(gens):
                    try:
                        next(g_)
                    except StopIteration:
                        gens.remove(g_)

        def dbg_dump(k, mbase, nt):
            if dbgd is None:
                return
            for j in range(nt):
                m = mbase + j
                DMA("pool", dbgd[k, m * 128:(m + 1) * 128, :], h[:, j, :], [h_b[j]], [])

        with ExitStack() as ws:
            memsb = sb("memsb", [128, 2, D], F32, ws)
            b_mem = P.bufs(2, "mem")
            for j in range(2):
                DMA("pool", memsb[:, j, :], mem[j * 128:(j + 1) * 128, :], [], [b_mem[j]])
            memT = sb("memT", [128, KC, 256], BF16, ws)
            b_memT = P.bufs(2, "memT")
            for l in range(2):
                g, g_b = gload("g_mem", l)
                prenorm(lambda j: memsb[:, j, :], b_mem, 2, g, g_b, memT, b_memT, ws, f"m{l}")
                for pc in range(2):
                    wv, wb = acquire(f"xkv{l}")
                    for c in range(4):
                        pt, pb = PS()
                        for kc in range(KC):
                            MM(pt[:, 0:256], wv[:, kc, c * 128:(c + 1) * 128], memT[:, kc, :], kc == 0, kc == KC - 1,
                               b_memT + [wb], [pb])
                        CPo("act", KT[:, l, pc * 4 + c, :], pt[:, 0:256], [pb], [b_kv])
                    release()
                for pc in range(2):
                    wv, wb = acquire(f"xkv{l}")
                    for j in range(2):
                        pt, pb = proj_tm(memT, b_memT, j, wv, wb, 0, 512)
                        CPo("dve", VM[:, l, j, pc * 512:(pc + 1) * 512], pt[:], [pb], [b_kv])
                    release()
            P.barrier()

        def layer0_mix(nt, ranges, state_only, pcs, reset_before=None):
            with ExitStack() as ws:
                g, g_b = gload("g_mix_pre", 0)
                if not state_only:
                    gp, gp_b = gload("g_mix_post", 0)
                uT = sb("uT", [128, KC, TB], BF16, ws)
                uT_b = P.bufs(nt, "uT")
                prenorm(lambda j: h[:, j, :], h_b, nt, g, g_b, uT, uT_b, ws, "a")
                g1 = sb("g1", [4, TB], F32, ws)
                g2 = sb("g2", [4, TB], F32, ws)
                g3 = sb("g3", [4, TB], F32, ws)
                gsm = sb("gsm", [4, 4 * NTB + 8 + 4 * NTB], F32, ws)
                tmx = gsm[:, 0:NTB]
                MgE = gsm[:, NTB:2 * NTB + 1]
                wpv = gsm[:, 2 * NTB + 4:3 * NTB + 4]
                wpd = gsm[:, 3 * NTB + 8:3 * NTB + 8 + 4 * NTB]
                gtm = sb("gtm", [128, NTB, 8], F32, ws)
                wpr = sb("wpr", [128, NTB, 4], F32, ws)
                b_g = P.buf("gates")
                k_tm = sb("k_tm", [128, NTB, 512], BF16, ws)
                v_aug = sb("v_aug", [128, NTB, 4, 129], BF16, ws)
                b_kt = P.bufs(nt, "ktm")
                b_va = P.bufs(nt, "vaug")
                MSET("pool", v_aug[:, :, :, 128:129], 1.0, b_va)
                ktl = [sb(f"ktl{i}", [128, 4, 128], BF16, ws) for i in range(2)]
                b_ktl = P.bufs(2, "ktl")
                if not state_only:
                    qT = sb("qT", [128, 4, TB], BF16, ws)
                    kT = sb("kT", [128, 4, TB], BF16, ws)
                    b_qT = P.bufs(nt, "qT")
                    b_kT = P.bufs(nt, "kT")
                    sig_o = sb("sig_o", [128, NTB, 512], BF16, ws)
                    b_so = P.bufs(nt, "sigo")
                    yT = sb("yT", [128, KC, TB], BF16, ws)
                    b_yTa = P.bufs(nt, "yTa")
                    b_yTb = P.bufs(nt, "yTb")
                    Cbf = sb("Cbf", [128, 4, 129], BF16, ws)
                    b_Cbf = P.buf("Cbf")
                    ptl = [sb(f"ptl{i}", [128, 4, 128], BF16, ws) for i in range(2)]
                    b_ptl = P.bufs(2, "ptl")
                    yal = [sb(f"yal{i}", [128, 512], BF16, ws) for i in range(2)]
                    b_yal = P.bufs(2, "yal")
                    dn = sb("dn", [128, 8], F32, ws)
                    b_dn = P.bufs(2, "dn")
                rgs = []
                for i_ in range(2):
                    rgs.append(dict(rxn=sb("rxn", [128, TB + 3], F32, ws), xc=sb("xc", [128, TB], F32, ws),
                                    xcb=sb("xcb", [128, TB], BF16, ws), rr=sb("rr", [128, TB], F32, ws),
                                    ii=sb("ii", [128, TB], F32, ws), a2=sb("a2", [128, TB], F32, ws),
                                    hs=sb("hs", [128, TB], F32, ws), b=P.bufs(7, "rgw")))

                for ri, (t0, n) in enumerate(ranges):
                    if reset_before is not None and ri == reset_before:
                        TSo("dve", Cst[:], Cst[:], flag[:, 0:1], None, ALU.mult, None, [b_C, b_c], [b_C])
                        TSo("dve", gcar[:], gcar[:], flag[0:4, 0:1], None, ALU.mult, None, [b_gc, b_c], [b_gc])
                        TSo("dve", hst[:], hst[:], flag[:, 0:1], None, ALU.mult, None, b_rgs + [b_c], b_rgs)
                        TSo("dve", rxhist[:].rearrange("p a b -> p (a b)"), rxhist[:].rearrange("p a b -> p (a b)"),
                            flag[:, 0:1], None, ALU.mult, None, b_rgs + [b_c], b_rgs)
                    T_ = n * 128
                    c0 = t0 * 128
                    R = slice(c0, c0 + T_)
                    ub = [uT_b[j] for j in range(t0, t0 + n)]
                    wq, wqb = pcs["in_q"]
                    wk_, wkb = pcs["in_k"]
                    wv_, wvb = pcs["in_v"]
                    wo_, wob = pcs["in_o"]
                    wrx, wrxb = pcs["in_rx"]
                    wry, wryb = pcs["in_ry"]
                    for (j0, gn) in tok_groups(t0, n):
                        cs = slice(j0 * 128, (j0 + gn) * 128)
                        gub = [uT_b[j] for j in range(j0, j0 + gn)]
                        pti, pbi = PS()
                        for kc in range(KC):
                            MM(pti[0:4, 0:gn * 128], wgif[:, kc, 0:4], uT[:, kc, cs], kc == 0, kc == KC - 1, gub + [b_c], [pbi])
                        ACTF(g1[:, cs], pti[0:4, 0:gn * 128], AF.Identity, [pbi, b_c], [b_g], bias=bif[:, 0:1])
                        ptf, pbf = PS()
                        for kc in range(KC):
                            MM(ptf[0:4, 0:gn * 128], wgif[:, kc, 4:8], uT[:, kc, cs], kc == 0, kc == KC - 1, gub + [b_c], [pbf])
                        ACTF(g2[:, cs], ptf[0:4, 0:gn * 128], AF.Exp, [pbf, b_c], [b_g], scale=-1.0, bias=bif[:, 1:2])
                    ACTF(g2[:, R], g2[:, R], AF.Ln, [b_g], [b_g], bias=1.0)
                    P.dve(lambda e, R=R, T_=T_: e.tensor_tensor_scan(out=g3[:, R], data0=onesrow[:, 0:T_], data1=g2[:, R],
                                                                     initial=gcar[:, 0:1], op0=ALU.mult, op1=ALU.subtract),
                          [b_g, b_gc, b_c], [b_g])
                    CPo("dve", gcar[:, 0:1], g3[:, c0 + T_ - 1:c0 + T_], [b_g], [b_gc])
                    TTo("dve", g1[:, R], g1[:, R], g3[:, R], ALU.subtract, [b_g], [b_g])
                    P.dve(lambda e, R=R, n=n: e.tensor_reduce(out=tmx[:, 0:n], in_=g1[:, R].rearrange("p (j t) -> p j t", t=128),
                                                              axis=AX.X, op=ALU.max), [b_g], [b_g])
                    CPo("dve", MgE[:, 0:1], gcar[:, 1:2], [b_gc], [b_g])
                    P.dve(lambda e, n=n: e.tensor_tensor_scan(out=MgE[:, 1:n + 1], data0=onesrow[:, 0:n], data1=tmx[:, 0:n],
                                                              initial=MgE[:, 0:1], op0=ALU.mult, op1=ALU.max),
                          [b_g, b_c], [b_g])
                    CPo("dve", gcar[:, 1:2], MgE[:, n:n + 1], [b_g], [b_gc])
                    TTo("dve", wpv[:, 0:n], MgE[:, 0:n], MgE[:, 1:n + 1], ALU.subtract, [b_g], [b_g])
                    ACTF(wpv[:, 0:n], wpv[:, 0:n], AF.Exp, [b_g], [b_g])
                    Mb = MgE[:, 1:n + 1].unsqueeze(2).to_broadcast([4, n, 128])
                    g1v = g1[:, R].rearrange("p (j t) -> p j t", t=128)
                    g2v = g2[:, R].rearrange("p (j t) -> p j t", t=128)
                    g3v = g3[:, R].rearrange("p (j t) -> p j t", t=128)
                    TTo("dve", g2v, g1v, Mb, ALU.subtract, [b_g], [b_g])
                    ACTF(g2[:, R], g2[:, R], AF.Exp, [b_g], [b_g], bias=math.log(128 ** -0.5))
                    TTo("dve", g3v, g3v, Mb, ALU.add, [b_g], [b_g])
                    ACTF(g3[:, R], g3[:, R], AF.Exp, [b_g], [b_g], scale=-1.0)
                    if not state_only:
                        for c in range(4):
                            proj_fm(uT, uT_b, t0, n, wq, wqb, c * 128,
                                    lambda pt, pb, j0, gn, c=c: CPo("act", qT[:, c, j0 * 128:(j0 + gn) * 128], pt[:, 0:gn * 128],
                                                                    [pb], [b_qT[j] for j in range(j0, j0 + gn)]))
                            proj_fm(uT, uT_b, t0, n, wk_, wkb, c * 128,
                                    lambda pt, pb, j0, gn, c=c: CPo("dve", kT[:, c, j0 * 128:(j0 + gn) * 128], pt[:, 0:gn * 128],
                                                                    [pb], [b_kT[j] for j in range(j0, j0 + gn)]))
                    for j in range(t0, t0 + n):
                        pt, pb = proj_tm(uT, uT_b, j, wk_, wkb, 0, 512)
                        CPo("act", k_tm[:, j, :], pt[:], [pb], [b_kt[j]])
                        pt, pb = proj_tm(uT, uT_b, j, wv_, wvb, 0, 512)
                        CPo("dve", v_aug[:, j, :, 0:128], pt[:].rearrange("p (h d) -> p h d", h=4), [pb], [b_va[j]])
                        if not state_only:
                            pt, pb = proj_tm(uT, uT_b, j, wo_, wob, 0, 512)
                            ACTF(sig_o[:, j, :], pt[:], AF.Sigmoid, [pb], [b_so[j]])

                    wpdv = wpd[:, 0:4 * n].rearrange("p (j h) -> p j h", h=4)
                    TTo("dve", wpdv, wpv[:, 0:n].unsqueeze(2).to_broadcast([4, n, 4]),
                        ident_f[0:4, 0:4].unsqueeze(1).to_broadcast([4, n, 4]), ALU.mult, [b_g, b_c], [b_g])
                    pt, pb = PS()
                    MM(pt[:, 0:4 * n], ones4[:], wpd[:, 0:4 * n], True, True, [b_g, b_c], [pb])
                    CPo("dve", wpr[:, t0:t0 + n, :], pt[:, 0:4 * n].rearrange("p (j h) -> p j h", h=4), [pb], [b_g])
                    pt, pb = PS()
                    for jj in range(n):
                        cj = slice(c0 + jj * 128, c0 + (jj + 1) * 128)
                        MM(pt[:, jj * 8:jj * 8 + 4], g2[:, cj], ident_f[0:4, 0:4], True, True, [b_g, b_c], [pb])
                        MM(pt[:, jj * 8 + 4:jj * 8 + 8], g3[:, cj], ident_f[0:4, 0:4], True, True, [b_g, b_c], [pb])
                    CPo("dve", gtm[:, t0:t0 + n, :], pt[:, 0:8 * n].rearrange("p (j h) -> p j h", h=8), [pb], [b_g])

                    for j in range(t0, t0 + n):
                        cj = slice(j * 128, (j + 1) * 128)
                        wk4 = gtm[:, j, 0:4]
                        kt = ktl[j % 2]
                        TTo("pool", kt[:], k_tm[:, j, :].rearrange("p (h d) -> p h d", h=4),
                            wk4.unsqueeze(2).to_broadcast([128, 4, 128]), ALU.mult, [b_kt[j], b_g], [b_ktl[j % 2]])
                        TTo("dve", Cst[:], Cst[:], wpr[:, j, :].unsqueeze(2).to_broadcast([128, 4, 129]), ALU.mult,
                            [b_C, b_g], [b_C])
                        if not state_only:
                            CPo("act", Cbf[:], Cst[:], [b_C], [b_Cbf])
                            pss, pssb = PS()
                            for h_ in range(4):
                                MM(pss[:, h_ * 128:(h_ + 1) * 128], kT[:, h_, cj], qT[:, h_, cj], True, True,
                                   [b_kT[j], b_qT[j]], [pssb])
                            ptb_ = ptl[j % 2]
                            for h_ in range(4):
                                STTo(ptb_[:, h_, :], pss[:, h_ * 128:(h_ + 1) * 128], gtm[:, j, h_:h_ + 1], tri[:],
                                     ALU.mult, ALU.mult, [pssb, b_g, b_c], [b_ptl[j % 2]])
                            psn = [PS(), PS()]
                            for h_ in range(4):
                                pn_, pnb = psn[h_ // 2]
                                reg = pn_[:, (h_ % 2) * 129:(h_ % 2) * 129 + 129]
                                MM(reg, ptb_[:, h_, :], v_aug[:, j, h_, :], True, False, [b_ptl[j % 2], b_va[j]], [pnb])
                                MM(reg, qT[:, h_, cj], Cbf[:, h_, :], False, True, [b_qT[j], b_Cbf], [pnb])
                            d_ = dn[:, 4 * (j % 2):4 * (j % 2) + 4]
                            for bk in range(2):
                                pn_, pnb = psn[bk]
                                ACTF(d_[:, 2 * bk:2 * bk + 2], pn_[:, 0:258].rearrange("p (h e) -> p h e", e=129)[:, :, 128],
                                     AF.Abs, [pnb], [b_dn[j % 2]])
                            TTo("dve", d_, d_, gtm[:, j, 4:8], ALU.max, [b_dn[j % 2], b_g], [b_dn[j % 2]])
                            P.dve(lambda e, d_=d_: e.reciprocal(out=d_, in_=d_), [b_dn[j % 2]], [b_dn[j % 2]])
                            ya = yal[j % 2]
                            for h_ in range(4):
                                pn_, pnb = psn[h_ // 2]
                                STTo(ya[:, h_ * 128:(h_ + 1) * 128], pn_[:, (h_ % 2) * 129:(h_ % 2) * 129 + 128], d_[:, h_:h_ + 1],
                                     sig_o[:, j, h_ * 128:(h_ + 1) * 128], ALU.mult, ALU.mult,
                                     [pnb, b_dn[j % 2], b_so[j]], [b_yal[j % 2]])
                            pt, pb = PS()
                            ptb16 = pt[:].bitcast(BF16)
                            for h_ in range(4):
                                P.pe(lambda e, h_=h_, ya=ya, ptb16=ptb16: e.transpose(out=ptb16[:, h_ * 128:(h_ + 1) * 128],
                                                                                      in_=ya[:, h_ * 128:(h_ + 1) * 128],
                                                                                      identity=ident_bf[:]),
                                     [b_yal[j % 2], b_c], [pb])
                            CPo("act", yT[:, 0:4, cj], ptb16[:, 0:512].rearrange("p (h t) -> p h t", h=4), [pb], [b_yTa[j]])
                        psk = [PS(), PS()]
                        for h_ in range(4):
                            pk_, pkb = psk[h_ // 2]
                            MM(pk_[:, (h_ % 2) * 129:(h_ % 2) * 129 + 129], kt[:, h_, :], v_aug[:, j, h_, :], True, True,
                               [b_ktl[j % 2], b_va[j]], [pkb])
                        for bk in range(2):
                            pk_, pkb = psk[bk]
                            TTo("dve", Cst[:, 2 * bk:2 * bk + 2, :], Cst[:, 2 * bk:2 * bk + 2, :],
                                pk_[:, 0:258].rearrange("p (h e) -> p h e", e=129), ALU.add, [pkb, b_C], [b_C])

                    def rg_chain(n_):
                        rg_ = rgs[n_ % 2]
                        b_rg = b_rgs[n_]
                        rxn, xc, xcb, rr, ii, a2, hs = (rg_["rxn"], rg_["xc"], rg_["xcb"], rg_["rr"], rg_["ii"], rg_["a2"], rg_["hs"])
                        b_rxn, b_xc, b_xcb, b_rr, b_ii, b_a2, b_hs = rg_["b"]
                        proj_fm(uT, uT_b, t0, n, wrx, wrxb, n_ * 128,
                                lambda pt, pb, j0, gn: CPo("act", rxn[:, 3 + (j0 - t0) * 128:3 + (j0 - t0 + gn) * 128],
                                                           pt[:, 0:gn * 128], [pb], [b_rxn]))
                        CPo("pool", rxn[:, 0:3], rxhist[:, n_, :], [b_rg], [b_rxn])
                        yield
                        TSo("dve", xc[:, 0:T_], rxn[:, 0:T_], cw[:, 0, n_:n_ + 1], cb[:, n_:n_ + 1], ALU.mult, ALU.add,
                            [b_rxn, b_c], [b_xc])
                        for jj in range(1, 4):
                            STTo(xc[:, 0:T_], rxn[:, jj:jj + T_], cw[:, jj, n_:n_ + 1], xc[:, 0:T_], ALU.mult, ALU.add,
                                 [b_rxn, b_c, b_xc], [b_xc])
                        CPo("pool", rxhist[:, n_, :], rxn[:, T_:T_ + 3], [b_rxn], [b_rg])
                        CPo("pool", xcb[:, 0:T_], xc[:, 0:T_], [b_xc], [b_xcb])
                        yield
                        for (j0, gn) in tok_groups(0, n):
                            cs = slice(j0 * 128, (j0 + gn) * 128)
                            pr, prb = PS()
                            MM(pr[:, 0:gn * 128], wrg[:, 0, n_, :], xcb[:, cs], True, True, [b_xcb, b_c], [prb])
                            ACTF(rr[:, cs], pr[:, 0:gn * 128], AF.Sigmoid, [prb, b_c], [b_rr], bias=rba[:, n_:n_ + 1])
                            pi_, pib = PS()
                            MM(pi_[:, 0:gn * 128], wrg[:, 1, n_, :], xcb[:, cs], True, True, [b_xcb, b_c], [pib])
                            ACTF(ii[:, cs], pi_[:, 0:gn * 128], AF.Sigmoid, [pib, b_c], [b_ii], bias=rbx[:, n_:n_ + 1])
                        yield
                        ACTF(a2[:, 0:T_], rr[:, 0:T_], AF.Exp, [b_rr, b_c], [b_a2], scale=cl[:, 4 + n_:5 + n_])
                        ACTF(rr[:, 0:T_], rr[:, 0:T_], AF.Exp, [b_rr, b_c], [b_rr], scale=cl[:, n_:n_ + 1])
                        TSo("dve", a2[:, 0:T_], a2[:, 0:T_], 1.0, None, ALU.min, None, [b_a2], [b_a2])
                        ACTF(a2[:, 0:T_], a2[:, 0:T_], AF.Sqrt, [b_a2], [b_a2], scale=-1.0, bias=1.0)
                        yield
                        TTo("pool", ii[:, 0:T_], ii[:, 0:T_], xc[:, 0:T_], ALU.mult, [b_ii, b_xc], [b_ii])
                        TTo("pool", a2[:, 0:T_], a2[:, 0:T_], ii[:, 0:T_], ALU.mult, [b_a2, b_ii], [b_a2])
                        yield
                        P.dve(lambda e, n_=n_, T_=T_, hs=hs, rr=rr, a2=a2: e.tensor_tensor_scan(out=hs[:, 0:T_], data0=rr[:, 0:T_], data1=a2[:, 0:T_],
                                                                           initial=hst[:, n_:n_ + 1], op0=ALU.mult, op1=ALU.add),
                              [b_rr, b_a2, b_rg], [b_hs])
                        CPo("pool", hst[:, n_:n_ + 1], hs[:, T_ - 1:T_], [b_hs], [b_rg])
                        yield
                        if not state_only:
                            proj_fm(uT, uT_b, t0, n, wry, wryb, n_ * 128,
                                    lambda pt, pb, j0, gn: ACTF(yT[:, 4 + n_, j0 * 128:(j0 + gn) * 128], pt[:, 0:gn * 128],
                                                                AF.Gelu_apprx_tanh, [pb],
                                                                [b_yTb[j] for j in range(j0, j0 + gn)]))
                            ybs = [b_yTb[j] for j in range(t0, t0 + n)]
                            TTo("dve", yT[:, 4 + n_, R], yT[:, 4 + n_, R], hs[:, 0:T_], ALU.mult, ybs + [b_hs], ybs)
                    for pair in ((0, 1), (2, 3)):
                        interleave([rg_chain(a_) for a_ in pair])
                if not state_only:
                    release(6)
                    wo0, wo0b = acquire("out")
                    wo1, wo1b = acquire("out")
                    pws = mk_post_ws(ws, "a")
                    for j in range(nt):
                        yb_ = [b_yTa[j], b_yTb[j]]
                        pA, pAb = PS()
                        pB, pBb = PS()
                        for kc in range(KC):
                            MM(pA[:], yT[:, kc, j * 128:(j + 1) * 128], wo0[:, kc, :], kc == 0, kc == KC - 1, yb_ + [wo0b], [pAb])
                        for kc in range(KC):
                            MM(pB[:], yT[:, kc, j * 128:(j + 1) * 128], wo1[:, kc, :], kc == 0, kc == KC - 1, yb_ + [wo1b], [pBb])
                        postnorm(pA, pAb, pB, pBb, gp, gp_b, j, pws)
                    release(2)
                P.barrier()

        def xattn(l, nt):
            SC = 256 ** -0.5
            with ExitStack() as ws:
                g, g_b = gload("g_x_pre", l)
                gp, gp_b = gload("g_x_post", l)
                uT = sb("uT", [128, KC, TB], BF16, ws)
                uT_b = P.bufs(nt, "uT")
                prenorm(lambda j: h[:, j, :], h_b, nt, g, g_b, uT, uT_b, ws, "x")
                xqT = sb("xqT", [128, 8, TB], BF16, ws)
                b_xq = P.bufs(nt, "xq")
                for pc in range(2):
                    wv, wb = acquire(f"xq{l}")
                    for c in range(4):
                        proj_fm(uT, uT_b, 0, nt, wv, wb, c * 128,
                                lambda pt, pb, j0, gn, cc=pc * 4 + c: CPo("act", xqT[:, cc, j0 * 128:(j0 + gn) * 128],
                                                                          pt[:, 0:gn * 128], [pb],
                                                                          [b_xq[j] for j in range(j0, j0 + gn)]))
                    release()
                wo0, wo0b = acquire(f"xo{l}")
                wo1, wo1b = acquire(f"xo{l}")
                pf = [sb(f"xpf{i}", [128, 4, 256], F32, ws) for i in range(2)]
                pn = [sb(f"xpn{i}", [128, 4, 256], BF16, ws) for i in range(2)]
                pT = [sb(f"xpT{i}", [128, 8, 128], BF16, ws) for i in range(2)]
                xoT = [sb(f"xoT{i}", [128, 8, 128], BF16, ws) for i in range(2)]
                sm = sb("xsm", [128, 2, 12], F32, ws)
                b_pf, b_pn, b_pT, b_xo, b_sm = (P.bufs(2, "xpf"), P.bufs(2, "xpn"), P.bufs(2, "xpT"),
                                                 P.bufs(2, "xoT"), P.bufs(2, "xsm"))
                pws = mk_post_ws(ws, "x")

                def xa_tile(j):
                    q = j % 2
                    cj = slice(j * 128, (j + 1) * 128)
                    psc = [PS(), PS()]
                    for h_ in range(4):
                        pt, pb = psc[h_ // 2]
                        for dc in range(2):
                            MM(pt[:, (h_ % 2) * 256:(h_ % 2) * 256 + 256], xqT[:, 2 * h_ + dc, cj], KT[:, l, 2 * h_ + dc, :],
                               dc == 0, dc == 1, [b_xq[j], b_kv], [pb])
                    yield
                    mx = sm[:, q, 0:4]
                    rs = sm[:, q, 4:8]
                    rrs = sm[:, q, 8:12]
                    for bk in range(2):
                        pt, pb = psc[bk]
                        P.dve(lambda e, pt=pt, mx=mx, bk=bk: e.tensor_reduce(out=mx[:, 2 * bk:2 * bk + 2],
                                                                             in_=pt[:].rearrange("p (h m) -> p h m", h=2),
                                                                             axis=AX.X, op=ALU.max), [pb], [b_sm[q]])
                    TSo("dve", mx, mx, -SC, None, ALU.mult, None, [b_sm[q]], [b_sm[q]])
                    for h_ in range(4):
                        pt, pb = psc[h_ // 2]
                        ACTF(pf[q][:, h_, :], pt[:, (h_ % 2) * 256:(h_ % 2) * 256 + 256], AF.Exp, [pb, b_sm[q]], [b_pf[q], b_sm[q]],
                             scale=SC, bias=mx[:, h_:h_ + 1], accum_out=rs[:, h_:h_ + 1])
                    P.dve(lambda e, rs=rs, rrs=rrs: e.reciprocal(out=rrs, in_=rs), [b_sm[q]], [b_sm[q]])
                    TTo("pool", pn[q][:], pf[q][:], rrs.unsqueeze(2).to_broadcast([128, 4, 256]), ALU.mult,
                        [b_pf[q], b_sm[q]], [b_pn[q]])
                    yield
                    pt, pb = PS()
                    ptb16 = pt[:].bitcast(BF16)
                    for h_ in range(4):
                        for mc in range(2):
                            P.pe(lambda e, h_=h_, mc=mc, q=q, ptb16=ptb16: e.transpose(
                                out=ptb16[:, (h_ * 2 + mc) * 128:(h_ * 2 + mc + 1) * 128],
                                in_=pn[q][:, h_, mc * 128:(mc + 1) * 128], identity=ident_bf[:]), [b_pn[q], b_c], [pb])
                    CPo("act", pT[q][:], ptb16.rearrange("p (a t) -> p a t", a=8), [pb], [b_pT[q]])
                    yield
                    pso = [PS(), PS()]
                    for h_ in range(4):
                        for dc in range(2):
                            idx = 2 * h_ + dc
                            pt, pb = pso[idx // 4]
                            for mc in range(2):
                                MM(pt[:, (idx % 4) * 128:(idx % 4 + 1) * 128],
                                   VM[:, l, mc, h_ * 256 + dc * 128:h_ * 256 + (dc + 1) * 128], pT[q][:, h_ * 2 + mc, :],
                                   mc == 0, mc == 1, [b_kv, b_pT[q]], [pb])
                    for bk in range(2):
                        pt, pb = pso[bk]
                        CPo("dve", xoT[q][:, 4 * bk:4 * bk + 4, :], pt[:].rearrange("p (a t) -> p a t", a=4), [pb], [b_xo[q]])
                    yield
                    pA, pAb = PS()
                    pB, pBb = PS()
                    for kc in range(KC):
                        MM(pA[:], xoT[q][:, kc, :], wo0[:, kc, :], kc == 0, kc == KC - 1, [b_xo[q], wo0b], [pAb])
                    for kc in range(KC):
                        MM(pB[:], xoT[q][:, kc, :], wo1[:, kc, :], kc == 0, kc == KC - 1, [b_xo[q], wo1b], [pBb])
                    postnorm(pA, pAb, pB, pBb, gp, gp_b, j, pws)

                for j0_ in range(0, nt, 2):
                    interleave([xa_tile(j_) for j_ in range(j0_, min(nt, j0_ + 2))])
                release(2)
                P.barrier()

        def mlp(l, nt):
            with ExitStack() as ws:
                g, g_b = gload("g_ff_pre", l)
                gp, gp_b = gload("g_ff_post", l)
                uT = sb("uT", [128, KC, TB], BF16, ws)
                uT_b = P.bufs(nt, "uT")
                prenorm(lambda j: h[:, j, :], h_b, nt, g, g_b, uT, uT_b, ws, "f")
                hid = sb("hid", [128, 16, TB], BF16, ws)
                acc = sb("acc", [128, NTB, D], F32, ws)
                b_acc = P.bufs(nt, "acc")
                rl = [sb(f"rl{i}", [128, 512], BF16, ws) for i in range(2)]
                b_rl = P.bufs(2, "rl")
                cnt = [0]
                pws = mk_post_ws(ws, "f")
                b_hid = P.bufs(nt, "hid")
                for half in range(2):
                    for pc in range(4):
                        wv, wb = acquire(f"ff1_{l}")
                        for c in range(4):
                            ch = pc * 4 + c

                            def ev(pt, pb, j0, gn, ch=ch, b_hid=b_hid):
                                k = cnt[0] % 2
                                cnt[0] += 1
                                ACTF(rl[k][:, 0:gn * 128], pt[:, 0:gn * 128], AF.Relu, [pb], [b_rl[k]])
                                TTo("pool", hid[:, ch, j0 * 128:(j0 + gn) * 128], rl[k][:, 0:gn * 128], rl[k][:, 0:gn * 128],
                                    ALU.mult, [b_rl[k]], [b_hid[j] for j in range(j0, j0 + gn)])
                            proj_fm(uT, uT_b, 0, nt, wv, wb, c * 128, ev)
                        release()
                    w2 = [acquire(f"ff2_{l}") for _ in range(4)]
                    for j in range(nt):
                        pA, pAb = PS()
                        pB, pBb = PS()
                        for (pp, ppb, c0) in ((pA, pAb, 0), (pB, pBb, 512)):
                            for k in range(16):
                                wv, wb = w2[k // 4]
                                MM(pp[:], hid[:, k, j * 128:(j + 1) * 128], wv[:, k % 4, c0:c0 + 512], k == 0, k == 15,
                                   [b_hid[j], wb], [ppb])
                        if half == 0:
                            CPo("act", acc[:, j, 0:512], pA[:], [pAb], [b_acc[j]])
                            CPo("dve", acc[:, j, 512:1024], pB[:], [pBb], [b_acc[j]])
                        else:
                            TTo("dve", acc[:, j, 0:512], acc[:, j, 0:512], pA[:], ALU.add, [pAb, b_acc[j]], [b_acc[j]])
                            TTo("dve", acc[:, j, 512:1024], acc[:, j, 512:1024], pB[:], ALU.add, [pBb, b_acc[j]], [b_acc[j]])
                            postnorm(acc[:, j, 0:512], b_acc[j], acc[:, j, 512:1024], b_acc[j], gp, gp_b, j, pws)
                    release(4)
                P.barrier()

        def swa(nt, mbase):
            SC = 64 ** -0.5
            PIB = 3.1415925
            with ExitStack() as ws:
                g, g_b = gload("g_mix_pre", 1)
                gp, gp_b = gload("g_mix_post", 1)
                uT = sb("uT", [128, KC, TB], BF16, ws)
                uT_b = P.bufs(nt, "uT")
                prenorm(lambda j: h[:, j, :], h_b, nt, g, g_b, uT, uT_b, ws, "s")
                wq0, wq0b = acquire("qkv")
                wq1, wq1b = acquire("qkv")
                wkv, wkvb = acquire("qkv")
                wo0, wo0b = acquire("o1")
                wo1, wo1b = acquire("o1")
                ang = sb("ang", [128, NTB, 32], F32, ws)
                tmd = sb("tmd", [128, NTB, 32], F32, ws)
                cosv = sb("cosv", [128, NTB, 32], F32, ws)
                sinv = sb("sinv", [128, NTB, 32], F32, ws)
                b_rp = P.buf("rope")
                A_ = ang[:, 0:nt, :]
                TTo("dve", A_, posf[:, mbase:mbase + nt].unsqueeze(2).to_broadcast([128, nt, 32]),
                    invf[:].unsqueeze(1).to_broadcast([128, nt, 32]), ALU.mult, [b_c], [b_rp])
                kf = sb("kf", [128, NTB, 32], F32, ws)

                def red_sin(dst, shift):
                    T_ = tmd[:, 0:nt, :]
                    K_ = kf[:, 0:nt, :]
                    TSo("dve", T_, A_, shift, None, ALU.add, None, [b_rp], [b_rp])
                    TSo("dve", K_, T_, 1.0 / (2 * PI), None, ALU.mult, None, [b_rp], [b_rp])
                    TSo("dve", K_, K_, 12582912.0, None, ALU.add, None, [b_rp], [b_rp])
                    TSo("dve", K_, K_, 12582912.0, None, ALU.subtract, None, [b_rp], [b_rp])
                    STTo(T_, K_, -2 * PI, T_, ALU.mult, ALU.add, [b_rp], [b_rp])
                    TSo("dve", K_, T_, PI, 2 * PI, ALU.is_gt, ALU.mult, [b_rp], [b_rp])
                    TTo("dve", T_, T_, K_, ALU.subtract, [b_rp], [b_rp])
                    TSo("dve", K_, T_, -PI, 2 * PI, ALU.is_lt, ALU.mult, [b_rp], [b_rp])
                    TTo("dve", T_, T_, K_, ALU.add, [b_rp], [b_rp])
                    TSo("dve", T_, T_, PIB, -PIB, ALU.min, ALU.max, [b_rp], [b_rp])
                    ACTF(dst, T_, AF.Sin, [b_rp], [b_rp])

                red_sin(sinv[:, 0:nt, :], 0.0)
                red_sin(cosv[:, 0:nt, :], PI / 2)
                qkf = [sb(f"qkf{i}", [128, 20, 64], F32, ws) for i in range(2)]
                rt = [sb(f"rt{i}", [128, 20, 32], F32, ws) for i in range(4)]
                qkr = [sb(f"qkr{i}", [128, 24, 64], BF16, ws) for i in range(2)]
                qTl = [sb(f"qTl{i}", [128, 8, 128], BF16, ws) for i in range(2)]
                sf = [sb(f"sf{i}", [128, 4, 256], F32, ws) for i in range(2)]
                pn = [sb(f"spn{i}", [128, 4, 256], BF16, ws) for i in range(2)]
                pT = [sb(f"spT{i}", [128, 8, 128], BF16, ws) for i in range(2)]
                obf = [sb(f"obf{i}", [128, D], BF16, ws) for i in range(2)]
                oT = [sb(f"oT{i}", [128, 8, 128], BF16, ws) for i in range(2)]
                sm = sb("ssm", [128, 2, 20], F32, ws)
                b_qkf, b_qkr, b_qTl, b_sf, b_pn, b_pT, b_obf, b_oT, b_sm = [P.bufs(2, "sw") for _ in range(9)]
                b_rt = P.bufs(4, "rt")
                pws = mk_post_ws(ws, "s")
                gi = [0]
                for j in range(nt):
                    m = mbase + j
                    q = j % 2
                    cur, prv = m % 2, (m - 1) % 2
                    pq0, pq0b = proj_tm(uT, uT_b, j, wq0, wq0b, 0, 512)
                    pq1, pq1b = proj_tm(uT, uT_b, j, wq1, wq1b, 0, 512)
                    pkv, pkvb = proj_tm(uT, uT_b, j, wkv, wkvb, 0, 512)
                    CPo("act", qkf[q][:, 0:8, :], pq0[:].rearrange("p (a d) -> p a d", d=64), [pq0b], [b_qkf[q]])
                    CPo("act", qkf[q][:, 8:16, :], pq1[:].rearrange("p (a d) -> p a d", d=64), [pq1b], [b_qkf[q]])
                    CPo("dve", qkf[q][:, 16:20, :], pkv[:, 0:256].rearrange("p (a d) -> p a d", d=64), [pkvb], [b_qkf[q]])
                    CPo("dve", vs[:, cur, :], pkv[:, 256:512], [pkvb], [kv_b[cur]])
                    t1 = qkf[q][:, :, 0:32]
                    t2 = qkf[q][:, :, 32:64]
                    cbb = cosv[:, j, :].unsqueeze(1).to_broadcast([128, 20, 32])
                    sbb = sinv[:, j, :].unsqueeze(1).to_broadcast([128, 20, 32])
                    TTo("dve", rt[0][:], t1, cbb, ALU.mult, [b_qkf[q], b_rp], [b_rt[0]])
                    TTo("dve", rt[1][:], t2, sbb, ALU.mult, [b_qkf[q], b_rp], [b_rt[1]])
                    TTo("pool", rt[2][:], t2, cbb, ALU.mult, [b_qkf[q], b_rp], [b_rt[2]])
                    TTo("pool", rt[3][:], t1, sbb, ALU.mult, [b_qkf[q], b_rp], [b_rt[3]])
                    TTo("dve", qkr[q][:, 0:20, 0:32], rt[0][:], rt[1][:], ALU.subtract, [b_rt[0], b_rt[1]], [b_qkr[q]])
                    TTo("pool", qkr[q][:, 0:20, 32:64], rt[2][:], rt[3][:], ALU.add, [b_rt[2], b_rt[3]], [b_qkr[q]])
                    for dst, src in ((20, 17), (21, 16), (22, 19), (23, 18)):
                        CPo("pool", qkr[q][:, dst, :], qkr[q][:, src, :], [b_qkr[q]], [b_qkr[q]])
                    pt, pb = PS()
                    ptb16 = pt[:].bitcast(BF16)
                    for p_ in range(8):
                        P.pe(lambda e, p_=p_, q=q, ptb16=ptb16: e.transpose(
                            out=ptb16[:, p_ * 128:(p_ + 1) * 128],
                            in_=qkr[q][:, 2 * p_:2 * p_ + 2, :].rearrange("p a d -> p (a d)"), identity=ident_bf[:]),
                            [b_qkr[q], b_c], [pb])
                    CPo("act", qTl[q][:], ptb16.rearrange("p (a t) -> p a t", a=8), [pb], [b_qTl[q]])
                    pt, pb = PS()
                    ptb16 = pt[:].bitcast(BF16)
                    for i_ in range(4):
                        P.pe(lambda e, i_=i_, q=q, ptb16=ptb16: e.transpose(
                            out=ptb16[:, i_ * 128:(i_ + 1) * 128],
                            in_=qkr[q][:, 16 + 2 * i_:18 + 2 * i_, :].rearrange("p a d -> p (a d)"), identity=ident_bf[:]),
                            [b_qkr[q], b_c], [pb])
                    for i_, (ga, gb) in enumerate(((0, 1), (2, 3), (1, 0), (3, 2))):
                        CPo("dve", kTs[0:64, cur, ga * 2, :], ptb16[0:64, i_ * 128:(i_ + 1) * 128], [pb], [kv_b[cur]])
                        CPo("act", kTs[64:128, cur, gb * 2 + 1, :], ptb16[64:128, i_ * 128:(i_ + 1) * 128], [pb], [kv_b[cur]])
                    mask = mask_fc if m == 1 else mask_pc
                    pso = [(ps_t[6], ps_b[6]), (ps_t[7], ps_b[7])]
                    SWL = int(os.environ.get("MK_SWA", "9"))
                    if SWL < 2:
                        continue
                    for gq in range(4):
                        w_ = gi[0] % 2
                        gi[0] += 1
                        psc = [PS(), PS()]
                        for hh in range(4):
                            h_ = 4 * gq + hh
                            p_ = h_ // 2
                            o_ = (h_ % 2) * 64
                            var = gq * 2 + (1 if o_ else 0)
                            pt, pb = psc[hh // 2]
                            base = (hh % 2) * 256
                            MM(pt[:, base:base + 128], qTl[q][:, p_, :], kTs[:, prv, var, :], True, True,
                               [b_qTl[q], kv_b[prv]], [pb])
                            MM(pt[:, base + 128:base + 256], qTl[q][:, p_, :], kTs[:, cur, var, :], True, True,
                               [b_qTl[q], kv_b[cur]], [pb])
                        for bk in range(2):
                            pt, pb = psc[bk]
                            TTo("dve", sf[w_][:, 2 * bk:2 * bk + 2, :], pt[:].rearrange("p (a c) -> p a c", a=2),
                                mask[:].unsqueeze(1).to_broadcast([128, 2, 256]), ALU.add, [pb, b_c], [b_sf[w_]])
                        if SWL < 3:
                            continue
                        mx = sm[:, w_, 0:4]
                        ngm = sm[:, w_, 4:8]
                        rs = sm[:, w_, 8:12]
                        t4 = sm[:, w_, 12:16]
                        rrs = sm[:, w_, 16:20]
                        sk = sinks[:, 4 * gq:4 * gq + 4]
                        P.dve(lambda e, w_=w_, mx=mx: e.tensor_reduce(out=mx, in_=sf[w_][:], axis=AX.X, op=ALU.max),
                              [b_sf[w_]], [b_sm[w_]])
                        STTo(mx, mx, SC, sk, ALU.mult, ALU.max, [b_sm[w_], b_c], [b_sm[w_]])
                        TSo("dve", ngm, mx, -1.0, None, ALU.mult, None, [b_sm[w_]], [b_sm[w_]])
                        for hh in range(4):
                            ACTF(sf[w_][:, hh, :], sf[w_][:, hh, :], AF.Exp, [b_sf[w_], b_sm[w_]], [b_sf[w_], b_sm[w_]],
                                 scale=SC, bias=ngm[:, hh:hh + 1], accum_out=rs[:, hh:hh + 1])
                        TTo("dve", t4, sk, ngm, ALU.add, [b_sm[w_], b_c], [b_sm[w_]])
                        ACTF(t4, t4, AF.Exp, [b_sm[w_]], [b_sm[w_]])
                        TTo("dve", rs, rs, t4, ALU.add, [b_sm[w_]], [b_sm[w_]])
                        P.dve(lambda e, rs=rs, rrs=rrs: e.reciprocal(out=rrs, in_=rs), [b_sm[w_]], [b_sm[w_]])
                        TTo("pool", pn[w_][:], sf[w_][:], rrs.unsqueeze(2).to_broadcast([128, 4, 256]), ALU.mult,
                            [b_sf[w_], b_sm[w_]], [b_pn[w_]])
                        pt, pb = PS()
                        ptb16 = pt[:].bitcast(BF16)
                        for hh in range(4):
                            for kc2 in range(2):
                                P.pe(lambda e, hh=hh, kc2=kc2, w_=w_, ptb16=ptb16: e.transpose(
                                    out=ptb16[:, (hh * 2 + kc2) * 128:(hh * 2 + kc2 + 1) * 128],
                                    in_=pn[w_][:, hh, kc2 * 128:(kc2 + 1) * 128], identity=ident_bf[:]), [b_pn[w_], b_c], [pb])
                        CPo("act", pT[w_][:], ptb16.rearrange("p (a t) -> p a t", a=8), [pb], [b_pT[w_]])
                        for hh in range(4):
                            h_ = 4 * gq + hh
                            po, pob = pso[h_ // 8]
                            col = (h_ % 8) * 64
                            MM(po[:, col:col + 64], pT[w_][:, hh * 2, :], vs[:, prv, gq * 64:(gq + 1) * 64], True, False,
                               [b_pT[w_], kv_b[prv]], [pob])
                            MM(po[:, col:col + 64], pT[w_][:, hh * 2 + 1, :], vs[:, cur, gq * 64:(gq + 1) * 64], False, True,
                               [b_pT[w_], kv_b[cur]], [pob])
                    if SWL < 4:
                        continue
                    CPo("act", obf[q][:, 0:512], pso[0][0][:], [pso[0][1]], [b_obf[q]])
                    CPo("dve", obf[q][:, 512:1024], pso[1][0][:], [pso[1][1]], [b_obf[q]])
                    pt, pb = PS()
                    ptb16 = pt[:].bitcast(BF16)
                    for kc in range(KC):
                        P.pe(lambda e, kc=kc, q=q, ptb16=ptb16: e.transpose(out=ptb16[:, kc * 128:(kc + 1) * 128],
                                                                           in_=obf[q][:, kc * 128:(kc + 1) * 128],
                                                                           identity=ident_bf[:]), [b_obf[q], b_c], [pb])
                    CPo("act", oT[q][:], ptb16.rearrange("p (a t) -> p a t", a=8), [pb], [b_oT[q]])
                    pA, pAb = PS()
                    pB, pBb = PS()
                    for kc in range(KC):
                        MM(pA[:], oT[q][:, kc, :], wo0[:, kc, :], kc == 0, kc == KC - 1, [b_oT[q], wo0b], [pAb])
                    for kc in range(KC):
                        MM(pB[:], oT[q][:, kc, :], wo1[:, kc, :], kc == 0, kc == KC - 1, [b_oT[q], wo1b], [pBb])
                    postnorm(pA, pAb, pB, pBb, gp, gp_b, j, pws)
                release(5)
                P.barrier()

        def load_x(loc0, nt):
            for j in range(nt):
                DMA("pool", h[:, j, :], xloc[(loc0 + j) * 128:(loc0 + j + 1) * 128, :], [], [h_b[j]])

        STOP = int(os.environ.get("MK_STOP", "99"))
        pcs = {}
        for nm in in_names:
            pcs[nm] = acquire(nm)
        ncast_grp = -(-len(cast_q) // max(1, len(tok_groups(0, NPRE))))
        for (j0, n) in tok_groups(0, NPRE):
            if STOP < 1:
                break
            load_x(j0, n)
            cast_some(ncast_grp)
            layer0_mix(n, [(0, n)], True, pcs)
        out_ops = []
        mbase = 0
        for bi, nt in enumerate(BLOCKS):
            if STOP < 2:
                break
            load_x(NPRE + mbase, nt)
            if bi > 0:
                pcs = {nm: acquire(nm) for nm in in_names}
            if mbase == 0 and nt > 1:
                layer0_mix(nt, [(0, 1), (1, nt - 1)], False, pcs, reset_before=1)
            elif mbase == 1:
                layer0_mix(nt, [(0, nt)], False, pcs, reset_before=0)
            else:
                layer0_mix(nt, [(0, nt)], False, pcs)
            dbg_dump(0, mbase, nt)
            if STOP >= 3:
                xattn(0, nt)
            dbg_dump(1, mbase, nt)
            if STOP >= 4:
                mlp(0, nt)
            dbg_dump(2, mbase, nt)
            if STOP >= 5:
                swa(nt, mbase)
            dbg_dump(3, mbase, nt)
            if STOP >= 6:
                xattn(1, nt)
            dbg_dump(4, mbase, nt)
            if STOP >= 7:
                mlp(1, nt)
            dbg_dump(5, mbase, nt)
            for j in range(nt):
                m = mbase + j
                if m >= 1:
                    out_ops.append(DMA("pool", outd[(m - 1) * 128:m * 128, :], h[:, j, :], [h_b[j]], []))
            mbase += nt
        P.emit(out_ops)
        nc._prog_stats = P.stats
    return nc


def _host_consts():
    r = np.arange(128)[:, None]
    c = np.arange(128)[None, :]
    mprev = np.where(c > r, 0.0, NEG).astype(np.float32)
    inv = np.power(10000.0, -np.arange(32, dtype=np.float32) * 2.0 / 64).astype(np.float32)
    invf = np.broadcast_to(inv[None, :], (128, 32)).copy()
    return mprev, invf


def run_module(inputs, NOWN, NPRE, BLOCKS, dbg=False):
    x = np.asarray(inputs["x"], dtype=np.float32)
    B = x.shape[0]
    ncores = 2 * B
    S = x.shape[1]
    assert S == 2 * NOWN * 128 and NPRE == NOWN - 1 and sum(BLOCKS) == NOWN + 1
    mprev, invf = _host_consts()
    nc = build_nc(NPRE, BLOCKS, dbg=dbg)
    in_maps = []
    NLOC = NPRE + 1 + NOWN
    pos = np.asarray(inputs["positions"]).astype(np.int32)
    for c in range(ncores):
        b, s = c // 2, c % 2
        xl = np.zeros((NLOC * 128, D), np.float32)
        pl = np.zeros((NOWN + 1, 128), np.int32)
        if s == 1:
            xl[:] = x[b]
            pl[:] = pos[b].reshape(2 * NOWN, 128)[NOWN - 1:]
        else:
            xl[(NPRE + 1) * 128:] = x[b, :NOWN * 128]
            pl[1:] = pos[b].reshape(2 * NOWN, 128)[:NOWN]
        m = {"xloc": xl, "mem": np.ascontiguousarray(inputs["mem"][b], dtype=np.float32), "pos": pl,
             "flag": np.full((128, 1), float(s), np.float32),
             "mask_first": mprev if s == 1 else np.full((128, 128), NEG, np.float32),
             "invf": invf}
        for n_ in W_NAMES:
            m[n_] = np.ascontiguousarray(inputs[n_], dtype=np.float32)
        in_maps.append(m)
    res = run_bass_kernel_spmd(nc, in_maps, core_ids=list(range(ncores)))
    if os.environ.get("MK_VERBOSE"):
        print("exec_time_ns", getattr(res, "exec_time_ns", None), flush=True)
    out = np.zeros((B, S, D), np.float32)
    dbgs = None
    for c in range(ncores):
        b, s = c // 2, c % 2
        out[b, s * NOWN * 128:(s + 1) * NOWN * 128] = res.results[c]["out"]
    if dbg:
        dbgs = [res.results[c]["dbg"] for c in range(ncores)]
    return out, dbgs, nc


def kernel(**inputs):
    out, _, _ = run_module(inputs, 32, 31, [1, 4, 4, 4, 4, 4, 4, 4, 4])
    return out
```

```python
import math
import os
from contextlib import ExitStack

import numpy as np
import concourse.bass as bass
import concourse.mybir as mybir
from concourse.bass_utils import run_bass_kernel_spmd

F32 = mybir.dt.float32
BF16 = mybir.dt.bfloat16
I32 = mybir.dt.int32
AF = mybir.ActivationFunctionType
ALU = mybir.AluOpType
AX = mybir.AxisListType


class Buf:
    __slots__ = ("name", "w", "r")

    def __init__(self, name):
        self.name = name
        self.w = None
        self.r = []


class Op:
    __slots__ = ("eng", "fn", "deps", "is_dma", "sem", "cnt", "has_dep", "idx", "pre")

    def __init__(self, eng, fn, is_dma):
        self.eng = eng
        self.fn = fn
        self.deps = []
        self.is_dma = is_dma
        self.sem = None
        self.cnt = None
        self.has_dep = False
        self.pre = None


class Prog:
    ENGS = ("pe", "act", "dve", "pool", "sp")
    NDMA = {"sp": 12, "pool": 12, "act": 4}

    def __init__(self, nc):
        self.nc = nc
        self.streams = {e: [] for e in self.ENGS}
        self.nbuf = 0

    def buf(self, name=None):
        self.nbuf += 1
        return Buf(name or f"b{self.nbuf}")

    def bufs(self, n, name="b"):
        return [self.buf(f"{name}{i}") for i in range(n)]

    def _add(self, o, reads, writes):
        deps = []
        for b in reads:
            if b.w is not None:
                deps.append(("raw", b.w))
            b.r.append(o)
        for b in writes:
            if b.w is not None:
                deps.append(("waw", b.w))
            for r in b.r:
                if r is not o:
                    deps.append(("war", r))
            b.r = []
            b.w = o
        seen = set()
        for kind, d in deps:
            if d is o or id(d) in seen:
                continue
            if (not d.is_dma) and d.eng == o.eng and not o.is_dma:
                if kind != "raw" or o.eng == "pe":
                    continue
            seen.add(id(d))
            o.deps.append(d)
            d.has_dep = True
        self.streams[o.eng].append(o)
        return o

    def op(self, eng, fn, reads=(), writes=()):
        return self._add(Op(eng, fn, False), list(reads), list(writes))

    def dma(self, eng, fn, reads=(), writes=()):
        return self._add(Op(eng, fn, True), list(reads), list(writes))

    def pe(self, fn, reads=(), writes=()):
        return self.op("pe", fn, reads, writes)

    def act(self, fn, reads=(), writes=()):
        return self.op("act", fn, reads, writes)

    def dve(self, fn, reads=(), writes=()):
        return self.op("dve", fn, reads, writes)

    def pool(self, fn, reads=(), writes=()):
        return self.op("pool", fn, reads, writes)

    def barrier(self):
        lasts = []
        for e in ("pe", "act", "dve", "pool"):
            for o in reversed(self.streams[e]):
                if not o.is_dma and o.fn is not None:
                    lasts.append(o)
                    break
        for e in ("pe", "act", "dve", "pool"):
            w = Op(e, None, False)
            for d in lasts:
                w.deps.append(d)
                d.has_dep = True
            self.streams[e].append(w)

    def emit(self, final_deps=()):
        nc = self.nc
        es = ExitStack()
        with es:
            csem = {e: es.enter_context(nc.semaphore(f"c_{e}")) for e in ("pe", "act", "dve", "pool")}
            dsem = {q: [es.enter_context(nc.semaphore(f"d_{q}{i}")) for i in range(n)]
                    for q, n in self.NDMA.items()}
            fin = Op("pool", None, False)
            for d in final_deps:
                fin.deps.append(d)
                d.has_dep = True
            for e in self.ENGS:
                if e != "pool":
                    for last in reversed(self.streams[e]):
                        if last.fn is not None:
                            fin.deps.append(last)
                            last.has_dep = True
                            break
            self.streams["pool"].append(fin)
            ccount = {e: 0 for e in csem}
            dcount = {q: 0 for q in dsem}
            for e in self.ENGS:
                for o in self.streams[e]:
                    if o.is_dma:
                        j = dcount[e]
                        dcount[e] += 1
                        k = len(dsem[e])
                        o.sem = dsem[e][j % k]
                        o.cnt = 16 * (j // k + 1)
                        o.pre = (o.sem, 16 * (j // k)) if j >= k else None
                    elif o.has_dep:
                        ccount[e] += 1
                        o.sem = csem[e]
                        o.cnt = ccount[e]
            self.stats = {"ccount": ccount, "dcount": dcount,
                          "n": {e: len(self.streams[e]) for e in self.ENGS}}
            engobj = {"pe": "tensor", "act": "scalar", "dve": "vector", "pool": "gpsimd", "sp": "sync"}

            def run_stream(e, eng):
                waited = {}
                nwait = 0
                for o in self.streams[e]:
                    need = {}
                    if o.pre is not None:
                        need[o.pre[0]] = o.pre[1]
                    for d in o.deps:
                        assert d.sem is not None, "dep without semaphore"
                        if need.get(d.sem, 0) < d.cnt:
                            need[d.sem] = d.cnt
                    for s, c in need.items():
                        if waited.get(s, 0) < c:
                            eng.wait_ge(s, c)
                            waited[s] = c
                            nwait += 1
                    if o.fn is None:
                        continue
                    ins = o.fn(eng)
                    if o.sem is not None:
                        ins.then_inc(o.sem, 16 if o.is_dma else 1)
                self.stats.setdefault("nwait", {})[e] = nwait

            with nc.Block() as block:
                for e in self.ENGS:
                    if not self.streams[e]:
                        continue

                    def f(eng, e=e):
                        run_stream(e, eng)

                    getattr(block, engobj[e])(f)


D = 1024
KC = 8
EPS = 1e-6
NEG = -1.0e9
PI = math.pi

W_NAMES = ["ev_w_in", "ev_b_if", "ev_conv_w", "ev_conv_b", "ev_rg_wa", "ev_rg_ba", "ev_rg_wx", "ev_rg_bx",
           "ev_rg_lambda", "ev_w_out", "od_w_qkv", "od_sinks", "od_w_o", "g_mix_pre", "g_mix_post",
           "g_x_pre", "g_x_post", "g_mem", "w_xq", "w_xkv", "w_xo", "g_ff_pre", "g_ff_post", "w_ff1", "w_ff2"]
W_SHAPES = {"ev_w_in": [1, 1024, 3080], "ev_b_if": [1, 8], "ev_conv_w": [1, 4, 512], "ev_conv_b": [1, 512],
            "ev_rg_wa": [1, 4, 128, 128], "ev_rg_ba": [1, 512], "ev_rg_wx": [1, 4, 128, 128], "ev_rg_bx": [1, 512],
            "ev_rg_lambda": [1, 512], "ev_w_out": [1, 1024, 1024], "od_w_qkv": [1, 1024, 1536],
            "od_sinks": [1, 16], "od_w_o": [1, 1024, 1024], "g_mix_pre": [2, 1024], "g_mix_post": [2, 1024],
            "g_x_pre": [2, 1024], "g_x_post": [2, 1024], "g_mem": [2, 1024], "w_xq": [2, 1024, 1024],
            "w_xkv": [2, 1024, 2048], "w_xo": [2, 1024, 1024], "g_ff_pre": [2, 1024], "g_ff_post": [2, 1024],
            "w_ff1": [2, 1024, 4096], "w_ff2": [2, 4096, 1024]}


def build_nc(NPRE, BLOCKS, dbg=False):
    nc = bass.Bass("TRN2", target_bir_lowering=False)
    NMAIN = sum(BLOCKS)
    NOWN = NMAIN - 1
    NLOC = NPRE + NMAIN
    NTB = max(BLOCKS)
    TB = NTB * 128

    def din(name, shape, dt=F32):
        return nc.dram_tensor(name, list(shape), dt, kind="ExternalInput").ap()

    xloc = din("xloc", [NLOC * 128, D])
    mem = din("mem", [256, D])
    posd = din("pos", [NMAIN, 128], I32)
    flagd = din("flag", [128, 1])
    maskfd = din("mask_first", [128, 128])
    invfd = din("invf", [128, 32])
    Wd = {n: din(n, W_SHAPES[n]) for n in W_NAMES}
    outd = nc.dram_tensor("out", [NOWN * 128, D], F32, kind="ExternalOutput").ap()
    dbgd = nc.dram_tensor("dbg", [6, NMAIN * 128, D], F32, kind="ExternalOutput").ap() if dbg else None

    P = Prog(nc)
    es = ExitStack()
    with es:
        ncd = es.enter_context(nc.allow_non_contiguous_dma(reason="small parameter loads"))

        sbn = [0]

        def sb(name, shape, dt, st=es):
            sbn[0] += 1
            return st.enter_context(nc.sbuf_tensor(f"{name}_{sbn[0]}", list(shape), dt))

        def MM(out, lhsT, rhs, start, stop, r, w):
            return P.pe(lambda e: e.matmul(out, lhsT=lhsT, rhs=rhs, start=start, stop=stop), r, w)

        def ACTF(out, in_, func, r, w, **kw):
            return P.act(lambda e: e.activation(out=out, in_=in_, func=func, **kw), r, w)

        def TTo(eng, out, in0, in1, op, r, w):
            return P.op(eng, lambda e: e.tensor_tensor(out=out, in0=in0, in1=in1, op=op), r, w)

        def TSo(eng, out, in0, s1, s2, op0, op1, r, w):
            if s2 is None:
                return P.op(eng, lambda e: e.tensor_scalar(out=out, in0=in0, scalar1=s1, scalar2=None, op0=op0), r, w)
            return P.op(eng, lambda e: e.tensor_scalar(out=out, in0=in0, scalar1=s1, scalar2=s2, op0=op0, op1=op1), r, w)

        def STTo(out, in0, scalar, in1, op0, op1, r, w):
            return P.dve(lambda e: e.scalar_tensor_tensor(out=out, in0=in0, scalar=scalar, in1=in1, op0=op0, op1=op1), r, w)

        def CPo(eng, out, in_, r, w):
            if eng == "act":
                return P.act(lambda e: e.activation(out=out, in_=in_, func=AF.Copy), r, w)
            return P.op(eng, lambda e: e.tensor_copy(out=out, in_=in_), r, w)

        def MSET(eng, ap, val, w):
            return P.op(eng, lambda e: e.memset(ap, val), [], w)

        def DMA(q, out, in_, r, w):
            return P.dma(q, lambda e: e.dma_start(out=out, in_=in_), r, w)

        ps_t = [es.enter_context(nc.psum_tensor(f"ps{i}", [128, 512], F32)) for i in range(8)]
        ps_b = P.bufs(8, "ps")
        ps_i = [0]

        def PS():
            i = ps_i[0]
            ps_i[0] = (i + 1) % 6
            return ps_t[i], ps_b[i]

        b_c = P.buf("const")
        ident_f = sb("ident_f", [128, 128], F32)
        ident_bf = sb("ident_bf", [128, 128], BF16)
        ones4 = sb("ones4", [4, 128], F32)
        onesrow = sb("onesrow", [4, TB], F32)
        tri = sb("tri", [128, 128], F32)
        mask_pc = sb("mask_pc", [128, 256], F32)
        mask_fc = sb("mask_fc", [128, 256], F32)
        invf = sb("invf_sb", [128, 32], F32)
        flag = sb("flag_sb", [128, 1], F32)
        MSET("pool", ident_f[:], 0.0, [b_c])
        P.pool(lambda e: e.affine_select(out=ident_f[:], in_=ident_f[:], pattern=[[-1, 128]], compare_op=ALU.not_equal,
                                         fill=1.0, base=0, channel_multiplier=1), [b_c], [b_c])
        CPo("dve", ident_bf[:], ident_f[:], [b_c], [b_c])
        MSET("pool", ones4[:], 1.0, [b_c])
        MSET("pool", onesrow[:], 1.0, [b_c])
        MSET("pool", tri[:], 1.0, [b_c])
        P.pool(lambda e: e.affine_select(out=tri[:], in_=tri[:], pattern=[[1, 128]], compare_op=ALU.is_ge,
                                         fill=0.0, base=0, channel_multiplier=-1), [b_c], [b_c])
        MSET("pool", mask_pc[:], 0.0, [b_c])
        P.pool(lambda e: e.affine_select(out=mask_pc[:, 0:128], in_=mask_pc[:, 0:128], pattern=[[1, 128]],
                                         compare_op=ALU.is_gt, fill=NEG, base=0, channel_multiplier=-1), [b_c], [b_c])
        P.pool(lambda e: e.affine_select(out=mask_pc[:, 128:256], in_=mask_pc[:, 128:256], pattern=[[-1, 128]],
                                         compare_op=ALU.is_ge, fill=NEG, base=0, channel_multiplier=1), [b_c], [b_c])
        DMA("pool", mask_fc[:, 0:128], maskfd, [], [b_c])
        CPo("pool", mask_fc[:, 128:256], mask_pc[:, 128:256], [b_c], [b_c])
        DMA("pool", invf[:], invfd, [], [b_c])
        DMA("pool", flag[:], flagd, [], [b_c])

        bif = sb("bif", [4, 2], F32)
        DMA("pool", bif[:, 0:1], Wd["ev_b_if"][0, 0:4].rearrange("(p o) -> p o", o=1), [], [b_c])
        DMA("pool", bif[:, 1:2], Wd["ev_b_if"][0, 4:8].rearrange("(p o) -> p o", o=1), [], [b_c])
        TSo("dve", bif[:, 1:2], bif[:, 1:2], -1.0, None, ALU.mult, None, [b_c], [b_c])
        cw = sb("cw", [128, 4, 4], F32)
        for j in range(4):
            DMA("pool", cw[:, j, :], Wd["ev_conv_w"][0, j, :].rearrange("(n p) -> p n", p=128), [], [b_c])
        cb = sb("cb", [128, 4], F32)
        rba = sb("rba", [128, 4], F32)
        rbx = sb("rbx", [128, 4], F32)
        lam = sb("lam", [128, 4], F32)
        cl = sb("cl", [128, 8], F32)
        for t_, n_ in ((cb, "ev_conv_b"), (rba, "ev_rg_ba"), (rbx, "ev_rg_bx"), (lam, "ev_rg_lambda")):
            DMA("pool", t_[:], Wd[n_][0, :].rearrange("(n p) -> p n", p=128), [], [b_c])
        ACTF(lam[:], lam[:], AF.Exp, [b_c], [b_c], scale=-1.0)
        ACTF(lam[:], lam[:], AF.Ln, [b_c], [b_c], bias=1.0)
        TSo("dve", cl[:, 0:4], lam[:], -8.0, None, ALU.mult, None, [b_c], [b_c])
        TSo("dve", cl[:, 4:8], lam[:], -16.0, None, ALU.mult, None, [b_c], [b_c])
        sinks = sb("sinks", [128, 16], F32)
        DMA("pool", sinks[:], Wd["od_sinks"][0:1, :].to_broadcast([128, 16]), [], [b_c])
        posi = sb("posi", [128, NMAIN], I32)
        posf = sb("posf", [128, NMAIN], F32)
        DMA("pool", posi[:], posd.rearrange("m p -> p m"), [], [b_c])
        CPo("dve", posf[:], posi[:], [b_c], [b_c])
        wgif = sb("wgif", [128, 8, 8], BF16)
        DMA("pool", wgif[:], Wd["ev_w_in"][0, :, 2048:2056].rearrange("(k p) n -> p k n", p=128), [], [b_c])
        wrg = sb("wrg", [128, 2, 4, 128], BF16)
        DMA("pool", wrg[:, 0], Wd["ev_rg_wa"][0].rearrange("n c d -> c n d"), [], [b_c])
        DMA("pool", wrg[:, 1], Wd["ev_rg_wx"][0].rearrange("n c d -> c n d"), [], [b_c])

        pieces = {}
        cast_q = []
        cast_done = {}
        cast_pos = [0]

        def cast_some(n):
            while n > 0 and cast_pos[0] < len(cast_q):
                name, t, src3, b = cast_q[cast_pos[0]]
                cast_pos[0] += 1
                DMA("pool", t, src3, [], [b])
                cast_done[name] = True
                n -= 1

        def mk_piece(name, src3):
            kk = src3.shape[1]
            nn = src3.shape[2]
            t = nc.dram_tensor("wsc_" + name, [128, kk, nn], BF16, kind="Internal").ap()
            b = P.buf("wsc_" + name)
            cast_q.append((name, t, src3, b))
            pieces[name] = (t, b, kk, nn)

        def std_pieces(prefix, w2d, ncols):
            names = []
            for c in range(ncols // 512):
                nm = f"{prefix}_{c}"
                mk_piece(nm, w2d[:, c * 512:(c + 1) * 512].rearrange("(k p) n -> p k n", p=128))
                names.append(nm)
            return names

        def mlp_pieces(l):
            names = []
            for half in range(2):
                for c in range(4 * half, 4 * half + 4):
                    nm = f"ff1_{l}_{c}"
                    mk_piece(nm, Wd["w_ff1"][l][:, c * 512:(c + 1) * 512].rearrange("(k p) n -> p k n", p=128))
                    names.append(nm)
                for c in range(4 * half, 4 * half + 4):
                    nm = f"ff2_{l}_{c}"
                    mk_piece(nm, Wd["w_ff2"][l][c * 512:(c + 1) * 512, :].rearrange("(k p) n -> p k n", p=128))
                    names.append(nm)
            return names

        seq = []
        xkv_names = []
        for l in range(2):
            xkv_names.append(std_pieces(f"xkv{l}", Wd["w_xkv"][l], 2048))
        win = Wd["ev_w_in"][0]
        in_names = []
        for nm, c0 in (("in_q", 0), ("in_k", 512), ("in_v", 1024), ("in_o", 1536), ("in_rx", 2056), ("in_ry", 2568)):
            mk_piece(nm, win[:, c0:c0 + 512].rearrange("(k p) n -> p k n", p=128))
            in_names.append(nm)
        L0 = list(in_names)
        L0 += std_pieces("out", Wd["ev_w_out"][0], 1024)
        L0 += std_pieces("xq0", Wd["w_xq"][0], 1024)
        L0 += std_pieces("xo0", Wd["w_xo"][0], 1024)
        L0 += mlp_pieces(0)
        L1 = std_pieces("qkv", Wd["od_w_qkv"][0], 1536)
        L1 += std_pieces("o1", Wd["od_w_o"][0], 1024)
        L1 += std_pieces("xq1", Wd["w_xq"][1], 1024)
        L1 += std_pieces("xo1", Wd["w_xo"][1], 1024)
        L1 += mlp_pieces(1)
        seq = xkv_names[0] + xkv_names[1]
        for _ in BLOCKS:
            seq += L0 + L1

        NSLOT = 8
        slot_t = [sb(f"slot{i}", [128, 4096], BF16) for i in range(NSLOT)]
        slot_b = P.bufs(NSLOT, "slot")
        ring = {"use": 0, "done": 0, "loaded": 0}

        def ring_load():
            i = ring["loaded"]
            if i >= len(seq):
                return
            t, b, kk, nn = pieces[seq[i]]
            while seq[i] not in cast_done:
                cast_some(1)
            s = i % NSLOT
            DMA("sp", slot_t[s][:].rearrange("p (k n) -> p k n", k=kk), t, [b], [slot_b[s]])
            ring["loaded"] += 1

        def acquire(expect=None):
            i = ring["use"]
            ring["use"] += 1
            assert i < ring["loaded"], "ring underflow"
            if expect is not None and not os.environ.get("MK_STOP"):
                assert seq[i].startswith(expect), (seq[i], expect)
            t, b, kk, nn = pieces[seq[i]]
            s = i % NSLOT
            return slot_t[s][:].rearrange("p (k n) -> p k n", k=kk), slot_b[s]

        def release(n=1):
            for _ in range(n):
                ring["done"] += 1
                ring_load()

        for _ in range(NSLOT):
            ring_load()

        h = sb("h", [128, NTB, D], F32)
        h_b = P.bufs(NTB, "h")
        grep = [sb(f"grep{i}", [128, D], F32) for i in range(2)]
        grep_b = P.bufs(2, "grep")
        gcnt = [0]

        def gload(name, l):
            i = gcnt[0] % 2
            gcnt[0] += 1
            DMA("pool", grep[i][:], Wd[name][l:l + 1, :].to_broadcast([128, D]), [], [grep_b[i]])
            return grep[i], grep_b[i]

        KT = sb("KT", [128, 2, 8, 256], BF16)
        VM = sb("VM", [128, 2, 2, D], BF16)
        b_kv = P.buf("kvmem")
        Cst = sb("Cst", [128, 4, 129], F32)
        b_C = P.buf("C")
        gcar = sb("gcar", [4, 2], F32)
        b_gc = P.buf("gcar")
        hst = sb("hst", [128, 4], F32)
        rxhist = sb("rxhist", [128, 4, 3], F32)
        b_rgs = P.bufs(4, "rgst")
        MSET("dve", Cst[:], 0.0, [b_C])
        MSET("dve", gcar[:], 0.0, [b_gc])
        MSET("dve", hst[:], 0.0, b_rgs)
        MSET("dve", rxhist[:], 0.0, b_rgs)
        kTs = sb("kTs", [128, 2, 8, 128], BF16)
        vs = sb("vs", [128, 2, 256], BF16)
        kv_b = P.bufs(2, "swakv")
        MSET("dve", kTs[:], 0.0, [kv_b[0], kv_b[1]])
        MSET("dve", vs[:], 0.0, [kv_b[0], kv_b[1]])

        def prenorm(src_fn, src_bufs, nt, g, g_b, uT, uT_b, ws, tag):
            ssq = sb(f"pn_ssq{tag}", [128, NTB], F32, ws)
            junk = sb(f"pn_junk{tag}", [128, D], BF16, ws)
            ubf = [sb(f"pn_u{tag}{i}", [128, D], BF16, ws) for i in range(2)]
            b_ssq = P.bufs(nt, "ssq")
            b_junk = P.buf()
            b_u = P.bufs(2, "u")
            def pn_tile(j):
                src = src_fn(j)
                ACTF(junk[:], src, AF.Square, [src_bufs[j]], [b_junk, b_ssq[j]], accum_out=ssq[:, j:j + 1])
                ACTF(ssq[:, j:j + 1], ssq[:, j:j + 1], AF.Sqrt, [b_ssq[j]], [b_ssq[j]], scale=1.0 / D, bias=EPS)
                yield
                P.dve(lambda e, j=j: e.reciprocal(out=ssq[:, j:j + 1], in_=ssq[:, j:j + 1]), [b_ssq[j]], [b_ssq[j]])
                u = ubf[j % 2]
                STTo(u[:], src, ssq[:, j:j + 1], g[:], ALU.mult, ALU.mult, [src_bufs[j], b_ssq[j], g_b], [b_u[j % 2]])
                yield
                pt, pb = PS()
                ptb = pt[:].bitcast(BF16)
                for kc in range(KC):
                    P.pe(lambda e, kc=kc, u=u, ptb=ptb: e.transpose(out=ptb[:, kc * 128:(kc + 1) * 128],
                                                                   in_=u[:, kc * 128:(kc + 1) * 128],
                                                                   identity=ident_bf[:]),
                         [b_u[j % 2], b_c], [pb])
                yield
                CPo("act", uT[:, :, j * 128:(j + 1) * 128], ptb.rearrange("p (k t) -> p k t", k=KC), [pb], [uT_b[j]])

            for j0_ in range(0, nt, 2):
                interleave([pn_tile(j_) for j_ in range(j0_, min(nt, j0_ + 2))])

        def postnorm(psA, pbA, psB, pbB, g, g_b, j, ws_t):
            ss2, tmp, junk = ws_t["ss2"], ws_t["tmp"], ws_t["junk"]
            k = ws_t["cnt"] % 2
            ws_t["cnt"] += 1
            b_ss, b_tmp, b_jk = ws_t["b_ss"][k], ws_t["b_tmp"][k], ws_t["b_jk"]
            s2 = ss2[:, 4 * k:4 * k + 4]
            ACTF(junk[:, 0:512], psA[:], AF.Square, [pbA], [b_jk, b_ss], accum_out=s2[:, 0:1])
            ACTF(junk[:, 512:1024], psB[:], AF.Square, [pbB], [b_jk, b_ss], accum_out=s2[:, 1:2])
            TTo("dve", s2[:, 2:3], s2[:, 0:1], s2[:, 1:2], ALU.add, [b_ss], [b_ss])
            ACTF(s2[:, 2:3], s2[:, 2:3], AF.Sqrt, [b_ss], [b_ss], scale=1.0 / D, bias=EPS)
            P.dve(lambda e: e.reciprocal(out=s2[:, 3:4], in_=s2[:, 2:3]), [b_ss], [b_ss])
            tm = tmp[k]
            STTo(tm[:, 0:512], psA[:], s2[:, 3:4], g[:, 0:512], ALU.mult, ALU.mult, [pbA, b_ss, g_b], [b_tmp])
            STTo(tm[:, 512:1024], psB[:], s2[:, 3:4], g[:, 512:1024], ALU.mult, ALU.mult, [pbB, b_ss, g_b], [b_tmp])
            TTo("pool", h[:, j, :], h[:, j, :], tm[:], ALU.add, [h_b[j], b_tmp], [h_b[j]])

        def mk_post_ws(ws, tag):
            return {"ss2": sb(f"po_ss{tag}", [128, 8], F32, ws), "junk": sb(f"po_junk{tag}", [128, D], BF16, ws),
                    "tmp": [sb(f"po_tmp{tag}0", [128, D], F32, ws)] * 2,
                    "cnt": 0, "b_ss": P.bufs(2, "poss"), "b_tmp": [P.buf("potmp")] * 2, "b_jk": P.buf()}

        def proj_tm(uT, uT_b, j, wv, wb, c0, ncol):
            pt, pb = PS()
            for kc in range(KC):
                MM(pt[:, 0:ncol], uT[:, kc, j * 128:(j + 1) * 128], wv[:, kc, c0:c0 + ncol], kc == 0, kc == KC - 1,
                   [uT_b[j], wb], [pb])
            return pt, pb

        def tok_groups(t0, nt):
            out = []
            j = t0
            while j < t0 + nt:
                n = min(4, t0 + nt - j)
                out.append((j, n))
                j += n
            return out

        def proj_fm(uT, uT_b, t0, nt, wv, wb, c0, evac):
            for (j0, n) in tok_groups(t0, nt):
                pt, pb = PS()
                for kc in range(KC):
                    MM(pt[:, 0:n * 128], wv[:, kc, c0:c0 + 128], uT[:, kc, j0 * 128:(j0 + n) * 128], kc == 0, kc == KC - 1,
                       [uT_b[j] for j in range(j0, j0 + n)] + [wb], [pb])
                evac(pt, pb, j0, n)

        def interleave(gens):
            gens = list(gens)
            while gens:
                for g_ in list(gens):
                    try:
                        next(g_)
                    except StopIteration:
                        gens.remove(g_)

        def dbg_dump(k, mbase, nt):
            if dbgd is None:
                return
            for j in range(nt):
                m = mbase + j
                DMA("pool", dbgd[k, m * 128:(m + 1) * 128, :], h[:, j, :], [h_b[j]], [])

        with ExitStack() as ws:
            memsb = sb("memsb", [128, 2, D], F32, ws)
            b_mem = P.bufs(2, "mem")
            for j in range(2):
                DMA("pool", memsb[:, j, :], mem[j * 128:(j + 1) * 128, :], [], [b_mem[j]])
            memT = sb("memT", [128, KC, 256], BF16, ws)
            b_memT = P.bufs(2, "memT")
            for l in range(2):
                g, g_b = gload("g_mem", l)
                prenorm(lambda j: memsb[:, j, :], b_mem, 2, g, g_b, memT, b_memT, ws, f"m{l}")
                for pc in range(2):
                    wv, wb = acquire(f"xkv{l}")
                    for c in range(4):
                        pt, pb = PS()
                        for kc in range(KC):
                            MM(pt[:, 0:256], wv[:, kc, c * 128:(c + 1) * 128], memT[:, kc, :], kc == 0, kc == KC - 1,
                               b_memT + [wb], [pb])
                        CPo("act", KT[:, l, pc * 4 + c, :], pt[:, 0:256], [pb], [b_kv])
                    release()
                for pc in range(2):
                    wv, wb = acquire(f"xkv{l}")
                    for j in range(2):
                        pt, pb = proj_tm(memT, b_memT, j, wv, wb, 0, 512)
                        CPo("dve", VM[:, l, j, pc * 512:(pc + 1) * 512], pt[:], [pb], [b_kv])
                    release()
            P.barrier()

        def layer0_mix(nt, ranges, state_only, pcs, reset_before=None):
            with ExitStack() as ws:
                g, g_b = gload("g_mix_pre", 0)
                if not state_only:
                    gp, gp_b = gload("g_mix_post", 0)
                uT = sb("uT", [128, KC, TB], BF16, ws)
                uT_b = P.bufs(nt, "uT")
                prenorm(lambda j: h[:, j, :], h_b, nt, g, g_b, uT, uT_b, ws, "a")
                g1 = sb("g1", [4, TB], F32, ws)
                g2 = sb("g2", [4, TB], F32, ws)
                g3 = sb("g3", [4, TB], F32, ws)
                gsm = sb("gsm", [4, 4 * NTB + 8 + 4 * NTB], F32, ws)
                tmx = gsm[:, 0:NTB]
                MgE = gsm[:, NTB:2 * NTB + 1]
                wpv = gsm[:, 2 * NTB + 4:3 * NTB + 4]
                wpd = gsm[:, 3 * NTB + 8:3 * NTB + 8 + 4 * NTB]
                gtm = sb("gtm", [128, NTB, 8], F32, ws)
                wpr = sb("wpr", [128, NTB, 4], F32, ws)
                b_g = P.buf("gates")
                k_tm = sb("k_tm", [128, NTB, 512], BF16, ws)
                v_aug = sb("v_aug", [128, NTB, 4, 129], BF16, ws)
                b_kt = P.bufs(nt, "ktm")
                b_va = P.bufs(nt, "vaug")
                MSET("pool", v_aug[:, :, :, 128:129], 1.0, b_va)
                ktl = [sb(f"ktl{i}", [128, 4, 128], BF16, ws) for i in range(2)]
                b_ktl = P.bufs(2, "ktl")
                if not state_only:
                    qT = sb("qT", [128, 4, TB], BF16, ws)
                    kT = sb("kT", [128, 4, TB], BF16, ws)
                    b_qT = P.bufs(nt, "qT")
                    b_kT = P.bufs(nt, "kT")
                    sig_o = sb("sig_o", [128, NTB, 512], BF16, ws)
                    b_so = P.bufs(nt, "sigo")
                    yT = sb("yT", [128, KC, TB], BF16, ws)
                    b_yTa = P.bufs(nt, "yTa")
                    b_yTb = P.bufs(nt, "yTb")
                    Cbf = sb("Cbf", [128, 4, 129], BF16, ws)
                    b_Cbf = P.buf("Cbf")
                    ptl = [sb(f"ptl{i}", [128, 4, 128], BF16, ws) for i in range(2)]
                    b_ptl = P.bufs(2, "ptl")
                    yal = [sb(f"yal{i}", [128, 512], BF16, ws) for i in range(2)]
                    b_yal = P.bufs(2, "yal")
                    dn = sb("dn", [128, 8], F32, ws)
                    b_dn = P.bufs(2, "dn")
                rgs = []
                for i_ in range(2):
                    rgs.append(dict(rxn=sb("rxn", [128, TB + 3], F32, ws), xc=sb("xc", [128, TB], F32, ws),
                                    xcb=sb("xcb", [128, TB], BF16, ws), rr=sb("rr", [128, TB], F32, ws),
                                    ii=sb("ii", [128, TB], F32, ws), a2=sb("a2", [128, TB], F32, ws),
                                    hs=sb("hs", [128, TB], F32, ws), b=P.bufs(7, "rgw")))

                for ri, (t0, n) in enumerate(ranges):
                    if reset_before is not None and ri == reset_before:
                        TSo("dve", Cst[:], Cst[:], flag[:, 0:1], None, ALU.mult, None, [b_C, b_c], [b_C])
                        TSo("dve", gcar[:], gcar[:], flag[0:4, 0:1], None, ALU.mult, None, [b_gc, b_c], [b_gc])
                        TSo("dve", hst[:], hst[:], flag[:, 0:1], None, ALU.mult, None, b_rgs + [b_c], b_rgs)
                        TSo("dve", rxhist[:].rearrange("p a b -> p (a b)"), rxhist[:].rearrange("p a b -> p (a b)"),
                            flag[:, 0:1], None, ALU.mult, None, b_rgs + [b_c], b_rgs)
                    T_ = n * 128
                    c0 = t0 * 128
                    R = slice(c0, c0 + T_)
                    ub = [uT_b[j] for j in range(t0, t0 + n)]
                    wq, wqb = pcs["in_q"]
                    wk_, wkb = pcs["in_k"]
                    wv_, wvb = pcs["in_v"]
                    wo_, wob = pcs["in_o"]
                    wrx, wrxb = pcs["in_rx"]
                    wry, wryb = pcs["in_ry"]
                    for (j0, gn) in tok_groups(t0, n):
                        cs = slice(j0 * 128, (j0 + gn) * 128)
                        gub = [uT_b[j] for j in range(j0, j0 + gn)]
                        pti, pbi = PS()
                        for kc in range(KC):
                            MM(pti[0:4, 0:gn * 128], wgif[:, kc, 0:4], uT[:, kc, cs], kc == 0, kc == KC - 1, gub + [b_c], [pbi])
                        ACTF(g1[:, cs], pti[0:4, 0:gn * 128], AF.Identity, [pbi, b_c], [b_g], bias=bif[:, 0:1])
                        ptf, pbf = PS()
                        for kc in range(KC):
                            MM(ptf[0:4, 0:gn * 128], wgif[:, kc, 4:8], uT[:, kc, cs], kc == 0, kc == KC - 1, gub + [b_c], [pbf])
                        ACTF(g2[:, cs], ptf[0:4, 0:gn * 128], AF.Exp, [pbf, b_c], [b_g], scale=-1.0, bias=bif[:, 1:2])
                    ACTF(g2[:, R], g2[:, R], AF.Ln, [b_g], [b_g], bias=1.0)
                    P.dve(lambda e, R=R, T_=T_: e.tensor_tensor_scan(out=g3[:, R], data0=onesrow[:, 0:T_], data1=g2[:, R],
                                                                     initial=gcar[:, 0:1], op0=ALU.mult, op1=ALU.subtract),
                          [b_g, b_gc, b_c], [b_g])
                    CPo("dve", gcar[:, 0:1], g3[:, c0 + T_ - 1:c0 + T_], [b_g], [b_gc])
                    TTo("dve", g1[:, R], g1[:, R], g3[:, R], ALU.subtract, [b_g], [b_g])
                    P.dve(lambda e, R=R, n=n: e.tensor_reduce(out=tmx[:, 0:n], in_=g1[:, R].rearrange("p (j t) -> p j t", t=128),
                                                              axis=AX.X, op=ALU.max), [b_g], [b_g])
                    CPo("dve", MgE[:, 0:1], gcar[:, 1:2], [b_gc], [b_g])
                    P.dve(lambda e, n=n: e.tensor_tensor_scan(out=MgE[:, 1:n + 1], data0=onesrow[:, 0:n], data1=tmx[:, 0:n],
                                                              initial=MgE[:, 0:1], op0=ALU.mult, op1=ALU.max),
                          [b_g, b_c], [b_g])
                    CPo("dve", gcar[:, 1:2], MgE[:, n:n + 1], [b_g], [b_gc])
                    TTo("dve", wpv[:, 0:n], MgE[:, 0:n], MgE[:, 1:n + 1], ALU.subtract, [b_g], [b_g])
                    ACTF(wpv[:, 0:n], wpv[:, 0:n], AF.Exp, [b_g], [b_g])
                    Mb = MgE[:, 1:n + 1].unsqueeze(2).to_broadcast([4, n, 128])
                    g1v = g1[:, R].rearrange("p (j t) -> p j t", t=128)
                    g2v = g2[:, R].rearrange("p (j t) -> p j t", t=128)
                    g3v = g3[:, R].rearrange("p (j t) -> p j t", t=128)
                    TTo("dve", g2v, g1v, Mb, ALU.subtract, [b_g], [b_g])
                    ACTF(g2[:, R], g2[:, R], AF.Exp, [b_g], [b_g], bias=math.log(128 ** -0.5))
                    TTo("dve", g3v, g3v, Mb, ALU.add, [b_g], [b_g])
                    ACTF(g3[:, R], g3[:, R], AF.Exp, [b_g], [b_g], scale=-1.0)
                    if not state_only:
                        for c in range(4):
                            proj_fm(uT, uT_b, t0, n, wq, wqb, c * 128,
                                    lambda pt, pb, j0, gn, c=c: CPo("act", qT[:, c, j0 * 128:(j0 + gn) * 128], pt[:, 0:gn * 128],
                                                                    [pb], [b_qT[j] for j in range(j0, j0 + gn)]))
                            proj_fm(uT, uT_b, t0, n, wk_, wkb, c * 128,
                                    lambda pt, pb, j0, gn, c=c: CPo("dve", kT[:, c, j0 * 128:(j0 + gn) * 128], pt[:, 0:gn * 128],
                                                                    [pb], [b_kT[j] for j in range(j0, j0 + gn)]))
                    for j in range(t0, t0 + n):
                        pt, pb = proj_tm(uT, uT_b, j, wk_, wkb, 0, 512)
                        CPo("act", k_tm[:, j, :], pt[:], [pb], [b_kt[j]])
                        pt, pb = proj_tm(uT, uT_b, j, wv_, wvb, 0, 512)
                        CPo("dve", v_aug[:, j, :, 0:128], pt[:].rearrange("p (h d) -> p h d", h=4), [pb], [b_va[j]])
                        if not state_only:
                            pt, pb = proj_tm(uT, uT_b, j, wo_, wob, 0, 512)
                            ACTF(sig_o[:, j, :], pt[:], AF.Sigmoid, [pb], [b_so[j]])

                    wpdv = wpd[:, 0:4 * n].rearrange("p (j h) -> p j h", h=4)
                    TTo("dve", wpdv, wpv[:, 0:n].unsqueeze(2).to_broadcast([4, n, 4]),
                        ident_f[0:4, 0:4].unsqueeze(1).to_broadcast([4, n, 4]), ALU.mult, [b_g, b_c], [b_g])
                    pt, pb = PS()
                    MM(pt[:, 0:4 * n], ones4[:], wpd[:, 0:4 * n], True, True, [b_g, b_c], [pb])
                    CPo("dve", wpr[:, t0:t0 + n, :], pt[:, 0:4 * n].rearrange("p (j h) -> p j h", h=4), [pb], [b_g])
                    pt, pb = PS()
                    for jj in range(n):
                        cj = slice(c0 + jj * 128, c0 + (jj + 1) * 128)
                        MM(pt[:, jj * 8:jj * 8 + 4], g2[:, cj], ident_f[0:4, 0:4], True, True, [b_g, b_c], [pb])
                        MM(pt[:, jj * 8 + 4:jj * 8 + 8], g3[:, cj], ident_f[0:4, 0:4], True, True, [b_g, b_c], [pb])
                    CPo("dve", gtm[:, t0:t0 + n, :], pt[:, 0:8 * n].rearrange("p (j h) -> p j h", h=8), [pb], [b_g])

                    for j in range(t0, t0 + n):
                        cj = slice(j * 128, (j + 1) * 128)
                        wk4 = gtm[:, j, 0:4]
                        kt = ktl[j % 2]
                        TTo("pool", kt[:], k_tm[:, j, :].rearrange("p (h d) -> p h d", h=4),
                            wk4.unsqueeze(2).to_broadcast([128, 4, 128]), ALU.mult, [b_kt[j], b_g], [b_ktl[j % 2]])
                        TTo("dve", Cst[:], Cst[:], wpr[:, j, :].unsqueeze(2).to_broadcast([128, 4, 129]), ALU.mult,
                            [b_C, b_g], [b_C])
                        if not state_only:
                            CPo("act", Cbf[:], Cst[:], [b_C], [b_Cbf])
                            pss, pssb = PS()
                            for h_ in range(4):
                                MM(pss[:, h_ * 128:(h_ + 1) * 128], kT[:, h_, cj], qT[:, h_, cj], True, True,
                                   [b_kT[j], b_qT[j]], [pssb])
                            ptb_ = ptl[j % 2]
                            for h_ in range(4):
                                STTo(ptb_[:, h_, :], pss[:, h_ * 128:(h_ + 1) * 128], gtm[:, j, h_:h_ + 1], tri[:],
                                     ALU.mult, ALU.mult, [pssb, b_g, b_c], [b_ptl[j % 2]])
                            psn = [PS(), PS()]
                            for h_ in range(4):
                                pn_, pnb = psn[h_ // 2]
                                reg = pn_[:, (h_ % 2) * 129:(h_ % 2) * 129 + 129]
                                MM(reg, ptb_[:, h_, :], v_aug[:, j, h_, :], True, False, [b_ptl[j % 2], b_va[j]], [pnb])
                                MM(reg, qT[:, h_, cj], Cbf[:, h_, :], False, True, [b_qT[j], b_Cbf], [pnb])
                            d_ = dn[:, 4 * (j % 2):4 * (j % 2) + 4]
                            for bk in range(2):
                                pn_, pnb = psn[bk]
                                ACTF(d_[:, 2 * bk:2 * bk + 2], pn_[:, 0:258].rearrange("p (h e) -> p h e", e=129)[:, :, 128],
                                     AF.Abs, [pnb], [b_dn[j % 2]])
                            TTo("dve", d_, d_, gtm[:, j, 4:8], ALU.max, [b_dn[j % 2], b_g], [b_dn[j % 2]])
                            P.dve(lambda e, d_=d_: e.reciprocal(out=d_, in_=d_), [b_dn[j % 2]], [b_dn[j % 2]])
                            ya = yal[j % 2]
                            for h_ in range(4):
                                pn_, pnb = psn[h_ // 2]
                                STTo(ya[:, h_ * 128:(h_ + 1) * 128], pn_[:, (h_ % 2) * 129:(h_ % 2) * 129 + 128], d_[:, h_:h_ + 1],
                                     sig_o[:, j, h_ * 128:(h_ + 1) * 128], ALU.mult, ALU.mult,
                                     [pnb, b_dn[j % 2], b_so[j]], [b_yal[j % 2]])
                            pt, pb = PS()
                            ptb16 = pt[:].bitcast(BF16)
                            for h_ in range(4):
                                P.pe(lambda e, h_=h_, ya=ya, ptb16=ptb16: e.transpose(out=ptb16[:, h_ * 128:(h_ + 1) * 128],
                                                                                      in_=ya[:, h_ * 128:(h_ + 1) * 128],
                                                                                      identity=ident_bf[:]),
                                     [b_yal[j % 2], b_c], [pb])
                            CPo("act", yT[:, 0:4, cj], ptb16[:, 0:512].rearrange("p (h t) -> p h t", h=4), [pb], [b_yTa[j]])
                        psk = [PS(), PS()]
                        for h_ in range(4):
                            pk_, pkb = psk[h_ // 2]
                            MM(pk_[:, (h_ % 2) * 129:(h_ % 2) * 129 + 129], kt[:, h_, :], v_aug[:, j, h_, :], True, True,
                               [b_ktl[j % 2], b_va[j]], [pkb])
                        for bk in range(2):
                            pk_, pkb = psk[bk]
                            TTo("dve", Cst[:, 2 * bk:2 * bk + 2, :], Cst[:, 2 * bk:2 * bk + 2, :],
                                pk_[:, 0:258].rearrange("p (h e) -> p h e", e=129), ALU.add, [pkb, b_C], [b_C])

                    def rg_chain(n_):
                        rg_ = rgs[n_ % 2]
                        b_rg = b_rgs[n_]
                        rxn, xc, xcb, rr, ii, a2, hs = (rg_["rxn"], rg_["xc"], rg_["xcb"], rg_["rr"], rg_["ii"], rg_["a2"], rg_["hs"])
                        b_rxn, b_xc, b_xcb, b_rr, b_ii, b_a2, b_hs = rg_["b"]
                        proj_fm(uT, uT_b, t0, n, wrx, wrxb, n_ * 128,
                                lambda pt, pb, j0, gn: CPo("act", rxn[:, 3 + (j0 - t0) * 128:3 + (j0 - t0 + gn) * 128],
                                                           pt[:, 0:gn * 128], [pb], [b_rxn]))
                        CPo("pool", rxn[:, 0:3], rxhist[:, n_, :], [b_rg], [b_rxn])
                        yield
                        TSo("dve", xc[:, 0:T_], rxn[:, 0:T_], cw[:, 0, n_:n_ + 1], cb[:, n_:n_ + 1], ALU.mult, ALU.add,
                            [b_rxn, b_c], [b_xc])
                        for jj in range(1, 4):
                            STTo(xc[:, 0:T_], rxn[:, jj:jj + T_], cw[:, jj, n_:n_ + 1], xc[:, 0:T_], ALU.mult, ALU.add,
                                 [b_rxn, b_c, b_xc], [b_xc])
                        CPo("pool", rxhist[:, n_, :], rxn[:, T_:T_ + 3], [b_rxn], [b_rg])
                        CPo("pool", xcb[:, 0:T_], xc[:, 0:T_], [b_xc], [b_xcb])
                        yield
                        for (j0, gn) in tok_groups(0, n):
                            cs = slice(j0 * 128, (j0 + gn) * 128)
                            pr, prb = PS()
                            MM(pr[:, 0:gn * 128], wrg[:, 0, n_, :], xcb[:, cs], True, True, [b_xcb, b_c], [prb])
                            ACTF(rr[:, cs], pr[:, 0:gn * 128], AF.Sigmoid, [prb, b_c], [b_rr], bias=rba[:, n_:n_ + 1])
                            pi_, pib = PS()
                            MM(pi_[:, 0:gn * 128], wrg[:, 1, n_, :], xcb[:, cs], True, True, [b_xcb, b_c], [pib])
                            ACTF(ii[:, cs], pi_[:, 0:gn * 128], AF.Sigmoid, [pib, b_c], [b_ii], bias=rbx[:, n_:n_ + 1])
                        yield
                        ACTF(a2[:, 0:T_], rr[:, 0:T_], AF.Exp, [b_rr, b_c], [b_a2], scale=cl[:, 4 + n_:5 + n_])
                        ACTF(rr[:, 0:T_], rr[:, 0:T_], AF.Exp, [b_rr, b_c], [b_rr], scale=cl[:, n_:n_ + 1])
                        TSo("dve", a2[:, 0:T_], a2[:, 0:T_], 1.0, None, ALU.min, None, [b_a2], [b_a2])
                        ACTF(a2[:, 0:T_], a2[:, 0:T_], AF.Sqrt, [b_a2], [b_a2], scale=-1.0, bias=1.0)
                        yield
                        TTo("pool", ii[:, 0:T_], ii[:, 0:T_], xc[:, 0:T_], ALU.mult, [b_ii, b_xc], [b_ii])
                        TTo("pool", a2[:, 0:T_], a2[:, 0:T_], ii[:, 0:T_], ALU.mult, [b_a2, b_ii], [b_a2])
                        yield
                        P.dve(lambda e, n_=n_, T_=T_, hs=hs, rr=rr, a2=a2: e.tensor_tensor_scan(out=hs[:, 0:T_], data0=rr[:, 0:T_], data1=a2[:, 0:T_],
                                                                           initial=hst[:, n_:n_ + 1], op0=ALU.mult, op1=ALU.add),
                              [b_rr, b_a2, b_rg], [b_hs])
                        CPo("pool", hst[:, n_:n_ + 1], hs[:, T_ - 1:T_], [b_hs], [b_rg])
                        yield
                        if not state_only:
                            proj_fm(uT, uT_b, t0, n, wry, wryb, n_ * 128,
                                    lambda pt, pb, j0, gn: ACTF(yT[:, 4 + n_, j0 * 128:(j0 + gn) * 128], pt[:, 0:gn * 128],
                                                                AF.Gelu_apprx_tanh, [pb],
                                                                [b_yTb[j] for j in range(j0, j0 + gn)]))
                            ybs = [b_yTb[j] for j in range(t0, t0 + n)]
                            TTo("dve", yT[:, 4 + n_, R], yT[:, 4 + n_, R], hs[:, 0:T_], ALU.mult, ybs + [b_hs], ybs)
                    for pair in ((0, 1), (2, 3)):
                        interleave([rg_chain(a_) for a_ in pair])
                if not state_only:
                    release(6)
                    wo0, wo0b = acquire("out")
                    wo1, wo1b = acquire("out")
                    pws = mk_post_ws(ws, "a")
                    for j in range(nt):
                        yb_ = [b_yTa[j], b_yTb[j]]
                        pA, pAb = PS()
                        pB, pBb = PS()
                        for kc in range(KC):
                            MM(pA[:], yT[:, kc, j * 128:(j + 1) * 128], wo0[:, kc, :], kc == 0, kc == KC - 1, yb_ + [wo0b], [pAb])
                        for kc in range(KC):
                            MM(pB[:], yT[:, kc, j * 128:(j + 1) * 128], wo1[:, kc, :], kc == 0, kc == KC - 1, yb_ + [wo1b], [pBb])
                        postnorm(pA, pAb, pB, pBb, gp, gp_b, j, pws)
                    release(2)
                P.barrier()

        def xattn(l, nt):
            SC = 256 ** -0.5
            with ExitStack() as ws:
                g, g_b = gload("g_x_pre", l)
                gp, gp_b = gload("g_x_post", l)
                uT = sb("uT", [128, KC, TB], BF16, ws)
                uT_b = P.bufs(nt, "uT")
                prenorm(lambda j: h[:, j, :], h_b, nt, g, g_b, uT, uT_b, ws, "x")
                xqT = sb("xqT", [128, 8, TB], BF16, ws)
                b_xq = P.bufs(nt, "xq")
                for pc in range(2):
                    wv, wb = acquire(f"xq{l}")
                    for c in range(4):
                        proj_fm(uT, uT_b, 0, nt, wv, wb, c * 128,
                                lambda pt, pb, j0, gn, cc=pc * 4 + c: CPo("act", xqT[:, cc, j0 * 128:(j0 + gn) * 128],
                                                                          pt[:, 0:gn * 128], [pb],
                                                                          [b_xq[j] for j in range(j0, j0 + gn)]))
                    release()
                wo0, wo0b = acquire(f"xo{l}")
                wo1, wo1b = acquire(f"xo{l}")
                pf = [sb(f"xpf{i}", [128, 4, 256], F32, ws) for i in range(2)]
                pn = [sb(f"xpn{i}", [128, 4, 256], BF16, ws) for i in range(2)]
                pT = [sb(f"xpT{i}", [128, 8, 128], BF16, ws) for i in range(2)]
                xoT = [sb(f"xoT{i}", [128, 8, 128], BF16, ws) for i in range(2)]
                sm = sb("xsm", [128, 2, 12], F32, ws)
                b_pf, b_pn, b_pT, b_xo, b_sm = (P.bufs(2, "xpf"), P.bufs(2, "xpn"), P.bufs(2, "xpT"),
                                                 P.bufs(2, "xoT"), P.bufs(2, "xsm"))
                pws = mk_post_ws(ws, "x")

                def xa_tile(j):
                    q = j % 2
                    cj = slice(j * 128, (j + 1) * 128)
                    psc = [PS(), PS()]
                    for h_ in range(4):
                        pt, pb = psc[h_ // 2]
                        for dc in range(2):
                            MM(pt[:, (h_ % 2) * 256:(h_ % 2) * 256 + 256], xqT[:, 2 * h_ + dc, cj], KT[:, l, 2 * h_ + dc, :],
                               dc == 0, dc == 1, [b_xq[j], b_kv], [pb])
                    yield
                    mx = sm[:, q, 0:4]
                    rs = sm[:, q, 4:8]
                    rrs = sm[:, q, 8:12]
                    for bk in range(2):
                        pt, pb = psc[bk]
                        P.dve(lambda e, pt=pt, mx=mx, bk=bk: e.tensor_reduce(out=mx[:, 2 * bk:2 * bk + 2],
                                                                             in_=pt[:].rearrange("p (h m) -> p h m", h=2),
                                                                             axis=AX.X, op=ALU.max), [pb], [b_sm[q]])
                    TSo("dve", mx, mx, -SC, None, ALU.mult, None, [b_sm[q]], [b_sm[q]])
                    for h_ in range(4):
                        pt, pb = psc[h_ // 2]
                        ACTF(pf[q][:, h_, :], pt[:, (h_ % 2) * 256:(h_ % 2) * 256 + 256], AF.Exp, [pb, b_sm[q]], [b_pf[q], b_sm[q]],
                             scale=SC, bias=mx[:, h_:h_ + 1], accum_out=rs[:, h_:h_ + 1])
                    P.dve(lambda e, rs=rs, rrs=rrs: e.reciprocal(out=rrs, in_=rs), [b_sm[q]], [b_sm[q]])
                    TTo("pool", pn[q][:], pf[q][:], rrs.unsqueeze(2).to_broadcast([128, 4, 256]), ALU.mult,
                        [b_pf[q], b_sm[q]], [b_pn[q]])
                    yield
                    pt, pb = PS()
                    ptb16 = pt[:].bitcast(BF16)
                    for h_ in range(4):
                        for mc in range(2):
                            P.pe(lambda e, h_=h_, mc=mc, q=q, ptb16=ptb16: e.transpose(
                                out=ptb16[:, (h_ * 2 + mc) * 128:(h_ * 2 + mc + 1) * 128],
                                in_=pn[q][:, h_, mc * 128:(mc + 1) * 128], identity=ident_bf[:]), [b_pn[q], b_c], [pb])
                    CPo("act", pT[q][:], ptb16.rearrange("p (a t) -> p a t", a=8), [pb], [b_pT[q]])
                    yield
                    pso = [PS(), PS()]
                    for h_ in range(4):
                        for dc in range(2):
                            idx = 2 * h_ + dc
                            pt, pb = pso[idx // 4]
                            for mc in range(2):
                                MM(pt[:, (idx % 4) * 128:(idx % 4 + 1) * 128],
                                   VM[:, l, mc, h_ * 256 + dc * 128:h_ * 256 + (dc + 1) * 128], pT[q][:, h_ * 2 + mc, :],
                                   mc == 0, mc == 1, [b_kv, b_pT[q]], [pb])
                    for bk in range(2):
                        pt, pb = pso[bk]
                        CPo("dve", xoT[q][:, 4 * bk:4 * bk + 4, :], pt[:].rearrange("p (a t) -> p a t", a=4), [pb], [b_xo[q]])
                    yield
                    pA, pAb = PS()
                    pB, pBb = PS()
                    for kc in range(KC):
                        MM(pA[:], xoT[q][:, kc, :], wo0[:, kc, :], kc == 0, kc == KC - 1, [b_xo[q], wo0b], [pAb])
                    for kc in range(KC):
                        MM(pB[:], xoT[q][:, kc, :], wo1[:, kc, :], kc == 0, kc == KC - 1, [b_xo[q], wo1b], [pBb])
                    postnorm(pA, pAb, pB, pBb, gp, gp_b, j, pws)

                for j0_ in range(0, nt, 2):
                    interleave([xa_tile(j_) for j_ in range(j0_, min(nt, j0_ + 2))])
                release(2)
                P.barrier()

        def mlp(l, nt):
            with ExitStack() as ws:
                g, g_b = gload("g_ff_pre", l)
                gp, gp_b = gload("g_ff_post", l)
                uT = sb("uT", [128, KC, TB], BF16, ws)
                uT_b = P.bufs(nt, "uT")
                prenorm(lambda j: h[:, j, :], h_b, nt, g, g_b, uT, uT_b, ws, "f")
                hid = sb("hid", [128, 16, TB], BF16, ws)
                acc = sb("acc", [128, NTB, D], F32, ws)
                b_acc = P.bufs(nt, "acc")
                rl = [sb(f"rl{i}", [128, 512], BF16, ws) for i in range(2)]
                b_rl = P.bufs(2, "rl")
                cnt = [0]
                pws = mk_post_ws(ws, "f")
                b_hid = P.bufs(nt, "hid")
                for half in range(2):
                    for pc in range(4):
                        wv, wb = acquire(f"ff1_{l}")
                        for c in range(4):
                            ch = pc * 4 + c

                            def ev(pt, pb, j0, gn, ch=ch, b_hid=b_hid):
                                k = cnt[0] % 2
                                cnt[0] += 1
                                ACTF(rl[k][:, 0:gn * 128], pt[:, 0:gn * 128], AF.Relu, [pb], [b_rl[k]])
                                TTo("pool", hid[:, ch, j0 * 128:(j0 + gn) * 128], rl[k][:, 0:gn * 128], rl[k][:, 0:gn * 128],
                                    ALU.mult, [b_rl[k]], [b_hid[j] for j in range(j0, j0 + gn)])
                            proj_fm(uT, uT_b, 0, nt, wv, wb, c * 128, ev)
                        release()
                    w2 = [acquire(f"ff2_{l}") for _ in range(4)]
                    for j in range(nt):
                        pA, pAb = PS()
                        pB, pBb = PS()
                        for (pp, ppb, c0) in ((pA, pAb, 0), (pB, pBb, 512)):
                            for k in range(16):
                                wv, wb = w2[k // 4]
                                MM(pp[:], hid[:, k, j * 128:(j + 1) * 128], wv[:, k % 4, c0:c0 + 512], k == 0, k == 15,
                                   [b_hid[j], wb], [ppb])
                        if half == 0:
                            CPo("act", acc[:, j, 0:512], pA[:], [pAb], [b_acc[j]])
                            CPo("dve", acc[:, j, 512:1024], pB[:], [pBb], [b_acc[j]])
                        else:
                            TTo("dve", acc[:, j, 0:512], acc[:, j, 0:512], pA[:], ALU.add, [pAb, b_acc[j]], [b_acc[j]])
                            TTo("dve", acc[:, j, 512:1024], acc[:, j, 512:1024], pB[:], ALU.add, [pBb, b_acc[j]], [b_acc[j]])
                            postnorm(acc[:, j, 0:512], b_acc[j], acc[:, j, 512:1024], b_acc[j], gp, gp_b, j, pws)
                    release(4)
                P.barrier()

        def swa(nt, mbase):
            SC = 64 ** -0.5
            PIB = 3.1415925
            with ExitStack() as ws:
                g, g_b = gload("g_mix_pre", 1)
                gp, gp_b = gload("g_mix_post", 1)
                uT = sb("uT", [128, KC, TB], BF16, ws)
                uT_b = P.bufs(nt, "uT")
                prenorm(lambda j: h[:, j, :], h_b, nt, g, g_b, uT, uT_b, ws, "s")
                wq0, wq0b = acquire("qkv")
                wq1, wq1b = acquire("qkv")
                wkv, wkvb = acquire("qkv")
                wo0, wo0b = acquire("o1")
                wo1, wo1b = acquire("o1")
                ang = sb("ang", [128, NTB, 32], F32, ws)
                tmd = sb("tmd", [128, NTB, 32], F32, ws)
                cosv = sb("cosv", [128, NTB, 32], F32, ws)
                sinv = sb("sinv", [128, NTB, 32], F32, ws)
                b_rp = P.buf("rope")
                A_ = ang[:, 0:nt, :]
                TTo("dve", A_, posf[:, mbase:mbase + nt].unsqueeze(2).to_broadcast([128, nt, 32]),
                    invf[:].unsqueeze(1).to_broadcast([128, nt, 32]), ALU.mult, [b_c], [b_rp])
                kf = sb("kf", [128, NTB, 32], F32, ws)

                def red_sin(dst, shift):
                    T_ = tmd[:, 0:nt, :]
                    K_ = kf[:, 0:nt, :]
                    TSo("dve", T_, A_, shift, None, ALU.add, None, [b_rp], [b_rp])
                    TSo("dve", K_, T_, 1.0 / (2 * PI), None, ALU.mult, None, [b_rp], [b_rp])
                    TSo("dve", K_, K_, 12582912.0, None, ALU.add, None, [b_rp], [b_rp])
                    TSo("dve", K_, K_, 12582912.0, None, ALU.subtract, None, [b_rp], [b_rp])
                    STTo(T_, K_, -2 * PI, T_, ALU.mult, ALU.add, [b_rp], [b_rp])
                    TSo("dve", K_, T_, PI, 2 * PI, ALU.is_gt, ALU.mult, [b_rp], [b_rp])
                    TTo("dve", T_, T_, K_, ALU.subtract, [b_rp], [b_rp])
                    TSo("dve", K_, T_, -PI, 2 * PI, ALU.is_lt, ALU.mult, [b_rp], [b_rp])
                    TTo("dve", T_, T_, K_, ALU.add, [b_rp], [b_rp])
                    TSo("dve", T_, T_, PIB, -PIB, ALU.min, ALU.max, [b_rp], [b_rp])
                    ACTF(dst, T_, AF.Sin, [b_rp], [b_rp])

                red_sin(sinv[:, 0:nt, :], 0.0)
                red_sin(cosv[:, 0:nt, :], PI / 2)
                qkf = [sb(f"qkf{i}", [128, 20, 64], F32, ws) for i in range(2)]
                rt = [sb(f"rt{i}", [128, 20, 32], F32, ws) for i in range(4)]
                qkr = [sb(f"qkr{i}", [128, 24, 64], BF16, ws) for i in range(2)]
                qTl = [sb(f"qTl{i}", [128, 8, 128], BF16, ws) for i in range(2)]
                sf = [sb(f"sf{i}", [128, 4, 256], F32, ws) for i in range(2)]
                pn = [sb(f"spn{i}", [128, 4, 256], BF16, ws) for i in range(2)]
                pT = [sb(f"spT{i}", [128, 8, 128], BF16, ws) for i in range(2)]
                obf = [sb(f"obf{i}", [128, D], BF16, ws) for i in range(2)]
                oT = [sb(f"oT{i}", [128, 8, 128], BF16, ws) for i in range(2)]
                sm = sb("ssm", [128, 2, 20], F32, ws)
                b_qkf, b_qkr, b_qTl, b_sf, b_pn, b_pT, b_obf, b_oT, b_sm = [P.bufs(2, "sw") for _ in range(9)]
                b_rt = P.bufs(4, "rt")
                pws = mk_post_ws(ws, "s")
                gi = [0]
                for j in range(nt):
                    m = mbase + j
                    q = j % 2
                    cur, prv = m % 2, (m - 1) % 2
                    pq0, pq0b = proj_tm(uT, uT_b, j, wq0, wq0b, 0, 512)
                    pq1, pq1b = proj_tm(uT, uT_b, j, wq1, wq1b, 0, 512)
                    pkv, pkvb = proj_tm(uT, uT_b, j, wkv, wkvb, 0, 512)
                    CPo("act", qkf[q][:, 0:8, :], pq0[:].rearrange("p (a d) -> p a d", d=64), [pq0b], [b_qkf[q]])
                    CPo("act", qkf[q][:, 8:16, :], pq1[:].rearrange("p (a d) -> p a d", d=64), [pq1b], [b_qkf[q]])
                    CPo("dve", qkf[q][:, 16:20, :], pkv[:, 0:256].rearrange("p (a d) -> p a d", d=64), [pkvb], [b_qkf[q]])
                    CPo("dve", vs[:, cur, :], pkv[:, 256:512], [pkvb], [kv_b[cur]])
                    t1 = qkf[q][:, :, 0:32]
                    t2 = qkf[q][:, :, 32:64]
                    cbb = cosv[:, j, :].unsqueeze(1).to_broadcast([128, 20, 32])
                    sbb = sinv[:, j, :].unsqueeze(1).to_broadcast([128, 20, 32])
                    TTo("dve", rt[0][:], t1, cbb, ALU.mult, [b_qkf[q], b_rp], [b_rt[0]])
                    TTo("dve", rt[1][:], t2, sbb, ALU.mult, [b_qkf[q], b_rp], [b_rt[1]])
                    TTo("pool", rt[2][:], t2, cbb, ALU.mult, [b_qkf[q], b_rp], [b_rt[2]])
                    TTo("pool", rt[3][:], t1, sbb, ALU.mult, [b_qkf[q], b_rp], [b_rt[3]])
                    TTo("dve", qkr[q][:, 0:20, 0:32], rt[0][:], rt[1][:], ALU.subtract, [b_rt[0], b_rt[1]], [b_qkr[q]])
                    TTo("pool", qkr[q][:, 0:20, 32:64], rt[2][:], rt[3][:], ALU.add, [b_rt[2], b_rt[3]], [b_qkr[q]])
                    for dst, src in ((20, 17), (21, 16), (22, 19), (23, 18)):
                        CPo("pool", qkr[q][:, dst, :], qkr[q][:, src, :], [b_qkr[q]], [b_qkr[q]])
                    pt, pb = PS()
                    ptb16 = pt[:].bitcast(BF16)
                    for p_ in range(8):
                        P.pe(lambda e, p_=p_, q=q, ptb16=ptb16: e.transpose(
                            out=ptb16[:, p_ * 128:(p_ + 1) * 128],
                            in_=qkr[q][:, 2 * p_:2 * p_ + 2, :].rearrange("p a d -> p (a d)"), identity=ident_bf[:]),
                            [b_qkr[q], b_c], [pb])
                    CPo("act", qTl[q][:], ptb16.rearrange("p (a t) -> p a t", a=8), [pb], [b_qTl[q]])
                    pt, pb = PS()
                    ptb16 = pt[:].bitcast(BF16)
                    for i_ in range(4):
                        P.pe(lambda e, i_=i_, q=q, ptb16=ptb16: e.transpose(
                            out=ptb16[:, i_ * 128:(i_ + 1) * 128],
                            in_=qkr[q][:, 16 + 2 * i_:18 + 2 * i_, :].rearrange("p a d -> p (a d)"), identity=ident_bf[:]),
                            [b_qkr[q], b_c], [pb])
                    for i_, (ga, gb) in enumerate(((0, 1), (2, 3), (1, 0), (3, 2))):
                        CPo("dve", kTs[0:64, cur, ga * 2, :], ptb16[0:64, i_ * 128:(i_ + 1) * 128], [pb], [kv_b[cur]])
                        CPo("act", kTs[64:128, cur, gb * 2 + 1, :], ptb16[64:128, i_ * 128:(i_ + 1) * 128], [pb], [kv_b[cur]])
                    mask = mask_fc if m == 1 else mask_pc
                    pso = [(ps_t[6], ps_b[6]), (ps_t[7], ps_b[7])]
                    SWL = int(os.environ.get("MK_SWA", "9"))
                    if SWL < 2:
                        continue
                    for gq in range(4):
                        w_ = gi[0] % 2
                        gi[0] += 1
                        psc = [PS(), PS()]
                        for hh in range(4):
                            h_ = 4 * gq + hh
                            p_ = h_ // 2
                            o_ = (h_ % 2) * 64
                            var = gq * 2 + (1 if o_ else 0)
                            pt, pb = psc[hh // 2]
                            base = (hh % 2) * 256
                            MM(pt[:, base:base + 128], qTl[q][:, p_, :], kTs[:, prv, var, :], True, True,
                               [b_qTl[q], kv_b[prv]], [pb])
                            MM(pt[:, base + 128:base + 256], qTl[q][:, p_, :], kTs[:, cur, var, :], True, True,
                               [b_qTl[q], kv_b[cur]], [pb])
                        for bk in range(2):
                            pt, pb = psc[bk]
                            TTo("dve", sf[w_][:, 2 * bk:2 * bk + 2, :], pt[:].rearrange("p (a c) -> p a c", a=2),
                                mask[:].unsqueeze(1).to_broadcast([128, 2, 256]), ALU.add, [pb, b_c], [b_sf[w_]])
                        if SWL < 3:
                            continue
                        mx = sm[:, w_, 0:4]
                        ngm = sm[:, w_, 4:8]
                        rs = sm[:, w_, 8:12]
                        t4 = sm[:, w_, 12:16]
                        rrs = sm[:, w_, 16:20]
                        sk = sinks[:, 4 * gq:4 * gq + 4]
                        P.dve(lambda e, w_=w_, mx=mx: e.tensor_reduce(out=mx, in_=sf[w_][:], axis=AX.X, op=ALU.max),
                              [b_sf[w_]], [b_sm[w_]])
                        STTo(mx, mx, SC, sk, ALU.mult, ALU.max, [b_sm[w_], b_c], [b_sm[w_]])
                        TSo("dve", ngm, mx, -1.0, None, ALU.mult, None, [b_sm[w_]], [b_sm[w_]])
                        for hh in range(4):
                            ACTF(sf[w_][:, hh, :], sf[w_][:, hh, :], AF.Exp, [b_sf[w_], b_sm[w_]], [b_sf[w_], b_sm[w_]],
                                 scale=SC, bias=ngm[:, hh:hh + 1], accum_out=rs[:, hh:hh + 1])
                        TTo("dve", t4, sk, ngm, ALU.add, [b_sm[w_], b_c], [b_sm[w_]])
                        ACTF(t4, t4, AF.Exp, [b_sm[w_]], [b_sm[w_]])
                        TTo("dve", rs, rs, t4, ALU.add, [b_sm[w_]], [b_sm[w_]])
                        P.dve(lambda e, rs=rs, rrs=rrs: e.reciprocal(out=rrs, in_=rs), [b_sm[w_]], [b_sm[w_]])
                        TTo("pool", pn[w_][:], sf[w_][:], rrs.unsqueeze(2).to_broadcast([128, 4, 256]), ALU.mult,
                            [b_sf[w_], b_sm[w_]], [b_pn[w_]])
                        pt, pb = PS()
                        ptb16 = pt[:].bitcast(BF16)
                        for hh in range(4):
                            for kc2 in range(2):
                                P.pe(lambda e, hh=hh, kc2=kc2, w_=w_, ptb16=ptb16: e.transpose(
                                    out=ptb16[:, (hh * 2 + kc2) * 128:(hh * 2 + kc2 + 1) * 128],
                                    in_=pn[w_][:, hh, kc2 * 128:(kc2 + 1) * 128], identity=ident_bf[:]), [b_pn[w_], b_c], [pb])
                        CPo("act", pT[w_][:], ptb16.rearrange("p (a t) -> p a t", a=8), [pb], [b_pT[w_]])
                        for hh in range(4):
                            h_ = 4 * gq + hh
                            po, pob = pso[h_ // 8]
                            col = (h_ % 8) * 64
                            MM(po[:, col:col + 64], pT[w_][:, hh * 2, :], vs[:, prv, gq * 64:(gq + 1) * 64], True, False,
                               [b_pT[w_], kv_b[prv]], [pob])
                            MM(po[:, col:col + 64], pT[w_][:, hh * 2 + 1, :], vs[:, cur, gq * 64:(gq + 1) * 64], False, True,
                               [b_pT[w_], kv_b[cur]], [pob])
                    if SWL < 4:
                        continue
                    CPo("act", obf[q][:, 0:512], pso[0][0][:], [pso[0][1]], [b_obf[q]])
                    CPo("dve", obf[q][:, 512:1024], pso[1][0][:], [pso[1][1]], [b_obf[q]])
                    pt, pb = PS()
                    ptb16 = pt[:].bitcast(BF16)
                    for kc in range(KC):
                        P.pe(lambda e, kc=kc, q=q, ptb16=ptb16: e.transpose(out=ptb16[:, kc * 128:(kc + 1) * 128],
                                                                           in_=obf[q][:, kc * 128:(kc + 1) * 128],
                                                                           identity=ident_bf[:]), [b_obf[q], b_c], [pb])
                    CPo("act", oT[q][:], ptb16.rearrange("p (a t) -> p a t", a=8), [pb], [b_oT[q]])
                    pA, pAb = PS()
                    pB, pBb = PS()
                    for kc in range(KC):
                        MM(pA[:], oT[q][:, kc, :], wo0[:, kc, :], kc == 0, kc == KC - 1, [b_oT[q], wo0b], [pAb])
                    for kc in range(KC):
                        MM(pB[:], oT[q][:, kc, :], wo1[:, kc, :], kc == 0, kc == KC - 1, [b_oT[q], wo1b], [pBb])
                    postnorm(pA, pAb, pB, pBb, gp, gp_b, j, pws)
                release(5)
                P.barrier()

        def load_x(loc0, nt):
            for j in range(nt):
                DMA("pool", h[:, j, :], xloc[(loc0 + j) * 128:(loc0 + j + 1) * 128, :], [], [h_b[j]])

        STOP = int(os.environ.get("MK_STOP", "99"))
        pcs = {}
        for nm in in_names:
            pcs[nm] = acquire(nm)
        ncast_grp = -(-len(cast_q) // max(1, len(tok_groups(0, NPRE))))
        for (j0, n) in tok_groups(0, NPRE):
            if STOP < 1:
                break
            load_x(j0, n)
            cast_some(ncast_grp)
            layer0_mix(n, [(0, n)], True, pcs)
        out_ops = []
        mbase = 0
        for bi, nt in enumerate(BLOCKS):
            if STOP < 2:
                break
            load_x(NPRE + mbase, nt)
            if bi > 0:
                pcs = {nm: acquire(nm) for nm in in_names}
            if mbase == 0 and nt > 1:
                layer0_mix(nt, [(0, 1), (1, nt - 1)], False, pcs, reset_before=1)
            elif mbase == 1:
                layer0_mix(nt, [(0, nt)], False, pcs, reset_before=0)
            else:
                layer0_mix(nt, [(0, nt)], False, pcs)
            dbg_dump(0, mbase, nt)
            if STOP >= 3:
                xattn(0, nt)
            dbg_dump(1, mbase, nt)
            if STOP >= 4:
                mlp(0, nt)
            dbg_dump(2, mbase, nt)
            if STOP >= 5:
                swa(nt, mbase)
            dbg_dump(3, mbase, nt)
            if STOP >= 6:
                xattn(1, nt)
            dbg_dump(4, mbase, nt)
            if STOP >= 7:
                mlp(1, nt)
            dbg_dump(5, mbase, nt)
            for j in range(nt):
                m = mbase + j
                if m >= 1:
                    out_ops.append(DMA("pool", outd[(m - 1) * 128:m * 128, :], h[:, j, :], [h_b[j]], []))
            mbase += nt
        P.emit(out_ops)
        nc._prog_stats = P.stats
    return nc


def _host_consts():
    r = np.arange(128)[:, None]
    c = np.arange(128)[None, :]
    mprev = np.where(c > r, 0.0, NEG).astype(np.float32)
    inv = np.power(10000.0, -np.arange(32, dtype=np.float32) * 2.0 / 64).astype(np.float32)
    invf = np.broadcast_to(inv[None, :], (128, 32)).copy()
    return mprev, invf


def run_module(inputs, NOWN, NPRE, BLOCKS, dbg=False):
    x = np.asarray(inputs["x"], dtype=np.float32)
    B = x.shape[0]
    ncores = 2 * B
    S = x.shape[1]
    assert S == 2 * NOWN * 128 and NPRE == NOWN - 1 and sum(BLOCKS) == NOWN + 1
    mprev, invf = _host_consts()
    nc = build_nc(NPRE, BLOCKS, dbg=dbg)
    in_maps = []
    NLOC = NPRE + 1 + NOWN
    pos = np.asarray(inputs["positions"]).astype(np.int32)
    for c in range(ncores):
        b, s = c // 2, c % 2
        xl = np.zeros((NLOC * 128, D), np.float32)
        pl = np.zeros((NOWN + 1, 128), np.int32)
        if s == 1:
            xl[:] = x[b]
            pl[:] = pos[b].reshape(2 * NOWN, 128)[NOWN - 1:]
        else:
            xl[(NPRE + 1) * 128:] = x[b, :NOWN * 128]
            pl[1:] = pos[b].reshape(2 * NOWN, 128)[:NOWN]
        m = {"xloc": xl, "mem": np.ascontiguousarray(inputs["mem"][b], dtype=np.float32), "pos": pl,
             "flag": np.full((128, 1), float(s), np.float32),
             "mask_first": mprev if s == 1 else np.full((128, 128), NEG, np.float32),
             "invf": invf}
        for n_ in W_NAMES:
            m[n_] = np.ascontiguousarray(inputs[n_], dtype=np.float32)
        in_maps.append(m)
    res = run_bass_kernel_spmd(nc, in_maps, core_ids=list(range(ncores)))
    if os.environ.get("MK_VERBOSE"):
        print("exec_time_ns", getattr(res, "exec_time_ns", None), flush=True)
    out = np.zeros((B, S, D), np.float32)
    dbgs = None
    for c in range(ncores):
        b, s = c // 2, c % 2
        out[b, s * NOWN * 128:(s + 1) * NOWN * 128] = res.results[c]["out"]
    if dbg:
        dbgs = [res.results[c]["dbg"] for c in range(ncores)]
    return out, dbgs, nc


def kernel(**inputs):
    out, _, _ = run_module(inputs, 32, 31, [1, 4, 4, 4, 4, 4, 4, 4, 4])
    return out
```
